# Optimizing a Trainium2 kernel written in Bass

```python
import jax, jax.numpy as jnp
from jax import lax
import numpy as np

D_MODEL = 1024
BATCH = 2
SEQ = 8192
DEPTH = 1

D_MIX = D_MODEL
CONF_WIDTH = D_MIX // 2
CONF_HEADS = 8
SCONV_WIDTH = D_MIX - CONF_WIDTH
SCONV_HEADS = 8
CONF_KERNEL = 31
SCONV_KERNEL = 3
D_IN = 2 * CONF_WIDTH + 3 * SCONV_WIDTH
N_GROUPS = 4
EXPERTS_PER_GROUP = 4
N_EXPERTS = N_GROUPS * EXPERTS_PER_GROUP
TOP_K_IN_GROUP = 2
D_EXPERT = 256
D_PLE = 256
LN_EPS = 1e-5
ALPHA = (2.0 * DEPTH) ** 0.25
BETA = (8.0 * DEPTH) ** -0.25

kernel_name = "hybrid_conformer_shortconv_hiermoe_deepnorm"


def layer_norm(x, g, b):
    xf = x.astype(jnp.float32)
    mu = jnp.mean(xf, axis=-1, keepdims=True)
    var = jnp.mean(jnp.square(xf - mu), axis=-1, keepdims=True)
    y = (xf - mu) * lax.rsqrt(var + LN_EPS) * g.astype(jnp.float32) + b.astype(jnp.float32)
    return y.astype(x.dtype)


def causal_depthwise_conv(u, w, b):
    k = w.shape[0]
    y = lax.conv_general_dilated(
        u, w[:, None, :].astype(u.dtype), window_strides=(1,), padding=[(k - 1, 0)],
        dimension_numbers=("NWC", "WIO", "NWC"), feature_group_count=u.shape[-1])
    return y + b


def hybrid_mixer(x, w_in, b_in, conf_dw_w, conf_dw_b, conf_ln_g, conf_ln_b, sc_w, sc_b, w_out, b_out):
    h = x @ w_in + b_in
    splits = [CONF_WIDTH, 2 * CONF_WIDTH, 2 * CONF_WIDTH + SCONV_WIDTH, 2 * CONF_WIDTH + 2 * SCONV_WIDTH]
    a_val, a_gate, b_gate, c_gate, v = jnp.split(h, splits, axis=-1)
    a = a_val * jax.nn.sigmoid(a_gate)
    a = causal_depthwise_conv(a, conf_dw_w, conf_dw_b)
    a = jax.nn.silu(layer_norm(a, conf_ln_g, conf_ln_b))
    s = b_gate * causal_depthwise_conv(c_gate * v, sc_w, sc_b)
    return jnp.concatenate([a, s], axis=-1) @ w_out + b_out


def hierarchical_moe(x, w_rg, b_rg, w_re, b_re, w_gate, w_up, w_down):
    bsz, t, d = x.shape
    xt = x.reshape(bsz * t, d)
    g_probs = jax.nn.softmax((xt @ w_rg + b_rg).astype(jnp.float32), axis=-1)
    g_p, g_idx = lax.top_k(g_probs, 1)
    e_logits_all = (xt @ w_re + b_re).astype(jnp.float32).reshape(-1, N_GROUPS, EXPERTS_PER_GROUP)
    e_logits = jnp.take_along_axis(e_logits_all, g_idx[:, :, None], axis=1)[:, 0]
    e_p, e_idx = lax.top_k(jax.nn.softmax(e_logits, axis=-1), TOP_K_IN_GROUP)
    e_p = e_p / jnp.sum(e_p, axis=-1, keepdims=True)
    weights = g_p * e_p
    expert_ids = g_idx * EXPERTS_PER_GROUP + e_idx
    combine = jnp.sum(jax.nn.one_hot(expert_ids, N_EXPERTS, dtype=jnp.float32) * weights[..., None], axis=1)
    combine = combine.astype(x.dtype)
    hg = jnp.einsum("nd,edf->nef", xt, w_gate)
    hu = jnp.einsum("nd,edf->nef", xt, w_up)
    hidden = jax.nn.silu(hg) * hu * combine[:, :, None]
    y = jnp.einsum("nef,efd->nd", hidden, w_down)
    return y.reshape(bsz, t, d)


def setup_inputs(seed: int = 0) -> dict:
    key = jax.random.key(seed)
    ks = jax.random.split(key, 32)
    f32 = jnp.float32
    nrm = lambda k, shape, s: jax.random.normal(k, shape, f32) * s
    L = DEPTH
    return {
        "x": nrm(ks[0], (BATCH, SEQ, D_MODEL), 1.0),
        "p": nrm(ks[1], (DEPTH, BATCH, SEQ, D_PLE), 1.0),
        "ln_in_g": 1.0 + nrm(ks[2], (D_MODEL,), 0.02),
        "ln_in_b": nrm(ks[3], (D_MODEL,), 0.02),
        "w_in": nrm(ks[4], (L, D_MODEL, D_IN), D_MODEL ** -0.5),
        "b_in": nrm(ks[5], (L, D_IN), 0.02),
        "conf_dw_w": nrm(ks[6], (L, CONF_KERNEL, CONF_WIDTH), CONF_KERNEL ** -0.5),
        "conf_dw_b": nrm(ks[7], (L, CONF_WIDTH), 0.02),
        "conf_ln_g": 1.0 + nrm(ks[8], (L, CONF_WIDTH), 0.02),
        "conf_ln_b": nrm(ks[9], (L, CONF_WIDTH), 0.02),
        "sc_w": nrm(ks[10], (L, SCONV_KERNEL, SCONV_WIDTH), SCONV_KERNEL ** -0.5),
        "sc_b": nrm(ks[11], (L, SCONV_WIDTH), 0.02),
        "w_out": nrm(ks[12], (L, D_MIX, D_MODEL), BETA * D_MIX ** -0.5),
        "b_out": nrm(ks[13], (L, D_MODEL), 0.02),
        "ln1_g": 1.0 + nrm(ks[14], (L, D_MODEL), 0.02),
        "ln1_b": nrm(ks[15], (L, D_MODEL), 0.02),
        "w_rg": nrm(ks[16], (L, D_MODEL, N_GROUPS), D_MODEL ** -0.5),
        "b_rg": nrm(ks[17], (L, N_GROUPS), 0.01),
        "w_re": nrm(ks[18], (L, D_MODEL, N_EXPERTS), D_MODEL ** -0.5),
        "b_re": nrm(ks[19], (L, N_EXPERTS), 0.01),
        "w_gate": nrm(ks[20], (L, N_EXPERTS, D_MODEL, D_EXPERT), D_MODEL ** -0.5),
        "w_up": nrm(ks[21], (L, N_EXPERTS, D_MODEL, D_EXPERT), D_MODEL ** -0.5),
        "w_down": nrm(ks[22], (L, N_EXPERTS, D_EXPERT, D_MODEL), BETA * D_EXPERT ** -0.5),
        "w_pg": nrm(ks[23], (L, D_MODEL, D_MODEL), D_MODEL ** -0.5),
        "b_pg": nrm(ks[24], (L, D_MODEL), 0.02),
        "w_pp": nrm(ks[25], (L, D_PLE, D_MODEL), BETA * D_PLE ** -0.5),
        "ln2_g": 1.0 + nrm(ks[26], (L, D_MODEL), 0.02),
        "ln2_b": nrm(ks[27], (L, D_MODEL), 0.02),
    }


def reference(x, p, ln_in_g, ln_in_b, w_in, b_in, conf_dw_w, conf_dw_b, conf_ln_g, conf_ln_b,
              sc_w, sc_b, w_out, b_out, ln1_g, ln1_b, w_rg, b_rg, w_re, b_re,
              w_gate, w_up, w_down, w_pg, b_pg, w_pp, ln2_g, ln2_b):
    x = layer_norm(x, ln_in_g, ln_in_b)
    for i in range(DEPTH):
        mix = hybrid_mixer(x, w_in[i], b_in[i], conf_dw_w[i], conf_dw_b[i], conf_ln_g[i], conf_ln_b[i],
                           sc_w[i], sc_b[i], w_out[i], b_out[i])
        x = layer_norm(ALPHA * x + mix, ln1_g[i], ln1_b[i])
        r = ALPHA * x + hierarchical_moe(x, w_rg[i], b_rg[i], w_re[i], b_re[i], w_gate[i], w_up[i], w_down[i])
        gate = jax.nn.sigmoid(r @ w_pg[i] + b_pg[i])
        x = layer_norm(r + gate * (p[i] @ w_pp[i]), ln2_g[i], ln2_b[i])
    return x
```

```python
import contextlib
import os
import numpy as np
import concourse.bass as bass
import concourse.mybir as mybir
from concourse.bass_utils import run_bass_kernel_spmd

F32 = mybir.dt.float32
I32 = mybir.dt.int32
BF16 = mybir.dt.bfloat16
AF = mybir.ActivationFunctionType
ALU = mybir.AluOpType
AX = mybir.AxisListType

NCORES = 8
NT = 2048
HALO = 32
D = 1024
DIN = 2560
NE = 16
DE = 256
DPLE = 256
XHW = 1072
EPS = 1e-5
ALPHA = 2.0 ** 0.25
BIG = 1.0e4
ENGS = ["pe", "act", "dve", "pool", "sp"]

PP_BIN = 0
PP_LNG = 20
PP_LNB = 28
PP_DWW = 36
PP_DWB = 160
PP_CLG = 164
PP_CLB = 168
PP_SCW = 172
PP_SCB = 184
PP_MASK = 188
PP_N = 189
R_LNIG, R_LNIB, R_L1G, R_L1B, R_L2G, R_L2B, R_BOUT, R_BPG = [i * 1024 for i in range(8)]


class Sched:
    def __init__(self, nc, stack):
        self.nc = nc
        self.stack = stack
        self.prog = {e: [] for e in ENGS}
        self.sem = {}
        self.cnt = {}
        self.seen = {e: {} for e in ENGS}
        self.res = {}
        self.in_cond = False
        self.regs = {}
        self.dummy = {}
        self.dummy_ctr = {}
        for e in ENGS:
            self.new_sem(e)

    def new_sem(self, key):
        if key not in self.sem:
            self.sem[key] = self.stack.enter_context(self.nc.semaphore("s_" + key))
            self.cnt[key] = 0
        return key

    def _deps(self, e, reads, writes):
        deps = {}

        def add(k, v):
            if deps.get(k, 0) < v:
                deps[k] = v

        for r in reads:
            st = self.res.get(r)
            if st:
                for k, v in st["w"].items():
                    add(k, v)
        for w in writes:
            st = self.res.get(w)
            if st:
                for k, v in st["w"].items():
                    add(k, v)
                for k, v in st["r"].items():
                    add(k, v)
        waits = []
        for k, v in deps.items():
            if k == e and e in ("pe", "sp"):
                continue
            if self.seen[e].get(k, 0) >= v:
                continue
            self.seen[e][k] = v
            waits.append((k, v))
        return waits

    def _commit(self, me, reads, writes):
        for r in reads:
            st = self.res.setdefault(r, {"w": {}, "r": {}})
            if st["r"].get(me[0], 0) < me[1]:
                st["r"][me[0]] = me[1]
        for w in writes:
            if self.in_cond:
                st = self.res.setdefault(w, {"w": {}, "r": {}})
                if st["w"].get(me[0], 0) < me[1]:
                    st["w"][me[0]] = me[1]
            else:
                self.res[w] = {"w": {me[0]: me[1]}, "r": {}}

    def op(self, e, fn, reads=(), writes=()):
        waits = self._deps(e, reads, writes)
        self.cnt[e] += 1
        me = (e, self.cnt[e])
        self.prog[e].append((waits, fn, (e, 1)))
        self._commit(me, reads, writes)
        return me

    def dma(self, q, semkey, out, in_, reads=(), writes=()):
        if semkey is None:
            self.uniq = getattr(self, "uniq", 0) + 1
            semkey = "du%d" % self.uniq
        self.new_sem(semkey)
        waits = self._deps(q, reads, writes)
        self.cnt[semkey] += 16
        me = (semkey, self.cnt[semkey])
        self.prog[q].append((waits, lambda eng: eng.dma_start(out=out, in_=in_), (semkey, 16)))
        self._commit(me, reads, writes)
        return me

    def dma_fn(self, q, semkey, fn, reads=(), writes=()):
        self.new_sem(semkey)
        waits = self._deps(q, reads, writes)
        self.cnt[semkey] += 16
        me = (semkey, self.cnt[semkey])
        self.prog[q].append((waits, fn, (semkey, 16)))
        self._commit(me, reads, writes)
        return me

    def raw(self, e, fn, reads=()):
        waits = self._deps(e, reads, ())
        self.prog[e].append((waits, fn, None))

    def cond_begin(self, flag):
        assert not self.in_cond
        self.in_cond = True
        self.seen_saved = {e: dict(d) for e, d in self.seen.items()}
        self.cond_info = {"snap": dict(self.cnt), "ext": {}}
        for e in ENGS:
            self.prog[e].append(("cond_begin", flag, self.cond_info))

    def cond_end(self):
        self.in_cond = False
        self.seen = self.seen_saved
        info = self.cond_info
        snap = info["snap"]
        for e in ENGS:
            prog = self.prog[e]
            i = len(prog) - 1
            while prog[i][0] != "cond_begin":
                for k, v in prog[i][0]:
                    if v <= snap.get(k, 0) and info["ext"].get(k, 0) < v:
                        info["ext"][k] = v
                i -= 1
            self.prog[e].append(("cond_end", None, None))

    def barrier(self):
        for e in ENGS:
            waits = []
            for k, v in self.cnt.items():
                if v == 0 or k == e:
                    continue
                if self.seen[e].get(k, 0) >= v:
                    continue
                self.seen[e][k] = v
                waits.append((k, v))
            self.prog[e].append((waits, None, None))
        self.res = {}

    def final_wait(self, e, semkeys):
        waits = [(k, self.cnt[k]) for k in semkeys if self.cnt.get(k, 0) > 0]
        self.prog[e].append((waits, None, None))

    def emit(self):
        nc = self.nc
        with nc.Block() as block:
            def replay(e, eng):
                prog = self.prog[e]
                i = 0
                while i < len(prog):
                    waits, fn, inc = prog[i]
                    if waits == "cond_begin":
                        j = i + 1
                        while prog[j][0] != "cond_end":
                            j += 1
                        body = prog[i + 1:j]
                        n_inc = sum(1 for b in body if b[2] is not None and b[2][0] == e)
                        if body:
                            cm = eng.If(((self.regs[e] >> fn) & 1) != 0)
                            cm.__enter__()
                            for w_, f_, inc_ in body:
                                for k, v in w_:
                                    eng.wait_ge(self.sem[k], v)
                                if f_ is not None:
                                    ins = f_(eng)
                                    if inc_ is not None:
                                        ins.then_inc(self.sem[inc_[0]], inc_[1])
                            cm.__exit__(None, None, None)
                            if n_inc > 0:
                                info = inc
                                cm2 = eng.Else()
                                cm2.__enter__()
                                self.dummy_ctr[e] = self.dummy_ctr.get(e, 0) + 1
                                self.dummy[e](eng, self.dummy_ctr[e]).then_inc(self.sem[e], n_inc)
                                cm2.__exit__(None, None, None)
                        i = j + 1
                        continue
                    for k, v in waits:
                        eng.wait_ge(self.sem[k], v)
                    if fn is not None:
                        ins = fn(eng)
                        if inc is not None:
                            ins.then_inc(self.sem[inc[0]], inc[1])
                    i += 1
                self.prog[e] = []

            @block.tensor
            def _(eng):
                replay("pe", eng)

            @block.scalar
            def _(eng):
                replay("act", eng)

            @block.vector
            def _(eng):
                replay("dve", eng)

            @block.gpsimd
            def _(eng):
                replay("pool", eng)

            @block.sync
            def _(eng):
                replay("sp", eng)


def build_nc(stop_after=None):
    nc = bass.Bass("TRN2", target_bir_lowering=False)
    dt_in = lambda n, s: nc.dram_tensor(n, s, F32, kind="ExternalInput").ap()
    xs = dt_in("xs", [NT + HALO, D])
    ps = dt_in("ps", [NT, DPLE])
    pp = dt_in("pp", [128, PP_N])
    rows = dt_in("rows", [1, 8192])
    w_in = dt_in("w_in", [D, DIN])
    w_out = dt_in("w_out", [D, D])
    w_r = dt_in("w_r", [D, 20])
    b_r = dt_in("b_r", [20, 1])
    w_gate = dt_in("w_gate", [NE, D, DE])
    w_up = dt_in("w_up", [NE, D, DE])
    w_down = dt_in("w_down", [NE, DE, D])
    w_pg = dt_in("w_pg", [D, D])
    w_pp = dt_in("w_pp", [DPLE, D])
    ident = dt_in("ident", [128, 128])
    tri = dt_in("tri", [128, 128])
    tid = nc.dram_tensor("tid", [128, 16], I32, kind="ExternalInput").ap()
    out = nc.dram_tensor("out", [NT, D], F32, kind="ExternalOutput").ap()
    x1_dram = nc.dram_tensor("x1_spill", [NT, D], F32).ap()
    xh_dram = nc.dram_tensor("xh_spill", [NT, XHW], BF16).ap()
    inv_dram = nc.dram_tensor("inv_spill", [NT, 1], I32).ap()
    flags_dram = nc.dram_tensor("flags_spill", [1, 16], I32).ap()
    dbg = nc.dram_tensor("dbg", [128, 64], F32, kind="ExternalOutput").ap() if os.environ.get("K_DBG") else None

    w_in_v = w_in.rearrange("(dc p) f -> p dc f", p=128)
    w_out_v = w_out.rearrange("(dc p) f -> p dc f", p=128)
    w_pg_v = w_pg.rearrange("(dc p) f -> p dc f", p=128)
    w_pp_v = w_pp.rearrange("(dc p) f -> p dc f", p=128)
    w_r_v = w_r.rearrange("(dc p) f -> p dc f", p=128)

    with contextlib.ExitStack() as st:
        S = Sched(nc, st)

        def sb(stack, n, s, d):
            return stack.enter_context(nc.sbuf_tensor(n, s, d))

        banks = [st.enter_context(nc.psum_tensor("pb%d" % i, [128, 512], F32)) for i in range(8)]
        bank_ctr = [0]

        nbanks = [8]

        def nb():
            i = bank_ctr[0] % nbanks[0]
            bank_ctr[0] += 1
            return banks[i], "pb%d" % i

        def mm(o, l, r, start, stop, reads, writes):
            S.op("pe", lambda e: e.matmul(o, l, r, start=start, stop=stop), reads, writes)

        def tp(o, i_, idn, reads, writes):
            S.op("pe", lambda e: e.transpose(o, i_, idn), reads, writes)

        def act(o, i_, f, reads, writes, bias=0.0, scale=1.0):
            S.op("act", lambda e: e.activation(o, i_, f, bias=bias, scale=scale), reads, writes)

        def acp(o, i_, reads, writes):
            S.op("act", lambda e: e.copy(o, i_), reads, writes)

        def tt_(eng, o, a, b, op, reads, writes):
            S.op(eng, lambda e: e.tensor_tensor(o, a, b, op), reads, writes)

        def ts_(o, a, s1, s2, op0, op1, reads, writes):
            if s2 is None:
                S.op("dve", lambda e: e.tensor_scalar(o, a, s1, None, op0), reads, writes)
            else:
                S.op("dve", lambda e: e.tensor_scalar(o, a, s1, s2, op0, op1), reads, writes)

        def stt(o, a, sc, b, op0, op1, reads, writes):
            S.op("dve", lambda e: e.scalar_tensor_tensor(o, a, sc, b, op0, op1), reads, writes)

        def cp(eng, o, i_, reads, writes):
            S.op(eng, lambda e: e.tensor_copy(o, i_), reads, writes)

        def V(f, r, w):
            S.op("dve", f, r, w)

        def pipeline(stages, n):
            last = max(off for off, _ in stages)
            for t in range(n + last):
                for off, fn in stages:
                    i = t - off
                    if 0 <= i < n:
                        fn(i)

        idf = sb(st, "idf", [128, 128], F32)
        idb = sb(st, "idb", [128, 128], BF16)
        ones_f = sb(st, "ones_f", [128, 128], F32)
        ones_rb = sb(st, "ones_rb", [1, 128], BF16)
        pps = sb(st, "pps", [128, PP_N], F32)
        brow_b = sb(st, "brow_b", [1, 2048], BF16)
        mv_in = sb(st, "mv_in", [128, 17, 2], F32)
        rs_in = sb(st, "rs_in", [128, 17], F32)
        ohg_all = sb(st, "ohg_all", [128, 16, 4], F32)
        inv_sb = sb(st, "inv_sb", [128, 16], I32)
        big = sb(st, "big", [128, 16384], F32)
        x1T = sb(st, "x1T", [128, 8, NT], BF16)
        wob = sb(st, "wob", [128, 8, 1024], BF16)

        def ppc(c, n=1):
            return pps[:, c:c + n]

        S.dma("sp", None, idf[:], ident, writes=["idf"])
        S.dma("sp", None, pps[:], pp, writes=["pps"])
        S.dma("pool", None, brow_b[:], rows[:, R_BOUT:R_BOUT + 2048], writes=["brow"])
        cp("dve", idb[:], idf[:], ["idf"], ["idb"])
        S.op("dve", lambda e: e.memset(ones_f[:], 1.0), (), ["ones_f"])
        S.op("dve", lambda e: e.memset(ones_rb[:], 1.0), (), ["ones_rb"])

        catT = big[:, 0:8192].bitcast(BF16).rearrange("p (c t) -> p c t", c=8)

        with contextlib.ExitStack() as s1:
            diag = x1T[:, :, :].rearrange("p c t -> p (c t)")[:, 0:4 * 31 * 128].rearrange(
                "p (c k j) -> p c k j", c=4, k=31)
            NWS = 3
            wslab = [sb(s1, "wslab%d" % i, [128, 8, 384], BF16) for i in range(NWS)]
            xcb = sb(s1, "xcb", [128, 4, 1024], BF16)
            xnT = [sb(s1, "xnT%d" % i, [128, 8, 512], BF16) for i in range(2)]
            xnTh = sb(s1, "xnTh", [128, 8, 32], BF16)
            xth = xcb[0:32, 0:2, :].rearrange("p a b -> p (a b)").bitcast(F32)
            xcbh = xcb[0:32, 2, :]
            sth = sb(s1, "sth", [128, 5, 2, 6], F32)
            glu = sb(s1, "glu", [128, 4, 544], BF16)
            cv = sb(s1, "cv", [128, 4, 514], F32)
            NTMP = 8
            tmp = [sb(s1, "tmp%d" % i, [128, 512], F32) for i in range(NTMP)]
            htmp = [sb(s1, "htmp%d" % i, [128, 32], F32) for i in range(4)]
            c_mean = sb(s1, "c_mean", [128, 512], F32)
            c_var = sb(s1, "c_var", [128, 512], F32)
            xt4 = big[:, 8192:12288].rearrange("p (j d) -> p j d", j=4)
            confy = big[:, 12288:14336].rearrange("p (c t) -> p c t", c=4)
            sq = big[:, 14336:16384].rearrange("p (c t) -> p c t", c=4)
            tctr = [0]

            def ntmp():
                i = tctr[0] % NTMP
                tctr[0] += 1
                return tmp[i], "tmp%d" % i

            def maybe_stop(tag):
                if stop_after == tag:
                    S.barrier()
                    S.emit()
                    return True
                return False

            def build_diag(c):
                S.op("dve", lambda e: e.tensor_tensor(
                    diag[:, c, :, :], idf[:].unsqueeze(1).to_broadcast([128, 31, 128]),
                    pps[:, PP_DWW + c * 31:PP_DWW + (c + 1) * 31].unsqueeze(2).to_broadcast([128, 31, 128]),
                    ALU.mult), ["idf", "pps"], ["diag%d" % c])

            S.dma("sp", "d_xh", xth, xs[0:HALO, :], writes=["xcb0", "xcb1"])
            V(lambda e: e.bn_stats(sth[0:32, 4, 0, :], xth[:, 0:512]), ["xcb0", "xcb1"], ["sth4a"])
            V(lambda e: e.bn_stats(sth[0:32, 4, 1, :], xth[:, 512:1024]), ["xcb0", "xcb1"], ["sth4b"])
            V(lambda e: e.bn_aggr(mv_in[0:32, 16, :], sth[0:32, 4, :, :]), ["sth4a", "sth4b"], ["mvh"])
            act(rs_in[0:32, 16:17], mv_in[0:32, 16, 1:2], AF.Sqrt, ["mvh"], ["rsh0"], bias=EPS)
            V(lambda e: e.reciprocal(rs_in[0:32, 16:17], rs_in[0:32, 16:17]), ["rsh0"], ["rsh"])
            ts_(xcbh, xth, mv_in[0:32, 16, 0:1], rs_in[0:32, 16:17], ALU.subtract, ALU.mult,
                ["xcb0", "xcb1", "mvh", "rsh"], ["xcb2"])
            bk, bkk = nb()
            bkb = bk[:].bitcast(BF16)
            for dc in range(8):
                tp(bkb[:, dc * 32:(dc + 1) * 32], xcbh[:, dc * 128:(dc + 1) * 128], idb[0:32, 0:32],
                   ["xcb2", "idb"], [bkk])
            for dc in range(8):
                act(xnTh[:, dc, :], bkb[:, dc * 32:(dc + 1) * 32], AF.Identity, [bkk, "pps"], ["xnTh"],
                    bias=ppc(PP_LNB + dc), scale=ppc(PP_LNG + dc))

            def stepA_load(tt, j):
                g = tt * 4 + j
                S.dma("sp", "d_xt%d" % j, xt4[:, j, :], xs[HALO + g * 128:HALO + (g + 1) * 128, :],
                      writes=["xt%d" % j])
                V(lambda e: e.bn_stats(sth[:, j, 0, :], xt4[:, j, 0:512]), ["xt%d" % j], ["sth%da" % j])
                V(lambda e: e.bn_stats(sth[:, j, 1, :], xt4[:, j, 512:1024]), ["xt%d" % j], ["sth%db" % j])
                V(lambda e: e.bn_aggr(mv_in[:, g, :], sth[:, j, :, :]), ["sth%da" % j, "sth%db" % j], ["mv%d" % g])

            def stepA_norm(tt):
                g0 = tt * 4
                act(rs_in[:, g0:g0 + 4], mv_in[:, g0:g0 + 4, 1], AF.Sqrt, ["mv%d" % (g0 + j) for j in range(4)],
                    ["rs0_%d" % tt], bias=EPS)
                V(lambda e: e.reciprocal(rs_in[:, g0:g0 + 4], rs_in[:, g0:g0 + 4]), ["rs0_%d" % tt], ["rs_%d" % tt])
                for j in range(4):
                    g = g0 + j
                    ts_(xcb[:, j, :], xt4[:, j, :], mv_in[:, g, 0:1], rs_in[:, g:g + 1], ALU.subtract, ALU.mult,
                        ["xt%d" % j, "mv%d" % g, "rs_%d" % tt], ["xcb%d" % j])

            def stepA_tr(tt, dcs):
                xn = xnT[tt % 2]
                xnk = "xnT%d" % (tt % 2)
                for dc in dcs:
                    bk, bkk = nb()
                    bkb = bk[:].bitcast(BF16)
                    for j in range(4):
                        tp(bkb[:, j * 128:(j + 1) * 128], xcb[:, j, dc * 128:(dc + 1) * 128], idb[:],
                           ["xcb%d" % j, "idb"], [bkk])
                    act(xn[:, dc, :], bkb[:, 0:512], AF.Identity, [bkk, "pps"], [xnk + "_%d" % dc],
                        bias=ppc(PP_LNB + dc), scale=ppc(PP_LNG + dc))

            def load_slab(s):
                if s >= 32:
                    return
                slot = s % NWS
                if s % 8 < 4:
                    c0, ncol = (s % 8) * 256, 256
                else:
                    c0, ncol = 1024 + (s % 8 - 4) * 384, 384
                S.dma("pool", "d_ws%d" % slot, wslab[slot][:, :, 0:ncol], w_in_v[:, :, c0:c0 + ncol],
                      writes=["ws%d" % slot])

            def conv_job(tt, i):
                bc_, bck = nb()
                for k in range(31):
                    mm(bc_[:, :], diag[:, i, k, :], glu[:, i, k + 2:k + 2 + 512], k == 0, k == 30,
                       ["diag%d" % i, "glu%d" % i], [bck])
                act(confy[:, i, :], bc_[:], AF.Identity, [bck, "pps"], ["confy%d" % i], bias=ppc(PP_DWB + i))
                act(sq[:, i, :], bc_[:], AF.Square, [bck, "pps"], ["sq%d" % i], bias=ppc(PP_DWB + i))
                if tt < 3:
                    cp("pool", glu[:, i, 0:32], glu[:, i, 512:544], ["glu%d" % i], ["glu%d" % i])

            def slab(tt, sl):
                s = tt * 8 + sl
                slot = s % NWS
                ws = wslab[slot]
                wsk = "ws%d" % slot
                xn = xnT[tt % 2]
                xn_reads = ["xnT%d_%d" % (tt % 2, dc) for dc in range(8)]
                tcols = slice(tt * 512, (tt + 1) * 512)

                def proj(coff):
                    bk_, bkk_ = nb()
                    for dc in range(8):
                        mm(bk_[:, :], ws[:, dc, coff:coff + 128], xn[:, dc, :], dc == 0, dc == 7,
                           [wsk] + xn_reads, [bkk_])
                    return bk_, bkk_

                def proj_h(coff, bank, bcol):
                    bk_, bkk_ = bank
                    for dc in range(8):
                        mm(bk_[:, bcol:bcol + 32], ws[:, dc, coff:coff + 128], xnTh[:, dc, :],
                           dc == 0, dc == 7, [wsk, "xnTh"], [bkk_])

                if sl < 4:
                    i = sl
                    bv, bvk = proj(0)
                    bg, bgk = proj(128)
                    if tt == 0:
                        hb = nb()
                        proj_h(0, hb, 0)
                        proj_h(128, hb, 32)
                    sig, sigk = ntmp()
                    act(sig[:], bg[:], AF.Sigmoid, [bgk, "pps"], [sigk], bias=ppc(PP_BIN + 2 * i + 1))
                    stt(glu[:, i, 32:544], bv[:], ppc(PP_BIN + 2 * i), sig[:], ALU.add, ALU.mult,
                        [bvk, sigk, "pps"], ["glu%d" % i])
                    if tt == 0:
                        act(htmp[0][:], hb[0][:, 32:64], AF.Sigmoid, [hb[1], "pps"], ["ht0"],
                            bias=ppc(PP_BIN + 2 * i + 1))
                        stt(htmp[1][:], hb[0][:, 0:32], ppc(PP_BIN + 2 * i), htmp[0][:], ALU.add, ALU.mult,
                            [hb[1], "ht0", "pps"], ["ht1"])
                        ts_(glu[:, i, 0:32], htmp[1][:], ppc(PP_MASK), None, ALU.mult, ALU.bypass,
                            ["ht1", "pps"], ["glu%d" % i])
                else:
                    i = sl - 4
                    cb_ = PP_BIN + 8 + 3 * i
                    bC, bCk = proj(0)
                    bV, bVk = proj(128)
                    bB, bBk = proj(256)
                    if tt == 0:
                        hb = nb()
                        proj_h(0, hb, 0)
                        proj_h(128, hb, 32)
                    vv, vvk = ntmp()
                    act(vv[:], bV[:], AF.Identity, [bVk, "pps"], [vvk], bias=ppc(cb_ + 1))
                    stt(cv[:, i, 2:514], bC[:], ppc(cb_), vv[:], ALU.add, ALU.mult, [bCk, vvk, "pps"], ["cv%d" % i])
                    if tt == 0:
                        act(htmp[2][:], hb[0][:, 32:64], AF.Identity, [hb[1], "pps"], ["ht2"], bias=ppc(cb_ + 1))
                        stt(htmp[3][:], hb[0][:, 0:32], ppc(cb_), htmp[2][:], ALU.add, ALU.mult,
                            [hb[1], "ht2", "pps"], ["ht3"])
                        ts_(cv[:, i, 0:2], htmp[3][:, 30:32], ppc(PP_MASK), None, ALU.mult, ALU.bypass,
                            ["ht3", "pps"], ["cv%d" % i])
                    tc, tck = ntmp()
                    w0 = PP_SCW + i * 3
                    ts_(tc[:], cv[:, i, 2:514], ppc(w0 + 2), ppc(PP_SCB + i), ALU.mult, ALU.add,
                        ["cv%d" % i, "pps"], [tck])
                    stt(tc[:], cv[:, i, 1:513], ppc(w0 + 1), tc[:], ALU.mult, ALU.add, ["cv%d" % i, "pps", tck], [tck])
                    stt(tc[:], cv[:, i, 0:512], ppc(w0), tc[:], ALU.mult, ALU.add, ["cv%d" % i, "pps", tck], [tck])
                    stt(catT[:, 4 + i, tcols], bB[:], ppc(cb_ + 2), tc[:], ALU.add, ALU.mult,
                        [bBk, tck, "pps"], ["cat%d_%d" % (4 + i, tt)])
                    if tt < 3:
                        cp("pool", cv[:, i, 0:2], cv[:, i, 512:514], ["cv%d" % i], ["cv%d" % i])

            cst = {}

            def stepC1(tt):
                b1_, b1k = nb()
                b2_, b2k = nb()
                for i in range(4):
                    mm(b1_[:, :], ones_f[:], confy[:, i, :], i == 0, i == 3, ["ones_f", "confy%d" % i], [b1k])
                for i in range(4):
                    mm(b2_[:, :], ones_f[:], sq[:, i, :], i == 0, i == 3, ["ones_f", "sq%d" % i], [b2k])
                mean, meank = c_mean, "c_mean"
                m2, m2k = ntmp()
                var, vark = c_var, "c_var"
                ts_(mean[:], b1_[:], 1.0 / 512, None, ALU.mult, ALU.bypass, [b1k], [meank])
                tt_("pool", m2[:], mean[:], mean[:], ALU.mult, [meank], [m2k])
                stt(var[:], b2_[:], 1.0 / 512, m2[:], ALU.mult, ALU.subtract, [b2k, m2k], [vark])
                act(var[:], var[:], AF.Sqrt, [vark], [vark], bias=EPS)
                V(lambda e: e.reciprocal(var[:], var[:]), [vark], [vark])
                cst[tt] = (mean, meank, var, vark, [(confy[:, i, :], "confy%d" % i) for i in range(4)],
                           [(sq[:, i, :], "sq%d" % i) for i in range(4)])

            def stepC2(tt):
                mean, meank, var, vark, tas, sgs = cst[tt]
                for i in range(4):
                    ta, tak = tas[i]
                    tt_("pool", ta, confy[:, i, :], mean[:], ALU.subtract, ["confy%d" % i, meank], [tak])
                for i in range(4):
                    ta, tak = tas[i]
                    tt_("dve", ta, ta, var[:], ALU.mult, [tak, vark], [tak])

            def stepC3(tt):
                tcols = slice(tt * 512, (tt + 1) * 512)
                mean, meank, var, vark, tas, sgs = cst[tt]
                for i in range(4):
                    ta, tak = tas[i]
                    sg, sgk = sgs[i]
                    act(sg, ta, AF.Sigmoid, [tak, "pps"], [sgk], bias=ppc(PP_CLB + i), scale=ppc(PP_CLG + i))
                for i in range(4):
                    ta, tak = tas[i]
                    sg, sgk = sgs[i]
                    ts_(ta, ta, ppc(PP_CLG + i), ppc(PP_CLB + i), ALU.mult, ALU.add, [tak, "pps", sgk], [tak])
                for i in range(4):
                    ta, tak = tas[i]
                    sg, sgk = sgs[i]
                    tt_("pool", catT[:, i, tcols], ta, sg, ALU.mult, [tak, sgk], ["cat%d_%d" % (i, tt)])

            load_slab(0)
            load_slab(1)
            for j in range(4):
                stepA_load(0, j)
            stepA_norm(0)
            stepA_tr(0, range(8))
            for tt in range(4):
                pend = None
                for sl in range(8):
                    load_slab(tt * 8 + sl + 2)
                    if tt == 0 and sl < 4:
                        build_diag(sl)
                    slab(tt, sl)
                    if pend is not None:
                        conv_job(*pend)
                    pend = (tt, sl) if sl < 4 else None
                    if tt == 1 and sl == 0:
                        S.dma("pool", "d_wob", wob[:], w_out_v, writes=["wob"])
                    if tt < 3:
                        if sl < 4:
                            stepA_load(tt + 1, sl)
                        elif sl == 4:
                            stepA_norm(tt + 1)
                        elif sl == 6:
                            stepA_tr(tt + 1, range(0, 4))
                        elif sl == 7:
                            stepA_tr(tt + 1, range(4, 8))
                    if sl == 5:
                        stepC1(tt)
                    elif sl == 6:
                        stepC2(tt)
                    elif sl == 7:
                        stepC3(tt)
            S.barrier()
            S.emit()
        if stop_after == "1a":
            return nc

        combT = sb(st, "combT", [48, NT], BF16)
        sM = st.enter_context(contextlib.ExitStack())
        NSLOT = 3
        wg = [None] * NSLOT
        wu = [None] * NSLOT
        NWD = 4
        wd = [None] * NWD
        wg[0] = sb(sM, "wg0", [128, 8, DE], BF16)
        wu[0] = sb(sM, "wu0", [128, 8, DE], BF16)
        wd[0] = sb(sM, "wd0", [128, 2, D], BF16)

        def load_expert(e_):
            slot = e_ % NSLOT
            S.dma("pool", "d_mwg%d" % slot, wg[slot][:], w_gate[e_].rearrange("(dc p) f -> p dc f", p=128), writes=["wg%d" % slot])
            S.dma("pool", "d_mwu%d" % slot, wu[slot][:], w_up[e_].rearrange("(dc p) f -> p dc f", p=128), writes=["wu%d" % slot])
            S.dma("pool", "d_mwd%d" % (e_ % NWD), wd[e_ % NWD][:], w_down[e_].rearrange("(fc p) d -> p fc d", p=128),
                  writes=["wd%d" % (e_ % NWD)])

        with contextlib.ExitStack() as s2:
            bc_ag = sb(s2, "bc_ag", [128, 1024], F32)
            bc_g1 = sb(s2, "bc_g1", [128, 1024], F32)
            bc_b1 = sb(s2, "bc_b1", [128, 1024], F32)
            NH = 3
            hib = [sb(s2, "hib%d" % i, [128, 1024], BF16) for i in range(NH)]
            lob = [sb(s2, "lob%d" % i, [128, 1024], BF16) for i in range(NH)]
            x1loT = [sb(s2, "x1loT%d" % i, [128, 8, 512], BF16) for i in range(2)]
            r1x = sb(s2, "r1x", [128, 4, 1024], F32)
            wr_f = sb(s2, "wr_f", [128, 8, 20], F32)
            wr_hi = sb(s2, "wr_hi", [128, 8, 20], BF16)
            wr_lo = sb(s2, "wr_lo", [128, 8, 20], BF16)
            br_sb = sb(s2, "br_sb", [20, 1], F32)
            lgT = sb(s2, "lgT", [20, 512], F32)
            lg = sb(s2, "lg", [128, 4, 20], F32)
            chl = sb(s2, "chl", [128, 4, 48], BF16)
            osum = sb(s2, "osum", [128, 4, 1], F32)
            onm = sb(s2, "onm", [128, 4, 1], F32)
            tri_sb = sb(s2, "tri_sb", [128, 128], F32)
            tid_sb = sb(s2, "tid_sb", [128, 16], I32)
            Wt = sb(s2, "Wt", [128, 16, 4], F32)
            Tt = sb(s2, "Tt", [128, 16, 4], F32)
            Et = sb(s2, "Et", [128, 16, 4], F32)
            cntv = sb(s2, "cntv", [128, 4], F32)
            startv = sb(s2, "startv", [128, 4], F32)
            hiv = sb(s2, "hiv", [128, 4], F32)
            f1 = sb(s2, "f1", [128, 4], F32)
            f2 = sb(s2, "f2", [128, 4], F32)
            posf = sb(s2, "posf", [128, 16], F32)
            posi = sb(s2, "posi", [128, 16], I32)
            flagsf = sb(s2, "flagsf", [128, 4, 4], F32)
            flagsi = sb(s2, "flagsi", [128, 16], I32)
            bitsf = sb(s2, "bitsf", [128, 1], F32)
            st1 = sb(s2, "st1", [128, 16, 2, 6], F32)
            mv1 = sb(s2, "mv1", [128, 16, 2], F32)
            rs1 = sb(s2, "rs1", [128, 16], F32)
            nb1 = sb(s2, "nb1", [128, 16], F32)
            nb_in = sb(s2, "nb_in", [128, 16], F32)
            brow_hl = sb(s2, "brow_hl", [64, 1024], BF16)
            ones64 = sb(s2, "ones64", [64, 128], BF16)
            r_sm = [sb(s2, "rsm%d" % i, [128, 4, 16], F32) for i in range(6)]
            r_s4 = [sb(s2, "rs4_%d" % i, [128, 4, 4], F32) for i in range(4)]
            r_s1 = [sb(s2, "rs1_%d" % i, [128, 4], F32) for i in range(8)]
            r1b = big[:, 8192:12288].rearrange("p (j d) -> p j d", j=4)
            xrb = big[:, 12288:16384].rearrange("p (j d) -> p j d", j=4)

            def r1v(g):
                s_ = g % 8
                return (r1b[:, s_, :] if s_ < 4 else r1x[:, s_ - 4, :]), "r1s%d" % s_

            S.dma("sp", None, bc_ag[:], rows[:, R_LNIG:R_LNIG + 1024].partition_broadcast(128), writes=["bc_ag"])
            S.dma("sp", None, bc_g1[:], rows[:, R_L1G:R_L1G + 1024].partition_broadcast(128), writes=["bc_g1"])
            S.dma("sp", None, bc_b1[:], rows[:, R_L1B:R_L1B + 1024].partition_broadcast(128), writes=["bc_b1"])
            S.dma("sp", None, wr_f[:], w_r_v, writes=["wr_f"])
            S.dma("sp", None, br_sb[:], b_r, writes=["br_sb"])
            ts_(bc_ag[:], bc_ag[:], ALPHA, None, ALU.mult, ALU.bypass, ["bc_ag"], ["bc_ag"])
            cp("dve", wr_hi[:], wr_f[:], ["wr_f"], ["wr_hi"])
            tt_("dve", wr_lo[:], wr_f[:], wr_hi[:], ALU.subtract, ["wr_f", "wr_hi"], ["wr_lo"])
            load_expert(0)
            V(lambda e: e.memset(chl[:], 0.0), [], ["chl_z"])
            V(lambda e: e.scalar_tensor_tensor(nb_in[:], mv_in[:, 0:16, 0], -1.0, rs_in[:, 0:16], ALU.mult, ALU.mult),
              [], ["nb_in"])

            mixb = {}
            V(lambda e: e.memset(brow_hl[:], 0.0), [], ["brow_hl"])
            V(lambda e: e.memset(ones64[:], 1.0), [], ["ones64"])
            for pb_ in (0, 32):
                f_ = xrb[pb_:pb_ + 1, 2, :]
                f2_ = xrb[pb_:pb_ + 1, 3, :]
                S.dma("sp", None, f_, rows[:, R_BOUT:R_BOUT + 1024], writes=["xr2"])
                S.dma("sp", None, f2_, rows[:, R_LNIB:R_LNIB + 1024], writes=["xr3"])
                stt(f_, f2_, ALPHA, f_, ALU.mult, ALU.add, ["xr2", "xr3"], ["xr2"])
                cp("dve", brow_hl[pb_:pb_ + 1, :], f_, ["xr2", "brow_hl"], ["brow_hl"])
                if pb_ == 32:
                    tt_("dve", brow_hl[32:33, :], f_, brow_hl[32:33, :], ALU.subtract, ["xr2", "brow_hl"], ["brow_hl"])

            def b_dma(g):
                S.dma("sp", "d_xr%d" % (g % 4), xrb[:, g % 4, :], xs[HALO + g * 128:HALO + (g + 1) * 128, :],
                      writes=["xr%d" % (g % 4)])

            def b_xn(g):
                xk = "xr%d" % (g % 4)
                xr = xrb[:, g % 4, :]
                act(xr, xr, AF.Identity, [xk, "nb_in"], [xk], bias=nb_in[:, g:g + 1], scale=rs_in[:, g:g + 1])

            def b_mix(g):
                tt, j = divmod(g, 4)
                gc = slice(g * 128, (g + 1) * 128)
                xk = "xr%d" % (g % 4)
                xr = xrb[:, g % 4, :]
                tt_("pool", xr, xr, bc_ag[:], ALU.mult, [xk, "bc_ag"], [xk])
                pbk = []
                for h in range(2):
                    hs = slice(h * 512, (h + 1) * 512)
                    bk, bkk = nb()
                    for c in range(8):
                        mm(bk[:, :], catT[:, c, gc], wob[:, c, hs], c == 0, False, ["cat%d_%d" % (c, tt), "wob"], [bkk])
                    mm(bk[:, :], ones64[:, :], brow_hl[:, hs], False, True, ["ones64", "brow_hl"], [bkk])
                    pbk.append((bk, bkk))
                mixb[g] = pbk

            def b_r1(g):
                xk = "xr%d" % (g % 4)
                xr = xrb[:, g % 4, :]
                r1, r1k = r1v(g)
                pbk = mixb.pop(g)
                for h in range(2):
                    hs = slice(h * 512, (h + 1) * 512)
                    tt_("dve", r1[:, hs], xr[:, hs], pbk[h][0][:], ALU.add, [xk, pbk[h][1]], [r1k + "_%d" % h])
                V(lambda e: e.bn_stats(st1[:, g, 0, :], r1[:, 0:512]), [r1k + "_0"], ["st1a%d" % g])
                V(lambda e: e.bn_stats(st1[:, g, 1, :], r1[:, 512:1024]), [r1k + "_1"], ["st1b%d" % g])
                V(lambda e: e.bn_aggr(mv1[:, g, :], st1[:, g, :, :]), ["st1a%d" % g, "st1b%d" % g], ["mv1_%d" % g])

            def b_sqrt(g):
                act(rs1[:, g:g + 1], mv1[:, g, 1:2], AF.Sqrt, ["mv1_%d" % g], ["rs1a%d" % g], bias=EPS)

            def b_rcp(g):
                V(lambda e: e.reciprocal(rs1[:, g:g + 1], rs1[:, g:g + 1]), ["rs1a%d" % g], ["rs1_%d" % g])
                V(lambda e: e.scalar_tensor_tensor(nb1[:, g:g + 1], mv1[:, g, 0:1], -1.0, rs1[:, g:g + 1],
                                                   ALU.mult, ALU.mult), ["rs1_%d" % g, "mv1_%d" % g], ["nb1_%d" % g])

            def b_n1(g):
                r1, r1k = r1v(g)
                rk = [r1k + "_0", r1k + "_1"]
                act(r1, r1, AF.Identity, rk + ["nb1_%d" % g, "rs1_%d" % g], rk, bias=nb1[:, g:g + 1], scale=rs1[:, g:g + 1])

            def b_g1(g):
                r1, r1k = r1v(g)
                rk = [r1k + "_0", r1k + "_1"]
                tt_("pool", r1, r1, bc_g1[:], ALU.mult, rk + ["bc_g1"], rk)

            def b_b1(g):
                r1, r1k = r1v(g)
                rk = [r1k + "_0", r1k + "_1"]
                tt_("dve", r1, r1, bc_b1[:], ALU.add, rk + ["bc_b1"], rk)
                S.dma("sp", "d_x1s%d" % (g % 8), x1_dram[g * 128:(g + 1) * 128, :], r1, reads=rk, writes=["x1d%d" % g])

            def b_hi(g):
                r1, r1k = r1v(g)
                rk = [r1k + "_0", r1k + "_1"]
                acp(hib[g % NH][:], r1, rk, ["hib%d" % (g % NH)])
                S.dma("sp", "d_xh%d" % (g % NH), xh_dram[g * 128:(g + 1) * 128, 0:1024], hib[g % NH][:],
                      reads=["hib%d" % (g % NH)], writes=["xhd%d" % g])

            def b_lo(g):
                r1, r1k = r1v(g)
                rk = [r1k + "_0", r1k + "_1"]
                hb_, lb_ = hib[g % NH], lob[g % NH]
                hk, lk = "hib%d" % (g % NH), "lob%d" % (g % NH)
                tt_("dve", lb_[:], r1, hb_[:], ALU.subtract, rk + [hk], [lk])
                bk, bkk = nb()
                bkb = bk[:].bitcast(BF16)
                for dc in range(8):
                    tp(bkb[:, dc * 128:(dc + 1) * 128], hb_[:, dc * 128:(dc + 1) * 128], idb[:], [hk, "idb"], [bkk])
                mixb[("h", g)] = (bkb, bkk)

            def b_x1T(g):
                gc = slice(g * 128, (g + 1) * 128)
                lb_ = lob[g % NH]
                lk = "lob%d" % (g % NH)
                bkb, bkk = mixb.pop(("h", g))
                acp(x1T[:, :, gc], bkb.rearrange("p (c t) -> p c t", c=8), [bkk], ["x1T_%d" % g])
                bk2, bkk2 = nb()
                bkb2 = bk2[:].bitcast(BF16)
                for dc in range(8):
                    tp(bkb2[:, dc * 128:(dc + 1) * 128], lb_[:, dc * 128:(dc + 1) * 128], idb[:], [lk, "idb"], [bkk2])
                mixb[("l", g)] = (bkb2, bkk2)

            def b_x1lo(g):
                tt, j = divmod(g, 4)
                bkb2, bkk2 = mixb.pop(("l", g))
                acp(x1loT[tt % 2][:, :, j * 128:(j + 1) * 128], bkb2.rearrange("p (c t) -> p c t", c=8),
                    [bkk2], ["x1lo%d_%d" % (tt % 2, j)])

            gmax, gsum, gp, m1, m2_, d21, w1, w2 = [r_s1[i] for i in range(8)]
            _unused, dg, exg, pen = [r_s4[i] for i in range(4)]
            lem, oh1, lem2, oh2, cA, cB = [r_sm[i] for i in range(6)]
            lgG = lg[:, :, 0:4]
            lgE4 = lg[:, :, 4:20].rearrange("p a (g e) -> p a g e", g=4)
            bc4 = lambda a: a[:].unsqueeze(2).to_broadcast([128, 4, 4])
            bc16 = lambda a: a[:].unsqueeze(2).to_broadcast([128, 4, 16])

            def r_0(tt):
                tcols = slice(tt * 512, (tt + 1) * 512)
                x1k = ["x1T_%d" % (tt * 4 + j) for j in range(4)]
                lok = ["x1lo%d_%d" % (tt % 2, j) for j in range(4)]
                br_, brk = nb()
                n = 0
                for (wt, wk, xsrc, xk) in ((wr_hi, "wr_hi", "hi", x1k), (wr_hi, "wr_hi", "lo", lok), (wr_lo, "wr_lo", "hi", x1k)):
                    for dc in range(8):
                        rhs = x1T[:, dc, tcols] if xsrc == "hi" else x1loT[tt % 2][:, dc, :]
                        mm(br_[0:20, :], wt[:, dc, :], rhs, n == 0, n == 23, [wk] + xk, [brk])
                        n += 1
                act(lgT[:], br_[0:20, :], AF.Identity, [brk, "br_sb"], ["lgT"], bias=br_sb[:, 0:1])
                bt_, btk = nb()
                for j in range(4):
                    tp(bt_[:, j * 20:(j + 1) * 20], lgT[:, j * 128:(j + 1) * 128], idf[0:20, 0:20], ["lgT", "idf"], [btk])
                cp("dve", lg[:].rearrange("p a b -> p (a b)"), bt_[:, 0:80], [btk], ["lg"])

            def r_1(tt):
                ohg = ohg_all[:, tt * 4:(tt + 1) * 4, :]
                V(lambda e: e.tensor_reduce(gmax[:], lgG, AX.X, ALU.max), ["lg"], ["gmax"])
                V(lambda e: e.tensor_tensor(ohg, lgG, bc4(gmax), ALU.is_equal), ["lg", "gmax"], ["ohg"])
                V(lambda e: e.tensor_copy(osum[:], ohg[:, :, 0:1]), ["ohg"], ["osum"])
                for g_ in range(1, 4):
                    V(lambda e: e.tensor_scalar(onm[:], osum[:], -1.0, 1.0, ALU.mult, ALU.add), ["osum"], ["onm"])
                    V(lambda e, g_=g_: e.tensor_tensor(ohg[:, :, g_:g_ + 1], ohg[:, :, g_:g_ + 1], onm[:], ALU.mult), ["ohg", "onm"], ["ohg"])
                    if g_ < 3:
                        V(lambda e, g_=g_: e.tensor_tensor(osum[:], osum[:], ohg[:, :, g_:g_ + 1], ALU.add), ["osum", "ohg"], ["osum"])
                V(lambda e: e.tensor_tensor(dg[:], lgG, bc4(gmax), ALU.subtract), ["lg", "gmax"], ["dg"])
                act(exg[:], dg[:], AF.Exp, ["dg"], ["exg"])
                V(lambda e: e.tensor_scalar(pen[:], ohg, BIG, -BIG, ALU.mult, ALU.add), ["ohg"], ["pen"])
                V(lambda e: e.tensor_tensor(lem[:].rearrange("p a (g e) -> p a g e", g=4), lgE4,
                                            pen[:].unsqueeze(3).to_broadcast([128, 4, 4, 4]), ALU.add), ["lg", "pen"], ["lem"])
                V(lambda e: e.tensor_reduce(m1[:], lem[:], AX.X, ALU.max), ["lem"], ["m1"])
                V(lambda e: e.tensor_tensor(oh1[:], lem[:], bc16(m1), ALU.is_equal), ["lem", "m1"], ["oh1"])
                V(lambda e: e.scalar_tensor_tensor(lem2[:], oh1[:], -BIG, lem[:], ALU.mult, ALU.add), ["oh1", "lem"], ["lem2"])
                V(lambda e: e.tensor_reduce(m2_[:], lem2[:], AX.X, ALU.max), ["lem2"], ["m2"])
                V(lambda e: e.tensor_tensor(oh2[:], lem2[:], bc16(m2_), ALU.is_equal), ["lem2", "m2"], ["oh2"])
                V(lambda e: e.tensor_tensor(d21[:], m2_[:], m1[:], ALU.subtract), ["m1", "m2"], ["d21"])
                act(d21[:], d21[:], AF.Exp, ["d21"], ["d21"])

            def r_2(tt):
                tcols = slice(tt * 512, (tt + 1) * 512)
                V(lambda e: e.tensor_reduce(gsum[:], exg[:], AX.X, ALU.add), ["exg"], ["gsum"])
                V(lambda e: e.reciprocal(gp[:], gsum[:]), ["gsum"], ["gp"])
                V(lambda e: e.tensor_scalar(d21[:], d21[:], 1.0, None, ALU.add), ["d21"], ["d21"])
                V(lambda e: e.reciprocal(w1[:], d21[:]), ["d21"], ["w1"])
                V(lambda e: e.tensor_scalar(w2[:], w1[:], -1.0, 1.0, ALU.mult, ALU.add), ["w1"], ["w2"])
                V(lambda e: e.tensor_tensor(w1[:], w1[:], gp[:], ALU.mult), ["w1", "gp"], ["w1"])
                V(lambda e: e.tensor_tensor(w2[:], w2[:], gp[:], ALU.mult), ["w2", "gp"], ["w2"])
                V(lambda e: e.tensor_tensor(cA[:], oh1[:], bc16(w1), ALU.mult), ["oh1", "w1"], ["cA"])
                V(lambda e: e.tensor_tensor(cB[:], oh2[:], bc16(w2), ALU.mult), ["oh2", "w2"], ["cB"])
                V(lambda e: e.tensor_tensor(cA[:], cA[:], cB[:], ALU.add), ["cA", "cB"], ["cA"])
                V(lambda e: e.tensor_copy(chl[:, :, 0:16], cA[:]), ["cA"], ["chl_hi"])
                V(lambda e: e.tensor_tensor(chl[:, :, 32:48], cA[:], chl[:, :, 0:16], ALU.subtract), ["cA", "chl_hi"], ["chl_lo"])
                S.dma("sp", "d_chl", xh_dram[tt * 512:(tt + 1) * 512, 1024:XHW].rearrange("(j p) c -> p j c", p=128),
                      chl[:], reads=["chl_hi", "chl_lo", "chl_z"], writes=["chld%d" % tt])

            def grp(fn, off):
                return (off, lambda g: fn(g // 4) if g % 4 == 3 else None)

            pipeline([(0, b_dma), (1, b_xn), (2, b_mix), (3, b_r1), (4, b_sqrt), (5, b_rcp), (6, b_n1), (7, b_g1),
                      (8, b_b1), (9, b_hi), (10, b_lo), (11, b_x1T), (12, b_x1lo),
                      grp(r_0, 13), grp(r_1, 14), grp(r_2, 15)], 16)

            oflat = ohg_all[:].rearrange("p s g -> p (s g)")
            S.dma("sp", None, tri_sb[:], tri, writes=["tri_sb"])
            S.dma("sp", None, tid_sb[:], tid, writes=["tid_sb"])
            bw, bwk = nb()
            mm(bw[:, 0:64], tri_sb[:], oflat, True, True, ["tri_sb", "ohg"], [bwk])
            bt, btk = nb()
            mm(bt[:, 0:64], ones_f[:], oflat, True, True, ["ones_f", "ohg"], [btk])
            cp("dve", Wt[:].rearrange("p s g -> p (s g)"), bw[:, 0:64], [bwk], ["Wt"])
            cp("dve", Tt[:].rearrange("p s g -> p (s g)"), bt[:, 0:64], [btk], ["Tt"])
            V(lambda e: e.memset(Et[:, 0, :], 0.0), [], ["Et"])
            for s_ in range(1, 16):
                V(lambda e, s_=s_: e.tensor_tensor(Et[:, s_, :], Et[:, s_ - 1, :], Tt[:, s_ - 1, :], ALU.add), ["Et", "Tt"], ["Et"])
            V(lambda e: e.tensor_tensor(cntv[:], Et[:, 15, :], Tt[:, 15, :], ALU.add), ["Et", "Tt"], ["cntv"])
            V(lambda e: e.memset(startv[:, 0:1], 0.0), [], ["startv"])
            for g_ in range(1, 4):
                V(lambda e, g_=g_: e.tensor_tensor(startv[:, g_:g_ + 1], startv[:, g_ - 1:g_], cntv[:, g_ - 1:g_], ALU.add),
                  ["startv", "cntv"], ["startv"])
            V(lambda e: e.tensor_tensor(Wt[:], Wt[:], Et[:], ALU.add), ["Wt", "Et"], ["Wt"])
            V(lambda e: e.tensor_tensor(Wt[:], Wt[:], startv[:].unsqueeze(1).to_broadcast([128, 16, 4]), ALU.add),
              ["Wt", "startv"], ["Wt"])
            V(lambda e: e.tensor_tensor(Wt[:], Wt[:], ohg_all[:], ALU.mult), ["Wt", "ohg"], ["Wt"])
            V(lambda e: e.tensor_reduce(posf[:], Wt[:], AX.X, ALU.add), ["Wt"], ["posf"])
            V(lambda e: e.tensor_copy(posi[:], posf[:]), ["posf"], ["posi"])
            for s_ in range(16):
                S.dma_fn("pool", "d_inv", lambda eng, s_=s_: eng.indirect_dma_start(
                    out=inv_dram[:, :], out_offset=bass.IndirectOffsetOnAxis(ap=posi[:, s_:s_ + 1], axis=0),
                    in_=tid_sb[:, s_:s_ + 1], in_offset=None), reads=["posi", "tid_sb"], writes=["inv_dram%d" % s_])
            for q in range(16):
                S.dma("sp" if q % 2 == 0 else "act", "d_invl%d" % (q % 2), inv_sb[:, q:q + 1], inv_dram[q * 128:(q + 1) * 128, :],
                      reads=["inv_dram%d" % s_ for s_ in range(16)], writes=["inv_sb%d" % q])
            V(lambda e: e.tensor_tensor(hiv[:], startv[:], cntv[:], ALU.add), ["startv", "cntv"], ["hiv"])
            for tt in range(4):
                V(lambda e, tt=tt: e.tensor_scalar(f1[:], startv[:], float((tt + 1) * 512), None, ALU.is_lt), ["startv"], ["f1"])
                V(lambda e, tt=tt: e.tensor_scalar(f2[:], hiv[:], float(tt * 512), None, ALU.is_gt), ["hiv"], ["f2"])
                V(lambda e, tt=tt: e.tensor_tensor(flagsf[:, tt, :], f1[:], f2[:], ALU.mult), ["f1", "f2"], ["flagsf"])
            V(lambda e: e.memset(bitsf[:], 0.0), [], ["bitsf"])
            for k in range(16):
                V(lambda e, k=k: e.scalar_tensor_tensor(bitsf[:], flagsf[:, k // 4, k % 4:k % 4 + 1], float(2 ** k), bitsf[:],
                                                        ALU.mult, ALU.add), ["flagsf", "bitsf"], ["bitsf"])
            if os.environ.get("K_ALLFLAGS"):
                V(lambda e: e.memset(bitsf[:], 65535.0), ["bitsf"], ["bitsf"])
            V(lambda e: e.tensor_copy(flagsi[:, 0:1], bitsf[:]), ["bitsf"], ["flagsi"])
            S.dma("sp", None, flags_dram[0:1, 0:1], flagsi[0:1, 0:1], reads=["flagsi"], writes=["flags"])
            if dbg is not None:
                S.dma("sp", None, dbg[:, 0:16], flagsf[:].rearrange("p a b -> p (a b)"), reads=["flagsf"], writes=["dbg0"])
                S.dma("sp", None, dbg[:, 16:18], flagsi[:, 0:2].bitcast(F32), reads=["flagsi"], writes=["dbg1"])
                S.dma("sp", None, dbg[:, 20:24], startv[:], reads=["startv"], writes=["dbg2"])
                S.dma("sp", None, dbg[:, 24:28], cntv[:], reads=["cntv"], writes=["dbg3"])
                S.dma("sp", None, dbg[:, 32:48], posf[:], reads=["posf"], writes=["dbg4"])
            S.barrier()
            S.emit()
        if stop_after == "1b":
            return nc

        yacc = big[:, :].rearrange("p (g d) -> p g d", g=16)
        with contextlib.ExitStack() as s3:
            for i in range(1, NSLOT):
                wg[i] = sb(s3, "wg%d" % i, [128, 8, DE], BF16)
                wu[i] = sb(s3, "wu%d" % i, [128, 8, DE], BF16)
            for i in range(1, NWD):
                wd[i] = sb(s3, "wd%d" % i, [128, 2, D], BF16)
            NHID = 6
            hid_r = [sb(s3, "hidr%d" % i, [128, 2, 512], BF16) for i in range(2)]
            sel = sb(s3, "sel", [48, NE, 128], BF16)
            scr = sb(s3, "scr", [1, 512], F32)
            scrp = sb(s3, "scrp", [128, 160], F32)
            NXG = 6
            xg = [sb(s3, "xg%d" % i, [128, XHW], BF16) for i in range(NXG)]
            hid = [xg[i][:, 0:1024].rearrange("p (f t) -> p f t", f=2) for i in range(NXG)]
            hid = hid + hid_r
            hidk = lambda k: ["hid%d" % k, "xg%d" % k] if k < NXG else ["hid%d" % k]
            cbs = [sb(s3, "cbs%d" % i, [128, 512], F32) for i in range(2)]
            NSS = 3
            sbuf_s = [sb(s3, "mss%d" % i, [128, 512], F32) for i in range(NSS)]
            sbuf_t = [sb(s3, "mst%d" % i, [128, 512], F32) for i in range(NSS)]

            V(lambda e: e.memset(sel[:], 0.0), [], ["sel"])
            V(lambda e: e.tensor_copy(sel[0:16, :, :], idf[0:16, 0:16].unsqueeze(2).to_broadcast([16, NE, 128])),
              ["idf", "sel"], ["sel"])
            V(lambda e: e.tensor_copy(sel[32:48, :, :], idf[32:48, 32:48].unsqueeze(2).to_broadcast([16, NE, 128])),
              ["idf", "sel"], ["sel"])
            nbanks[0] = 7
            S.op("dve", lambda e: e.memset(scr[:], 0.0), [], ["scr"])
            S.dummy["pe"] = lambda eng, i: eng.matmul(banks[7][0:1, i:i + 1], ones_rb[0:1, 0:1], ones_rb[0:1, 0:1], start=True, stop=True)
            S.dummy["act"] = lambda eng, i: eng.copy(scr[0:1, 384 + i:385 + i], scr[0:1, 511:512])
            S.dummy["dve"] = lambda eng, i: eng.memset(scr[0:1, i:i + 1], 0.0)
            S.dummy["pool"] = lambda eng, i: eng.memset(scrp[:, i:i + 1], 0.0)

            def load_flags(e):
                def f(eng):
                    r = eng.alloc_register("fl_%s" % e)
                    eng.reg_load(r, flags_dram[0:1, 0:1])
                    S.regs[e] = eng.snap(r)
                    return None
                return f
            for e in ("pe", "act", "dve", "pool"):
                S.raw(e, load_flags(e))

            S.op("dve", lambda e: e.memset(big[:, 0:8192], 0.0), [], ["yz0"])


            def gather_q(q):
                xg_ = xg[q % NXG]
                S.dma_fn("pool", "d_xg%d" % (q % NXG), lambda eng: eng.indirect_dma_start(
                    out=xg_[:, :], out_offset=None, in_=xh_dram[:, :],
                    in_offset=bass.IndirectOffsetOnAxis(ap=inv_sb[:, q:q + 1], axis=0)), reads=[],
                    writes=["xg%d" % (q % NXG), "hid%d" % (q % NXG)])

            def trans_q(q):
                qc = slice(q * 128, (q + 1) * 128)
                xg_ = xg[q % NXG]
                xgk = "xg%d" % (q % NXG)
                bk, bkk = nb()
                bkb = bk[:].bitcast(BF16)
                for dc in range(8):
                    tp(bkb[:, dc * 128:(dc + 1) * 128], xg_[:, dc * 128:(dc + 1) * 128], idb[:], [xgk, "idb"], [bkk])
                acp(x1T[:, :, qc], bkb.rearrange("p (c t) -> p c t", c=8), [bkk], ["x1T_%d" % q])
                bk2, bkk2 = nb()
                bkb2 = bk2[:].bitcast(BF16)
                tp(bkb2[0:48, 0:128], xg_[:, 1024:XHW], idb[:], [xgk, "idb"], [bkk2])
                cp("dve", combT[:, qc], bkb2[0:48, 0:128], [bkk2], ["combT_%d" % q])

            for q in range(NXG):
                gather_q(q)
            load_expert(1)
            S.op("pool", lambda e: e.memset(big[:, 8192:16384], 0.0), [], ["yz1"])

            steps = [(e_, tt) for e_ in range(NE) for tt in range(4)]
            pend_b = {}
            uctr = 0

            def down(e_, tt, hslot):
                slot = e_ % NWD
                for j in range(4):
                    g = tt * 4 + j
                    for h in range(2):
                        bk, bkk = nb()
                        for fc in range(2):
                            mm(bk[:, :], hid[hslot][:, fc, j * 128:(j + 1) * 128], wd[slot][:, fc, h * 512:(h + 1) * 512],
                               fc == 0, fc == 1, hidk(hslot) + ["wd%d" % slot], [bkk])
                        ysl = yacc[:, g, h * 512:(h + 1) * 512]
                        yk = "y_%d_%d" % (g, h)
                        tt_("dve", ysl, ysl, bk[:], ALU.add, [bkk, yk, "yz0", "yz1"], [yk])

            for si, (e_, tt) in enumerate(steps):
                slot = e_ % NSLOT
                tcols = slice(tt * 512, (tt + 1) * 512)
                fl = tt * 4 + e_ // 4
                if e_ == 0:
                    for q in range(tt * 4, tt * 4 + 4):
                        trans_q(q)
                        if q + NXG < 16:
                            gather_q(q + NXG)
                    if tt == 1:
                        load_expert(2)
                if tt == 1 and e_ >= 1 and e_ + 2 < NE:
                    load_expert(e_ + 2)
                if tt == 2 and e_ == NE - 2:
                    S.dma("pool", "d_wob", wob[:], w_pg_v, writes=["wob"])
                S.cond_begin(fl)
                xk = ["x1T_%d" % (tt * 4 + j) for j in range(4)]
                ck = ["combT_%d" % (tt * 4 + j) for j in range(4)]
                bcb, bcbk = nb()
                mm(bcb[:, :], sel[:, e_, :], combT[:, tcols], True, True, ["sel"] + ck, [bcbk])
                pg = []
                pu = []
                for fc in range(2):
                    bk, bkk = nb()
                    for dc in range(8):
                        mm(bk[:, :], wg[slot][:, dc, fc * 128:(fc + 1) * 128], x1T[:, dc, tcols], dc == 0, dc == 7,
                           ["wg%d" % slot] + xk, [bkk])
                    pg.append((bk, bkk))
                for fc in range(2):
                    bk, bkk = nb()
                    for dc in range(8):
                        mm(bk[:, :], wu[slot][:, dc, fc * 128:(fc + 1) * 128], x1T[:, dc, tcols], dc == 0, dc == 7,
                           ["wu%d" % slot] + xk, [bkk])
                    pu.append((bk, bkk))
                cb = cbs[si % 2]
                cbk = "cbs%d" % (si % 2)
                acp(cb[:], bcb[:], [bcbk], [cbk])
                hslot = (NXG + tt % 2) if e_ == 0 else si % NHID
                for fc in range(2):
                    u = uctr % NSS
                    uctr += 1
                    act(sbuf_s[u][:], pg[fc][0][:], AF.Silu, [pg[fc][1]], ["ss%d" % u])
                    tt_("dve", sbuf_t[u][:], pu[fc][0][:], cb[:], ALU.mult, [pu[fc][1], cbk], ["st%d" % u])
                    tt_("pool", hid[hslot][:, fc, :], sbuf_s[u][:], sbuf_t[u][:], ALU.mult, ["ss%d" % u, "st%d" % u],
                        hidk(hslot))
                pend_b[(e_, tt)] = (e_, tt, hslot, fl)
                if e_ == 0:
                    prev = (0, tt - 1) if tt >= 1 else None
                elif e_ == 1:
                    prev = (0, 3) if tt == 0 else None
                else:
                    prev = (e_ - 1, tt)
                if e_ == 1 and tt == 3:
                    prev2 = None
                if prev is not None and pend_b[prev][3] == fl:
                    pb_ = pend_b.pop(prev)
                    down(*pb_[0:3])
                    S.cond_end()
                else:
                    S.cond_end()
                    if prev is not None:
                        pb_ = pend_b.pop(prev)
                        S.cond_begin(pb_[3])
                        down(*pb_[0:3])
                        S.cond_end()
            for e_r in (1,):
                pass
            for key in sorted(pend_b.keys()):
                pb_ = pend_b.pop(key)
                S.cond_begin(pb_[3])
                down(*pb_[0:3])
                S.cond_end()
            nbanks[0] = 8
            S.barrier()
            S.emit()
        sM.close()
        if stop_after == "2":
            return nc

        with contextlib.ExitStack() as s4:
            wpg = wob
            wpp = sb(s4, "wpp", [128, 2, D], BF16)
            bc_g2 = sb(s4, "bc_g2", [128, 1024], F32)
            bc_b2 = sb(s4, "bc_b2", [128, 1024], F32)
            N3 = 3
            x1r = [sb(s4, "x1r%d" % i, [128, 1024], F32) for i in range(N3)]
            pt = [sb(s4, "pt%d" % i, [128, DPLE], F32) for i in range(N3)]
            rb = [sb(s4, "rb%d" % i, [128, 1024], BF16) for i in range(N3)]
            pb16 = [sb(s4, "pb16_%d" % i, [128, DPLE], BF16) for i in range(N3)]
            rT = [sb(s4, "rT%d" % i, [128, 8, 128], BF16) for i in range(N3)]
            pT = [sb(s4, "pT%d" % i, [128, 2, 128], BF16) for i in range(N3)]
            gt = [sb(s4, "gt%d" % i, [128, 512], F32) for i in range(4)]
            gpt = [sb(s4, "gpt%d" % i, [128, 512], F32) for i in range(4)]
            ob = [sb(s4, "ob%d" % i, [128, 1024], F32) for i in range(N3)]
            st2 = sb(s4, "st2", [128, 16, 2, 6], F32)
            mv2 = sb(s4, "mv2", [128, 16, 2], F32)
            rs2 = sb(s4, "rs2", [128, 16], F32)
            nb2 = sb(s4, "nb2", [128, 16], F32)

            S.dma("pool", "d_wpp", wpp[:], w_pp_v, writes=["wpp"])
            S.dma("sp", None, bc_g2[:], rows[:, R_L2G:R_L2G + 1024].partition_broadcast(128), writes=["bc_g2"])
            S.dma("sp", None, bc_b2[:], rows[:, R_L2B:R_L2B + 1024].partition_broadcast(128), writes=["bc_b2"])
            hctr = [0]

            pbanks = {}

            def c_dma(g):
                k = g % N3
                S.dma_fn("pool", "d_x1r%d" % k, lambda eng: eng.indirect_dma_start(
                    out=x1r[k][:, :], out_offset=None, in_=x1_dram[:, :],
                    in_offset=bass.IndirectOffsetOnAxis(ap=inv_sb[:, g:g + 1], axis=0)), reads=[], writes=["x1r%d" % k])
                S.dma_fn("pool", "d_pt%d" % k, lambda eng: eng.indirect_dma_start(
                    out=pt[k][:, :], out_offset=None, in_=ps[:, :],
                    in_offset=bass.IndirectOffsetOnAxis(ap=inv_sb[:, g:g + 1], axis=0)), reads=[], writes=["pt%d" % k])

            def c_r(g):
                k = g % N3
                yk = ["y_%d_0" % g, "y_%d_1" % g]
                stt(yacc[:, g, :], x1r[k][:], ALPHA, yacc[:, g, :], ALU.mult, ALU.add, ["x1r%d" % k] + yk, yk)
                cp("dve", pb16[k][:], pt[k][:], ["pt%d" % k], ["pb16_%d" % k])

            def c_rb(g):
                k = g % N3
                yk = ["y_%d_0" % g, "y_%d_1" % g]
                acp(rb[k][:], yacc[:, g, :], yk, ["rb%d" % k])
                bk2, bkk2 = nb()
                bkb2 = bk2[:].bitcast(BF16)
                for kc in range(2):
                    tp(bkb2[:, kc * 128:(kc + 1) * 128], pb16[k][:, kc * 128:(kc + 1) * 128], idb[:], ["pb16_%d" % k, "idb"], [bkk2])
                pbanks[("p", g)] = (bkb2, bkk2)

            def c_tr(g):
                k = g % N3
                bkb2, bkk2 = pbanks.pop(("p", g))
                cp("dve", pT[k][:], bkb2[:, 0:256].rearrange("p (c t) -> p c t", c=2), [bkk2], ["pT%d" % k])
                bk, bkk = nb()
                bkb = bk[:].bitcast(BF16)
                for dc in range(8):
                    tp(bkb[:, dc * 128:(dc + 1) * 128], rb[k][:, dc * 128:(dc + 1) * 128], idb[:], ["rb%d" % k, "idb"], [bkk])
                pbanks[("r", g)] = (bkb, bkk)

            def c_rT(g):
                k = g % N3
                bkb, bkk = pbanks.pop(("r", g))
                acp(rT[k][:], bkb.rearrange("p (c t) -> p c t", c=8), [bkk], ["rT%d" % k])

            def c_mm(g):
                k = g % N3
                for h in range(2):
                    hs = slice(h * 512, (h + 1) * 512)
                    bg_, bgk = nb()
                    for dc in range(8):
                        mm(bg_[:, :], rT[k][:, dc, :], wpg[:, dc, hs], dc == 0, False, ["rT%d" % k, "wob"], [bgk])
                    mm(bg_[:, :], ones_rb[0:1, :], brow_b[0:1, 1024 + h * 512:1024 + (h + 1) * 512], False, True,
                       ["ones_rb", "brow"], [bgk])
                    bp_, bpk = nb()
                    for kc in range(2):
                        mm(bp_[:, :], pT[k][:, kc, :], wpp[:, kc, hs], kc == 0, kc == 1, ["pT%d" % k, "wpp"], [bpk])
                    u = hctr[0] % 4
                    hctr[0] += 1
                    act(gt[u][:], bg_[:], AF.Sigmoid, [bgk], ["gt%d" % u])
                    pbanks[("g", g, h)] = (u, bp_, bpk)

            def c_gp(g):
                for h in range(2):
                    u, bp_, bpk = pbanks.pop(("g", g, h))
                    tt_("dve", gpt[u][:], gt[u][:], bp_[:], ALU.mult, ["gt%d" % u, bpk], ["gpt%d" % u])
                    pbanks[("a", g, h)] = u

            def c_add(g):
                for h in range(2):
                    hs = slice(h * 512, (h + 1) * 512)
                    u = pbanks.pop(("a", g, h))
                    tt_("pool", yacc[:, g, hs], yacc[:, g, hs], gpt[u][:], ALU.add, ["y_%d_%d" % (g, h), "gpt%d" % u],
                        ["y_%d_%d" % (g, h)])

            def c_st(g):
                V(lambda e: e.bn_stats(st2[:, g, 0, :], yacc[:, g, 0:512]), ["y_%d_0" % g], ["st2a%d" % g])
                V(lambda e: e.bn_stats(st2[:, g, 1, :], yacc[:, g, 512:1024]), ["y_%d_1" % g], ["st2b%d" % g])
                V(lambda e: e.bn_aggr(mv2[:, g, :], st2[:, g, :, :]), ["st2a%d" % g, "st2b%d" % g], ["mv2_%d" % g])

            def c_sqrt(G):
                g0 = G * 4
                act(rs2[:, g0:g0 + 4], mv2[:, g0:g0 + 4, 1], AF.Sqrt, ["mv2_%d" % (g0 + j) for j in range(4)],
                    ["rs2a%d" % g0], bias=EPS)

            def c_rcp(G):
                g0 = G * 4
                V(lambda e: e.reciprocal(rs2[:, g0:g0 + 4], rs2[:, g0:g0 + 4]), ["rs2a%d" % g0], ["rs2_%d" % g0])
                V(lambda e: e.scalar_tensor_tensor(nb2[:, g0:g0 + 4], mv2[:, g0:g0 + 4, 0], -1.0, rs2[:, g0:g0 + 4],
                                                   ALU.mult, ALU.mult),
                  ["rs2_%d" % g0] + ["mv2_%d" % (g0 + j) for j in range(4)], ["nb2_%d" % g0])

            def c_on(g):
                k = g % N3
                g0 = g - g % 4
                yk = ["y_%d_0" % g, "y_%d_1" % g]
                act(ob[k][:], yacc[:, g, :], AF.Identity, yk + ["nb2_%d" % g0, "rs2_%d" % g0], ["ob%d" % k],
                    bias=nb2[:, g:g + 1], scale=rs2[:, g:g + 1])

            def c_og(g):
                k = g % N3
                tt_("pool", ob[k][:], ob[k][:], bc_g2[:], ALU.mult, ["ob%d" % k, "bc_g2"], ["ob%d" % k])

            def c_ob(g):
                k = g % N3
                tt_("dve", ob[k][:], ob[k][:], bc_b2[:], ALU.add, ["ob%d" % k, "bc_b2"], ["ob%d" % k])
                S.dma_fn("pool", "d_out%d" % k, lambda eng: eng.indirect_dma_start(
                    out=out[:, :], out_offset=bass.IndirectOffsetOnAxis(ap=inv_sb[:, g:g + 1], axis=0),
                    in_=ob[k][:, :], in_offset=None), reads=["ob%d" % k], writes=["outd%d" % g])

            def grp3(fn, off):
                return (off, lambda g: fn(g // 4) if g % 4 == 3 else None)

            pipeline([(0, c_dma), (1, c_r), (2, c_rb), (3, c_tr), (4, c_rT), (6, c_gp), (7, c_add), (8, c_st),
                      grp3(c_sqrt, 9), grp3(c_rcp, 10), (14, c_on), (15, c_og), (16, c_ob), (5, c_mm)], 16)
            S.final_wait("sp", ["d_out%d" % k for k in range(N3)])
            S.emit()
    return nc


_NC_CACHE = {}


def _prep_inputs(inp):
    f = lambda a: np.ascontiguousarray(np.asarray(a, dtype=np.float32))
    x = f(inp["x"])
    p = f(inp["p"])[0]
    w_in = f(inp["w_in"])[0]
    b_in = f(inp["b_in"])[0]
    perm = []
    for i in range(4):
        perm += list(range(i * 128, (i + 1) * 128)) + list(range(512 + i * 128, 512 + (i + 1) * 128))
    for i in range(4):
        perm += list(range(1536 + i * 128, 1536 + (i + 1) * 128))
        perm += list(range(2048 + i * 128, 2048 + (i + 1) * 128))
        perm += list(range(1024 + i * 128, 1024 + (i + 1) * 128))
    perm = np.array(perm)
    w_in_p = np.ascontiguousarray(w_in[:, perm])
    b_in_p = b_in[perm].reshape(20, 128).T
    col = lambda v, n: f(v).reshape(n, 128).T
    dww = f(inp["conf_dw_w"])[0]
    dww_p = dww.T.reshape(4, 128, 31).transpose(1, 0, 2).reshape(128, 124)
    scw = f(inp["sc_w"])[0]
    scw_p = scw.T.reshape(4, 128, 3).transpose(1, 0, 2).reshape(128, 12)
    rows = np.concatenate([f(inp["ln_in_g"]), f(inp["ln_in_b"]), f(inp["ln1_g"])[0], f(inp["ln1_b"])[0],
                           f(inp["ln2_g"])[0], f(inp["ln2_b"])[0], f(inp["b_out"])[0], f(inp["b_pg"])[0]])[None, :]
    w_r = np.ascontiguousarray(np.concatenate([f(inp["w_rg"])[0], f(inp["w_re"])[0]], axis=1))
    b_r = np.concatenate([f(inp["b_rg"])[0], f(inp["b_re"])[0]])[:, None]
    shared = {
        "rows": np.ascontiguousarray(rows), "w_in": w_in_p, "w_out": f(inp["w_out"])[0], "w_r": w_r,
        "b_r": np.ascontiguousarray(b_r), "w_gate": f(inp["w_gate"])[0], "w_up": f(inp["w_up"])[0],
        "w_down": f(inp["w_down"])[0], "w_pg": f(inp["w_pg"])[0], "w_pp": f(inp["w_pp"])[0],
        "ident": np.eye(128, dtype=np.float32),
        "tri": np.triu(np.ones((128, 128), np.float32), k=1),
        "tid": (np.arange(16, dtype=np.int32)[None, :] * 128 + np.arange(128, dtype=np.int32)[:, None]).astype(np.int32),
    }
    maps = []
    for c in range(NCORES):
        b, q = divmod(c, 4)
        lo = q * NT
        if q == 0:
            halo = np.zeros((HALO, D), np.float32)
            mask = 0.0
        else:
            halo = x[b, lo - HALO:lo]
            mask = 1.0
        xs = np.ascontiguousarray(np.concatenate([halo, x[b, lo:lo + NT]], axis=0))
        pp = np.concatenate([b_in_p, col(inp["ln_in_g"], 8), col(inp["ln_in_b"], 8), dww_p,
                             col(f(inp["conf_dw_b"])[0], 4), col(f(inp["conf_ln_g"])[0], 4), col(f(inp["conf_ln_b"])[0], 4),
                             scw_p, col(f(inp["sc_b"])[0], 4), np.full((128, 1), mask, np.float32)], axis=1)
        m = dict(shared)
        m["xs"] = xs
        m["ps"] = np.ascontiguousarray(p[b, lo:lo + NT])
        m["pp"] = np.ascontiguousarray(pp.astype(np.float32))
        maps.append(m)
    return maps


def kernel(**inputs):
    if "nc" not in _NC_CACHE:
        _NC_CACHE["nc"] = build_nc()
    nc = _NC_CACHE["nc"]
    maps = _prep_inputs(inputs)
    res = run_bass_kernel_spmd(nc, maps, core_ids=list(range(NCORES)))
    outs = [np.asarray(r["out"], dtype=np.float32) for r in res.results]
    full = np.stack(outs, axis=0).reshape(2, 4 * NT, D)
    return full
```

```python
import contextlib
import os
import numpy as np
import concourse.bass as bass
import concourse.mybir as mybir
from concourse.bass_utils import run_bass_kernel_spmd

F32 = mybir.dt.float32
I32 = mybir.dt.int32
BF16 = mybir.dt.bfloat16
AF = mybir.ActivationFunctionType
ALU = mybir.AluOpType
AX = mybir.AxisListType

NCORES = 8
NT = 2048
HALO = 32
D = 1024
DIN = 2560
NE = 16
DE = 256
DPLE = 256
XHW = 1072
EPS = 1e-5
ALPHA = 2.0 ** 0.25
BIG = 1.0e4
ENGS = ["pe", "act", "dve", "pool", "sp"]

PP_BIN = 0
PP_LNG = 20
PP_LNB = 28
PP_DWW = 36
PP_DWB = 160
PP_CLG = 164
PP_CLB = 168
PP_SCW = 172
PP_SCB = 184
PP_MASK = 188
PP_N = 189
R_LNIG, R_LNIB, R_L1G, R_L1B, R_L2G, R_L2B, R_BOUT, R_BPG = [i * 1024 for i in range(8)]


class Sched:
    def __init__(self, nc, stack):
        self.nc = nc
        self.stack = stack
        self.prog = {e: [] for e in ENGS}
        self.sem = {}
        self.cnt = {}
        self.seen = {e: {} for e in ENGS}
        self.res = {}
        self.in_cond = False
        self.regs = {}
        self.dummy = {}
        self.dummy_ctr = {}
        for e in ENGS:
            self.new_sem(e)

    def new_sem(self, key):
        if key not in self.sem:
            self.sem[key] = self.stack.enter_context(self.nc.semaphore("s_" + key))
            self.cnt[key] = 0
        return key

    def _deps(self, e, reads, writes):
        deps = {}

        def add(k, v):
            if deps.get(k, 0) < v:
                deps[k] = v

        for r in reads:
            st = self.res.get(r)
            if st:
                for k, v in st["w"].items():
                    add(k, v)
        for w in writes:
            st = self.res.get(w)
            if st:
                for k, v in st["w"].items():
                    add(k, v)
                for k, v in st["r"].items():
                    add(k, v)
        waits = []
        for k, v in deps.items():
            if k == e and e in ("pe", "sp"):
                continue
            if self.seen[e].get(k, 0) >= v:
                continue
            self.seen[e][k] = v
            waits.append((k, v))
        return waits

    def _commit(self, me, reads, writes):
        for r in reads:
            st = self.res.setdefault(r, {"w": {}, "r": {}})
            if st["r"].get(me[0], 0) < me[1]:
                st["r"][me[0]] = me[1]
        for w in writes:
            if self.in_cond:
                st = self.res.setdefault(w, {"w": {}, "r": {}})
                if st["w"].get(me[0], 0) < me[1]:
                    st["w"][me[0]] = me[1]
            else:
                self.res[w] = {"w": {me[0]: me[1]}, "r": {}}

    def op(self, e, fn, reads=(), writes=()):
        waits = self._deps(e, reads, writes)
        self.cnt[e] += 1
        me = (e, self.cnt[e])
        self.prog[e].append((waits, fn, (e, 1)))
        self._commit(me, reads, writes)
        return me

    def dma(self, q, semkey, out, in_, reads=(), writes=()):
        if semkey is None:
            self.uniq = getattr(self, "uniq", 0) + 1
            semkey = "du%d" % self.uniq
        self.new_sem(semkey)
        waits = self._deps(q, reads, writes)
        self.cnt[semkey] += 16
        me = (semkey, self.cnt[semkey])
        self.prog[q].append((waits, lambda eng: eng.dma_start(out=out, in_=in_), (semkey, 16)))
        self._commit(me, reads, writes)
        return me

    def dma_fn(self, q, semkey, fn, reads=(), writes=()):
        self.new_sem(semkey)
        waits = self._deps(q, reads, writes)
        self.cnt[semkey] += 16
        me = (semkey, self.cnt[semkey])
        self.prog[q].append((waits, fn, (semkey, 16)))
        self._commit(me, reads, writes)
        return me

    def raw(self, e, fn, reads=()):
        waits = self._deps(e, reads, ())
        self.prog[e].append((waits, fn, None))

    def cond_begin(self, flag):
        assert not self.in_cond
        self.in_cond = True
        self.seen_saved = {e: dict(d) for e, d in self.seen.items()}
        self.cond_info = {"snap": dict(self.cnt), "ext": {}}
        for e in ENGS:
            self.prog[e].append(("cond_begin", flag, self.cond_info))

    def cond_end(self):
        self.in_cond = False
        self.seen = self.seen_saved
        info = self.cond_info
        snap = info["snap"]
        for e in ENGS:
            prog = self.prog[e]
            i = len(prog) - 1
            while prog[i][0] != "cond_begin":
                for k, v in prog[i][0]:
                    if v <= snap.get(k, 0) and info["ext"].get(k, 0) < v:
                        info["ext"][k] = v
                i -= 1
            self.prog[e].append(("cond_end", None, None))

    def barrier(self):
        for e in ENGS:
            waits = []
            for k, v in self.cnt.items():
                if v == 0 or k == e:
                    continue
                if self.seen[e].get(k, 0) >= v:
                    continue
                self.seen[e][k] = v
                waits.append((k, v))
            self.prog[e].append((waits, None, None))
        self.res = {}

    def final_wait(self, e, semkeys):
        waits = [(k, self.cnt[k]) for k in semkeys if self.cnt.get(k, 0) > 0]
        self.prog[e].append((waits, None, None))

    def emit(self):
        nc = self.nc
        with nc.Block() as block:
            def replay(e, eng):
                prog = self.prog[e]
                i = 0
                while i < len(prog):
                    waits, fn, inc = prog[i]
                    if waits == "cond_begin":
                        j = i + 1
                        while prog[j][0] != "cond_end":
                            j += 1
                        body = prog[i + 1:j]
                        n_inc = sum(1 for b in body if b[2] is not None and b[2][0] == e)
                        if body:
                            cm = eng.If(((self.regs[e] >> fn) & 1) != 0)
                            cm.__enter__()
                            for w_, f_, inc_ in body:
                                for k, v in w_:
                                    eng.wait_ge(self.sem[k], v)
                                if f_ is not None:
                                    ins = f_(eng)
                                    if inc_ is not None:
                                        ins.then_inc(self.sem[inc_[0]], inc_[1])
                            cm.__exit__(None, None, None)
                            if n_inc > 0:
                                info = inc
                                cm2 = eng.Else()
                                cm2.__enter__()
                                self.dummy_ctr[e] = self.dummy_ctr.get(e, 0) + 1
                                self.dummy[e](eng, self.dummy_ctr[e]).then_inc(self.sem[e], n_inc)
                                cm2.__exit__(None, None, None)
                        i = j + 1
                        continue
                    for k, v in waits:
                        eng.wait_ge(self.sem[k], v)
                    if fn is not None:
                        ins = fn(eng)
                        if inc is not None:
                            ins.then_inc(self.sem[inc[0]], inc[1])
                    i += 1
                self.prog[e] = []

            @block.tensor
            def _(eng):
                replay("pe", eng)

            @block.scalar
            def _(eng):
                replay("act", eng)

            @block.vector
            def _(eng):
                replay("dve", eng)

            @block.gpsimd
            def _(eng):
                replay("pool", eng)

            @block.sync
            def _(eng):
                replay("sp", eng)


def build_nc(stop_after=None):
    nc = bass.Bass("TRN2", target_bir_lowering=False)
    dt_in = lambda n, s: nc.dram_tensor(n, s, F32, kind="ExternalInput").ap()
    xs = dt_in("xs", [NT + HALO, D])
    ps = dt_in("ps", [NT, DPLE])
    pp = dt_in("pp", [128, PP_N])
    rows = dt_in("rows", [1, 8192])
    w_in = dt_in("w_in", [D, DIN])
    w_out = dt_in("w_out", [D, D])
    w_r = dt_in("w_r", [D, 20])
    b_r = dt_in("b_r", [20, 1])
    w_gate = dt_in("w_gate", [NE, D, DE])
    w_up = dt_in("w_up", [NE, D, DE])
    w_down = dt_in("w_down", [NE, DE, D])
    w_pg = dt_in("w_pg", [D, D])
    w_pp = dt_in("w_pp", [DPLE, D])
    ident = dt_in("ident", [128, 128])
    tri = dt_in("tri", [128, 128])
    tid = nc.dram_tensor("tid", [128, 16], I32, kind="ExternalInput").ap()
    out = nc.dram_tensor("out", [NT, D], F32, kind="ExternalOutput").ap()
    x1_dram = nc.dram_tensor("x1_spill", [NT, D], F32).ap()
    xh_dram = nc.dram_tensor("xh_spill", [NT, XHW], BF16).ap()
    inv_dram = nc.dram_tensor("inv_spill", [NT, 1], I32).ap()
    flags_dram = nc.dram_tensor("flags_spill", [1, 16], I32).ap()
    dbg = nc.dram_tensor("dbg", [128, 64], F32, kind="ExternalOutput").ap() if os.environ.get("K_DBG") else None

    w_in_v = w_in.rearrange("(dc p) f -> p dc f", p=128)
    w_out_v = w_out.rearrange("(dc p) f -> p dc f", p=128)
    w_pg_v = w_pg.rearrange("(dc p) f -> p dc f", p=128)
    w_pp_v = w_pp.rearrange("(dc p) f -> p dc f", p=128)
    w_r_v = w_r.rearrange("(dc p) f -> p dc f", p=128)

    with contextlib.ExitStack() as st:
        S = Sched(nc, st)

        def sb(stack, n, s, d):
            return stack.enter_context(nc.sbuf_tensor(n, s, d))

        banks = [st.enter_context(nc.psum_tensor("pb%d" % i, [128, 512], F32)) for i in range(8)]
        bank_ctr = [0]

        nbanks = [8]

        def nb():
            i = bank_ctr[0] % nbanks[0]
            bank_ctr[0] += 1
            return banks[i], "pb%d" % i

        def mm(o, l, r, start, stop, reads, writes):
            S.op("pe", lambda e: e.matmul(o, l, r, start=start, stop=stop), reads, writes)

        def tp(o, i_, idn, reads, writes):
            S.op("pe", lambda e: e.transpose(o, i_, idn), reads, writes)

        def act(o, i_, f, reads, writes, bias=0.0, scale=1.0):
            S.op("act", lambda e: e.activation(o, i_, f, bias=bias, scale=scale), reads, writes)

        def acp(o, i_, reads, writes):
            S.op("act", lambda e: e.copy(o, i_), reads, writes)

        def tt_(eng, o, a, b, op, reads, writes):
            S.op(eng, lambda e: e.tensor_tensor(o, a, b, op), reads, writes)

        def ts_(o, a, s1, s2, op0, op1, reads, writes):
            if s2 is None:
                S.op("dve", lambda e: e.tensor_scalar(o, a, s1, None, op0), reads, writes)
            else:
                S.op("dve", lambda e: e.tensor_scalar(o, a, s1, s2, op0, op1), reads, writes)

        def stt(o, a, sc, b, op0, op1, reads, writes):
            S.op("dve", lambda e: e.scalar_tensor_tensor(o, a, sc, b, op0, op1), reads, writes)

        def cp(eng, o, i_, reads, writes):
            S.op(eng, lambda e: e.tensor_copy(o, i_), reads, writes)

        def V(f, r, w):
            S.op("dve", f, r, w)

        def pipeline(stages, n):
            offs = [(off if callable(off) else (lambda i, off=off: off), fn) for off, fn in stages]
            last = max(i + off(i) for off, _ in offs for i in range(n))
            for t in range(last + 1):
                for off, fn in offs:
                    for i in range(n):
                        if i + off(i) == t:
                            fn(i)

        idf = sb(st, "idf", [128, 128], F32)
        idb = sb(st, "idb", [128, 128], BF16)
        ones_f = sb(st, "ones_f", [128, 128], F32)
        ones_rb = sb(st, "ones_rb", [1, 128], BF16)
        pps = sb(st, "pps", [128, PP_N], F32)
        brow_b = sb(st, "brow_b", [1, 2048], BF16)
        mv_in = sb(st, "mv_in", [128, 17, 2], F32)
        rs_in = sb(st, "rs_in", [128, 17], F32)
        ohg_all = sb(st, "ohg_all", [128, 16, 4], F32)
        inv_sb = sb(st, "inv_sb", [128, 16], I32)
        big = sb(st, "big", [128, 16384], F32)
        x1T = sb(st, "x1T", [128, 8, NT], BF16)
        wob = sb(st, "wob", [128, 8, 1024], BF16)

        def ppc(c, n=1):
            return pps[:, c:c + n]

        S.dma("sp", None, idf[:], ident, writes=["idf"])
        S.dma("sp", None, pps[:], pp, writes=["pps"])
        S.dma("pool", None, brow_b[:], rows[:, R_BOUT:R_BOUT + 2048], writes=["brow"])
        cp("dve", idb[:], idf[:], ["idf"], ["idb"])
        S.op("dve", lambda e: e.memset(ones_f[:], 1.0), (), ["ones_f"])
        S.op("dve", lambda e: e.memset(ones_rb[:], 1.0), (), ["ones_rb"])

        catT = big[:, 0:8192].bitcast(BF16).rearrange("p (c t) -> p c t", c=8)

        with contextlib.ExitStack() as s1:
            diag = x1T[:, :, :].rearrange("p c t -> p (c t)")[:, 0:4 * 31 * 128].rearrange(
                "p (c k j) -> p c k j", c=4, k=31)
            NWS = 3
            wslab = [sb(s1, "wslab%d" % i, [128, 8, 384], BF16) for i in range(NWS)]
            xcb = sb(s1, "xcb", [128, 4, 1024], BF16)
            xnT = [sb(s1, "xnT%d" % i, [128, 8, 512], BF16) for i in range(2)]
            xnTh = sb(s1, "xnTh", [128, 8, 32], BF16)
            xth = xcb[0:32, 0:2, :].rearrange("p a b -> p (a b)").bitcast(F32)
            xcbh = xcb[0:32, 2, :]
            sth = sb(s1, "sth", [128, 5, 2, 6], F32)
            glu = sb(s1, "glu", [128, 4, 544], BF16)
            cv = sb(s1, "cv", [128, 4, 514], F32)
            NTMP = 8
            tmp = [sb(s1, "tmp%d" % i, [128, 512], F32) for i in range(NTMP)]
            htmp = [sb(s1, "htmp%d" % i, [128, 32], F32) for i in range(4)]
            c_mean = sb(s1, "c_mean", [128, 512], F32)
            c_var = sb(s1, "c_var", [128, 512], F32)
            xt4 = big[:, 8192:12288].rearrange("p (j d) -> p j d", j=4)
            confy = big[:, 12288:14336].rearrange("p (c t) -> p c t", c=4)
            sq = big[:, 14336:16384].rearrange("p (c t) -> p c t", c=4)
            tctr = [0]

            def ntmp():
                i = tctr[0] % NTMP
                tctr[0] += 1
                return tmp[i], "tmp%d" % i

            def maybe_stop(tag):
                if stop_after == tag:
                    S.barrier()
                    S.emit()
                    return True
                return False

            def build_diag(c):
                S.op("dve", lambda e: e.tensor_tensor(
                    diag[:, c, :, :], idf[:].unsqueeze(1).to_broadcast([128, 31, 128]),
                    pps[:, PP_DWW + c * 31:PP_DWW + (c + 1) * 31].unsqueeze(2).to_broadcast([128, 31, 128]),
                    ALU.mult), ["idf", "pps"], ["diag%d" % c])

            S.dma("sp", "d_xh", xth, xs[0:HALO, :], writes=["xcb0", "xcb1"])
            V(lambda e: e.bn_stats(sth[0:32, 4, 0, :], xth[:, 0:512]), ["xcb0", "xcb1"], ["sth4a"])
            V(lambda e: e.bn_stats(sth[0:32, 4, 1, :], xth[:, 512:1024]), ["xcb0", "xcb1"], ["sth4b"])
            V(lambda e: e.bn_aggr(mv_in[0:32, 16, :], sth[0:32, 4, :, :]), ["sth4a", "sth4b"], ["mvh"])
            act(rs_in[0:32, 16:17], mv_in[0:32, 16, 1:2], AF.Sqrt, ["mvh"], ["rsh0"], bias=EPS)
            V(lambda e: e.reciprocal(rs_in[0:32, 16:17], rs_in[0:32, 16:17]), ["rsh0"], ["rsh"])
            ts_(xcbh, xth, mv_in[0:32, 16, 0:1], rs_in[0:32, 16:17], ALU.subtract, ALU.mult,
                ["xcb0", "xcb1", "mvh", "rsh"], ["xcb2"])
            bk, bkk = nb()
            bkb = bk[:].bitcast(BF16)
            for dc in range(8):
                tp(bkb[:, dc * 32:(dc + 1) * 32], xcbh[:, dc * 128:(dc + 1) * 128], idb[0:32, 0:32],
                   ["xcb2", "idb"], [bkk])
            for dc in range(8):
                act(xnTh[:, dc, :], bkb[:, dc * 32:(dc + 1) * 32], AF.Identity, [bkk, "pps"], ["xnTh"],
                    bias=ppc(PP_LNB + dc), scale=ppc(PP_LNG + dc))

            def stepA_load(tt, j):
                g = tt * 4 + j
                S.dma("sp", "d_xt%d" % j, xt4[:, j, :], xs[HALO + g * 128:HALO + (g + 1) * 128, :],
                      writes=["xt%d" % j])
                V(lambda e: e.bn_stats(sth[:, j, 0, :], xt4[:, j, 0:512]), ["xt%d" % j], ["sth%da" % j])
                V(lambda e: e.bn_stats(sth[:, j, 1, :], xt4[:, j, 512:1024]), ["xt%d" % j], ["sth%db" % j])
                V(lambda e: e.bn_aggr(mv_in[:, g, :], sth[:, j, :, :]), ["sth%da" % j, "sth%db" % j], ["mv%d" % g])

            def stepA_norm(tt):
                g0 = tt * 4
                act(rs_in[:, g0:g0 + 4], mv_in[:, g0:g0 + 4, 1], AF.Sqrt, ["mv%d" % (g0 + j) for j in range(4)],
                    ["rs0_%d" % tt], bias=EPS)
                V(lambda e: e.reciprocal(rs_in[:, g0:g0 + 4], rs_in[:, g0:g0 + 4]), ["rs0_%d" % tt], ["rs_%d" % tt])
                for j in range(4):
                    g = g0 + j
                    ts_(xcb[:, j, :], xt4[:, j, :], mv_in[:, g, 0:1], rs_in[:, g:g + 1], ALU.subtract, ALU.mult,
                        ["xt%d" % j, "mv%d" % g, "rs_%d" % tt], ["xcb%d" % j])

            def stepA_tr(tt, dcs):
                xn = xnT[tt % 2]
                xnk = "xnT%d" % (tt % 2)
                for dc in dcs:
                    bk, bkk = nb()
                    bkb = bk[:].bitcast(BF16)
                    for j in range(4):
                        tp(bkb[:, j * 128:(j + 1) * 128], xcb[:, j, dc * 128:(dc + 1) * 128], idb[:],
                           ["xcb%d" % j, "idb"], [bkk])
                    act(xn[:, dc, :], bkb[:, 0:512], AF.Identity, [bkk, "pps"], [xnk + "_%d" % dc],
                        bias=ppc(PP_LNB + dc), scale=ppc(PP_LNG + dc))

            def load_slab(s):
                if s >= 32:
                    return
                slot = s % NWS
                if s % 8 < 4:
                    c0, ncol = (s % 8) * 256, 256
                else:
                    c0, ncol = 1024 + (s % 8 - 4) * 384, 384
                S.dma("pool", "d_ws%d" % slot, wslab[slot][:, :, 0:ncol], w_in_v[:, :, c0:c0 + ncol],
                      writes=["ws%d" % slot])

            def conv_job(tt, i):
                bc_, bck = nb()
                for k in range(31):
                    mm(bc_[:, :], diag[:, i, k, :], glu[:, i, k + 2:k + 2 + 512], k == 0, k == 30,
                       ["diag%d" % i, "glu%d" % i], [bck])
                act(confy[:, i, :], bc_[:], AF.Identity, [bck, "pps"], ["confy%d" % i], bias=ppc(PP_DWB + i))
                act(sq[:, i, :], bc_[:], AF.Square, [bck, "pps"], ["sq%d" % i], bias=ppc(PP_DWB + i))
                if tt < 3:
                    cp("pool", glu[:, i, 0:32], glu[:, i, 512:544], ["glu%d" % i], ["glu%d" % i])

            def slab(tt, sl):
                s = tt * 8 + sl
                slot = s % NWS
                ws = wslab[slot]
                wsk = "ws%d" % slot
                xn = xnT[tt % 2]
                xn_reads = ["xnT%d_%d" % (tt % 2, dc) for dc in range(8)]
                tcols = slice(tt * 512, (tt + 1) * 512)

                def proj(coff):
                    bk_, bkk_ = nb()
                    for dc in range(8):
                        mm(bk_[:, :], ws[:, dc, coff:coff + 128], xn[:, dc, :], dc == 0, dc == 7,
                           [wsk] + xn_reads, [bkk_])
                    return bk_, bkk_

                def proj_h(coff, bank, bcol):
                    bk_, bkk_ = bank
                    for dc in range(8):
                        mm(bk_[:, bcol:bcol + 32], ws[:, dc, coff:coff + 128], xnTh[:, dc, :],
                           dc == 0, dc == 7, [wsk, "xnTh"], [bkk_])

                if sl < 4:
                    i = sl
                    bv, bvk = proj(0)
                    bg, bgk = proj(128)
                    if tt == 0:
                        hb = nb()
                        proj_h(0, hb, 0)
                        proj_h(128, hb, 32)
                    sig, sigk = ntmp()
                    act(sig[:], bg[:], AF.Sigmoid, [bgk, "pps"], [sigk], bias=ppc(PP_BIN + 2 * i + 1))
                    stt(glu[:, i, 32:544], bv[:], ppc(PP_BIN + 2 * i), sig[:], ALU.add, ALU.mult,
                        [bvk, sigk, "pps"], ["glu%d" % i])
                    if tt == 0:
                        act(htmp[0][:], hb[0][:, 32:64], AF.Sigmoid, [hb[1], "pps"], ["ht0"],
                            bias=ppc(PP_BIN + 2 * i + 1))
                        stt(htmp[1][:], hb[0][:, 0:32], ppc(PP_BIN + 2 * i), htmp[0][:], ALU.add, ALU.mult,
                            [hb[1], "ht0", "pps"], ["ht1"])
                        ts_(glu[:, i, 0:32], htmp[1][:], ppc(PP_MASK), None, ALU.mult, ALU.bypass,
                            ["ht1", "pps"], ["glu%d" % i])
                else:
                    i = sl - 4
                    cb_ = PP_BIN + 8 + 3 * i
                    bC, bCk = proj(0)
                    bV, bVk = proj(128)
                    bB, bBk = proj(256)
                    if tt == 0:
                        hb = nb()
                        proj_h(0, hb, 0)
                        proj_h(128, hb, 32)
                    vv, vvk = ntmp()
                    act(vv[:], bV[:], AF.Identity, [bVk, "pps"], [vvk], bias=ppc(cb_ + 1))
                    stt(cv[:, i, 2:514], bC[:], ppc(cb_), vv[:], ALU.add, ALU.mult, [bCk, vvk, "pps"], ["cv%d" % i])
                    if tt == 0:
                        act(htmp[2][:], hb[0][:, 32:64], AF.Identity, [hb[1], "pps"], ["ht2"], bias=ppc(cb_ + 1))
                        stt(htmp[3][:], hb[0][:, 0:32], ppc(cb_), htmp[2][:], ALU.add, ALU.mult,
                            [hb[1], "ht2", "pps"], ["ht3"])
                        ts_(cv[:, i, 0:2], htmp[3][:, 30:32], ppc(PP_MASK), None, ALU.mult, ALU.bypass,
                            ["ht3", "pps"], ["cv%d" % i])
                    tc, tck = ntmp()
                    w0 = PP_SCW + i * 3
                    ts_(tc[:], cv[:, i, 2:514], ppc(w0 + 2), ppc(PP_SCB + i), ALU.mult, ALU.add,
                        ["cv%d" % i, "pps"], [tck])
                    stt(tc[:], cv[:, i, 1:513], ppc(w0 + 1), tc[:], ALU.mult, ALU.add, ["cv%d" % i, "pps", tck], [tck])
                    stt(tc[:], cv[:, i, 0:512], ppc(w0), tc[:], ALU.mult, ALU.add, ["cv%d" % i, "pps", tck], [tck])
                    stt(catT[:, 4 + i, tcols], bB[:], ppc(cb_ + 2), tc[:], ALU.add, ALU.mult,
                        [bBk, tck, "pps"], ["cat%d_%d" % (4 + i, tt)])
                    if tt < 3:
                        cp("pool", cv[:, i, 0:2], cv[:, i, 512:514], ["cv%d" % i], ["cv%d" % i])

            cst = {}

            def stepC1(tt):
                b1_, b1k = nb()
                b2_, b2k = nb()
                for i in range(4):
                    mm(b1_[:, :], ones_f[:], confy[:, i, :], i == 0, i == 3, ["ones_f", "confy%d" % i], [b1k])
                for i in range(4):
                    mm(b2_[:, :], ones_f[:], sq[:, i, :], i == 0, i == 3, ["ones_f", "sq%d" % i], [b2k])
                mean, meank = c_mean, "c_mean"
                m2, m2k = ntmp()
                var, vark = c_var, "c_var"
                ts_(mean[:], b1_[:], 1.0 / 512, None, ALU.mult, ALU.bypass, [b1k], [meank])
                tt_("pool", m2[:], mean[:], mean[:], ALU.mult, [meank], [m2k])
                stt(var[:], b2_[:], 1.0 / 512, m2[:], ALU.mult, ALU.subtract, [b2k, m2k], [vark])
                act(var[:], var[:], AF.Sqrt, [vark], [vark], bias=EPS)
                V(lambda e: e.reciprocal(var[:], var[:]), [vark], [vark])
                cst[tt] = (mean, meank, var, vark, [(confy[:, i, :], "confy%d" % i) for i in range(4)],
                           [(sq[:, i, :], "sq%d" % i) for i in range(4)])

            def stepC2(tt):
                mean, meank, var, vark, tas, sgs = cst[tt]
                for i in range(4):
                    ta, tak = tas[i]
                    tt_("pool", ta, confy[:, i, :], mean[:], ALU.subtract, ["confy%d" % i, meank], [tak])
                for i in range(4):
                    ta, tak = tas[i]
                    tt_("dve", ta, ta, var[:], ALU.mult, [tak, vark], [tak])

            def stepC3(tt):
                tcols = slice(tt * 512, (tt + 1) * 512)
                mean, meank, var, vark, tas, sgs = cst[tt]
                for i in range(4):
                    ta, tak = tas[i]
                    sg, sgk = sgs[i]
                    act(sg, ta, AF.Sigmoid, [tak, "pps"], [sgk], bias=ppc(PP_CLB + i), scale=ppc(PP_CLG + i))
                for i in range(4):
                    ta, tak = tas[i]
                    sg, sgk = sgs[i]
                    ts_(ta, ta, ppc(PP_CLG + i), ppc(PP_CLB + i), ALU.mult, ALU.add, [tak, "pps", sgk], [tak])
                for i in range(4):
                    ta, tak = tas[i]
                    sg, sgk = sgs[i]
                    tt_("pool", catT[:, i, tcols], ta, sg, ALU.mult, [tak, sgk], ["cat%d_%d" % (i, tt)])

            load_slab(0)
            load_slab(1)
            for j in range(4):
                stepA_load(0, j)
            stepA_norm(0)
            stepA_tr(0, range(8))
            for tt in range(4):
                pend = None
                for sl in range(8):
                    load_slab(tt * 8 + sl + 2)
                    if tt == 0 and sl < 4:
                        build_diag(sl)
                    slab(tt, sl)
                    if pend is not None:
                        conv_job(*pend)
                    pend = (tt, sl) if sl < 4 else None
                    if tt == 1 and sl == 0:
                        S.dma("pool", "d_wob", wob[:], w_out_v, writes=["wob"])
                    if tt < 3:
                        if sl < 4:
                            stepA_load(tt + 1, sl)
                        elif sl == 4:
                            stepA_norm(tt + 1)
                        elif sl == 6:
                            stepA_tr(tt + 1, range(0, 4))
                        elif sl == 7:
                            stepA_tr(tt + 1, range(4, 8))
                    if sl == 5:
                        stepC1(tt)
                    elif sl == 6:
                        stepC2(tt)
                    elif sl == 7:
                        stepC3(tt)
            S.barrier()
            S.emit()
        if stop_after == "1a":
            return nc

        combT = sb(st, "combT", [48, NT], BF16)
        sM = st.enter_context(contextlib.ExitStack())
        NSLOT = 3
        wg = [None] * NSLOT
        wu = [None] * NSLOT
        NWD = 4
        wd = [None] * NWD
        wg[0] = sb(sM, "wg0", [128, 8, DE], BF16)
        wu[0] = sb(sM, "wu0", [128, 8, DE], BF16)
        wd[0] = sb(sM, "wd0", [128, 2, D], BF16)

        def load_expert(e_):
            slot = e_ % NSLOT
            S.dma("pool", "d_mwg%d" % slot, wg[slot][:], w_gate[e_].rearrange("(dc p) f -> p dc f", p=128), writes=["wg%d" % slot])
            S.dma("pool", "d_mwu%d" % slot, wu[slot][:], w_up[e_].rearrange("(dc p) f -> p dc f", p=128), writes=["wu%d" % slot])
            S.dma("pool", "d_mwd%d" % (e_ % NWD), wd[e_ % NWD][:], w_down[e_].rearrange("(fc p) d -> p fc d", p=128),
                  writes=["wd%d" % (e_ % NWD)])

        with contextlib.ExitStack() as s2:
            bc_ag = sb(s2, "bc_ag", [128, 1024], F32)
            bc_g1 = sb(s2, "bc_g1", [128, 1024], F32)
            bc_b1 = sb(s2, "bc_b1", [128, 1024], F32)
            NH = 3
            hib = [sb(s2, "hib%d" % i, [128, 1024], BF16) for i in range(NH)]
            lob = [sb(s2, "lob%d" % i, [128, 1024], BF16) for i in range(NH)]
            x1loT = [sb(s2, "x1loT%d" % i, [128, 8, 512], BF16) for i in range(2)]
            r1x = sb(s2, "r1x", [128, 4, 1024], F32)
            wr_f = sb(s2, "wr_f", [128, 8, 20], F32)
            wr_hi = sb(s2, "wr_hi", [128, 8, 20], BF16)
            wr_lo = sb(s2, "wr_lo", [128, 8, 20], BF16)
            br_sb = sb(s2, "br_sb", [20, 1], F32)
            lgT = sb(s2, "lgT", [20, 512], F32)
            lg = sb(s2, "lg", [128, 4, 20], F32)
            chl = sb(s2, "chl", [128, 4, 48], BF16)
            osum = sb(s2, "osum", [128, 4, 1], F32)
            onm = sb(s2, "onm", [128, 4, 1], F32)
            tri_sb = sb(s2, "tri_sb", [128, 128], F32)
            tid_sb = sb(s2, "tid_sb", [128, 16], I32)
            Wt = sb(s2, "Wt", [128, 16, 4], F32)
            Tt = sb(s2, "Tt", [128, 16, 4], F32)
            Et = sb(s2, "Et", [128, 16, 4], F32)
            cntv = sb(s2, "cntv", [128, 4], F32)
            startv = sb(s2, "startv", [128, 4], F32)
            hiv = sb(s2, "hiv", [128, 4], F32)
            f1 = sb(s2, "f1", [128, 4], F32)
            f2 = sb(s2, "f2", [128, 4], F32)
            posf = sb(s2, "posf", [128, 16], F32)
            posi = sb(s2, "posi", [128, 16], I32)
            flagsf = sb(s2, "flagsf", [128, 4, 4], F32)
            flagsi = sb(s2, "flagsi", [128, 16], I32)
            bitsf = sb(s2, "bitsf", [128, 1], F32)
            st1 = sb(s2, "st1", [128, 16, 2, 6], F32)
            mv1 = sb(s2, "mv1", [128, 16, 2], F32)
            rs1 = sb(s2, "rs1", [128, 16], F32)
            nb1 = sb(s2, "nb1", [128, 16], F32)
            nb_in = sb(s2, "nb_in", [128, 16], F32)
            brow_hl = sb(s2, "brow_hl", [64, 1024], BF16)
            ones64 = sb(s2, "ones64", [64, 128], BF16)
            r_sm = [sb(s2, "rsm%d" % i, [128, 4, 16], F32) for i in range(6)]
            r_s4 = [sb(s2, "rs4_%d" % i, [128, 4, 4], F32) for i in range(4)]
            r_s1 = [sb(s2, "rs1_%d" % i, [128, 4], F32) for i in range(8)]
            r1b = big[:, 8192:12288].rearrange("p (j d) -> p j d", j=4)
            xrb = big[:, 12288:16384].rearrange("p (j d) -> p j d", j=4)

            def r1v(g):
                s_ = g % 8
                return (r1b[:, s_, :] if s_ < 4 else r1x[:, s_ - 4, :]), "r1s%d" % s_

            S.dma("sp", None, bc_ag[:], rows[:, R_LNIG:R_LNIG + 1024].partition_broadcast(128), writes=["bc_ag"])
            S.dma("sp", None, bc_g1[:], rows[:, R_L1G:R_L1G + 1024].partition_broadcast(128), writes=["bc_g1"])
            S.dma("sp", None, bc_b1[:], rows[:, R_L1B:R_L1B + 1024].partition_broadcast(128), writes=["bc_b1"])
            S.dma("sp", None, wr_f[:], w_r_v, writes=["wr_f"])
            S.dma("sp", None, br_sb[:], b_r, writes=["br_sb"])
            ts_(bc_ag[:], bc_ag[:], ALPHA, None, ALU.mult, ALU.bypass, ["bc_ag"], ["bc_ag"])
            cp("dve", wr_hi[:], wr_f[:], ["wr_f"], ["wr_hi"])
            tt_("dve", wr_lo[:], wr_f[:], wr_hi[:], ALU.subtract, ["wr_f", "wr_hi"], ["wr_lo"])
            load_expert(0)
            V(lambda e: e.memset(chl[:], 0.0), [], ["chl_z"])
            V(lambda e: e.scalar_tensor_tensor(nb_in[:], mv_in[:, 0:16, 0], -1.0, rs_in[:, 0:16], ALU.mult, ALU.mult),
              [], ["nb_in"])

            mixb = {}
            V(lambda e: e.memset(brow_hl[:], 0.0), [], ["brow_hl"])
            V(lambda e: e.memset(ones64[:], 1.0), [], ["ones64"])
            for pb_ in (0, 32):
                f_ = xrb[pb_:pb_ + 1, 2, :]
                f2_ = xrb[pb_:pb_ + 1, 3, :]
                S.dma("sp", None, f_, rows[:, R_BOUT:R_BOUT + 1024], writes=["xr2"])
                S.dma("sp", None, f2_, rows[:, R_LNIB:R_LNIB + 1024], writes=["xr3"])
                stt(f_, f2_, ALPHA, f_, ALU.mult, ALU.add, ["xr2", "xr3"], ["xr2"])
                cp("dve", brow_hl[pb_:pb_ + 1, :], f_, ["xr2", "brow_hl"], ["brow_hl"])
                if pb_ == 32:
                    tt_("dve", brow_hl[32:33, :], f_, brow_hl[32:33, :], ALU.subtract, ["xr2", "brow_hl"], ["brow_hl"])

            def b_dma(g):
                S.dma("sp", "d_xr%d" % (g % 4), xrb[:, g % 4, :], xs[HALO + g * 128:HALO + (g + 1) * 128, :],
                      writes=["xr%d" % (g % 4)])

            def b_xn(g):
                xk = "xr%d" % (g % 4)
                xr = xrb[:, g % 4, :]
                act(xr, xr, AF.Identity, [xk, "nb_in"], [xk], bias=nb_in[:, g:g + 1], scale=rs_in[:, g:g + 1])

            def b_mix(g):
                tt, j = divmod(g, 4)
                gc = slice(g * 128, (g + 1) * 128)
                xk = "xr%d" % (g % 4)
                xr = xrb[:, g % 4, :]
                tt_("pool", xr, xr, bc_ag[:], ALU.mult, [xk, "bc_ag"], [xk])
                pbk = []
                for h in range(2):
                    hs = slice(h * 512, (h + 1) * 512)
                    bk, bkk = nb()
                    for c in range(8):
                        mm(bk[:, :], catT[:, c, gc], wob[:, c, hs], c == 0, False, ["cat%d_%d" % (c, tt), "wob"], [bkk])
                    mm(bk[:, :], ones64[:, :], brow_hl[:, hs], False, True, ["ones64", "brow_hl"], [bkk])
                    pbk.append((bk, bkk))
                mixb[g] = pbk

            def b_r1(g):
                xk = "xr%d" % (g % 4)
                xr = xrb[:, g % 4, :]
                r1, r1k = r1v(g)
                pbk = mixb.pop(g)
                for h in range(2):
                    hs = slice(h * 512, (h + 1) * 512)
                    tt_("dve", r1[:, hs], xr[:, hs], pbk[h][0][:], ALU.add, [xk, pbk[h][1]], [r1k + "_%d" % h])
                V(lambda e: e.bn_stats(st1[:, g, 0, :], r1[:, 0:512]), [r1k + "_0"], ["st1a%d" % g])
                V(lambda e: e.bn_stats(st1[:, g, 1, :], r1[:, 512:1024]), [r1k + "_1"], ["st1b%d" % g])
                V(lambda e: e.bn_aggr(mv1[:, g, :], st1[:, g, :, :]), ["st1a%d" % g, "st1b%d" % g], ["mv1_%d" % g])

            def b_sqrt(g):
                act(rs1[:, g:g + 1], mv1[:, g, 1:2], AF.Sqrt, ["mv1_%d" % g], ["rs1a%d" % g], bias=EPS)

            def b_rcp(g):
                V(lambda e: e.reciprocal(rs1[:, g:g + 1], rs1[:, g:g + 1]), ["rs1a%d" % g], ["rs1_%d" % g])
                V(lambda e: e.scalar_tensor_tensor(nb1[:, g:g + 1], mv1[:, g, 0:1], -1.0, rs1[:, g:g + 1],
                                                   ALU.mult, ALU.mult), ["rs1_%d" % g, "mv1_%d" % g], ["nb1_%d" % g])

            def b_n1(g):
                r1, r1k = r1v(g)
                rk = [r1k + "_0", r1k + "_1"]
                act(r1, r1, AF.Identity, rk + ["nb1_%d" % g, "rs1_%d" % g], rk, bias=nb1[:, g:g + 1], scale=rs1[:, g:g + 1])

            def b_g1(g):
                r1, r1k = r1v(g)
                rk = [r1k + "_0", r1k + "_1"]
                tt_("pool", r1, r1, bc_g1[:], ALU.mult, rk + ["bc_g1"], rk)

            def b_b1(g):
                r1, r1k = r1v(g)
                rk = [r1k + "_0", r1k + "_1"]
                tt_("dve", r1, r1, bc_b1[:], ALU.add, rk + ["bc_b1"], rk)
                S.dma("sp", "d_x1s%d" % (g % 8), x1_dram[g * 128:(g + 1) * 128, :], r1, reads=rk, writes=["x1d%d" % g])

            def b_hi(g):
                r1, r1k = r1v(g)
                rk = [r1k + "_0", r1k + "_1"]
                acp(hib[g % NH][:], r1, rk, ["hib%d" % (g % NH)])
                S.dma("sp", "d_xh%d" % (g % NH), xh_dram[g * 128:(g + 1) * 128, 0:1024], hib[g % NH][:],
                      reads=["hib%d" % (g % NH)], writes=["xhd%d" % g])

            def b_lo(g):
                r1, r1k = r1v(g)
                rk = [r1k + "_0", r1k + "_1"]
                hb_, lb_ = hib[g % NH], lob[g % NH]
                hk, lk = "hib%d" % (g % NH), "lob%d" % (g % NH)
                tt_("dve", lb_[:], r1, hb_[:], ALU.subtract, rk + [hk], [lk])
                bk, bkk = nb()
                bkb = bk[:].bitcast(BF16)
                for dc in range(8):
                    tp(bkb[:, dc * 128:(dc + 1) * 128], hb_[:, dc * 128:(dc + 1) * 128], idb[:], [hk, "idb"], [bkk])
                mixb[("h", g)] = (bkb, bkk)

            def b_x1T(g):
                gc = slice(g * 128, (g + 1) * 128)
                lb_ = lob[g % NH]
                lk = "lob%d" % (g % NH)
                bkb, bkk = mixb.pop(("h", g))
                acp(x1T[:, :, gc], bkb.rearrange("p (c t) -> p c t", c=8), [bkk], ["x1T_%d" % g])
                bk2, bkk2 = nb()
                bkb2 = bk2[:].bitcast(BF16)
                for dc in range(8):
                    tp(bkb2[:, dc * 128:(dc + 1) * 128], lb_[:, dc * 128:(dc + 1) * 128], idb[:], [lk, "idb"], [bkk2])
                mixb[("l", g)] = (bkb2, bkk2)

            def b_x1lo(g):
                tt, j = divmod(g, 4)
                bkb2, bkk2 = mixb.pop(("l", g))
                acp(x1loT[tt % 2][:, :, j * 128:(j + 1) * 128], bkb2.rearrange("p (c t) -> p c t", c=8),
                    [bkk2], ["x1lo%d_%d" % (tt % 2, j)])

            gmax, gsum, gp, m1, m2_, d21, w1, w2 = [r_s1[i] for i in range(8)]
            _unused, dg, exg, pen = [r_s4[i] for i in range(4)]
            lem, oh1, lem2, oh2, cA, cB = [r_sm[i] for i in range(6)]
            lgG = lg[:, :, 0:4]
            lgE4 = lg[:, :, 4:20].rearrange("p a (g e) -> p a g e", g=4)
            bc4 = lambda a: a[:].unsqueeze(2).to_broadcast([128, 4, 4])
            bc16 = lambda a: a[:].unsqueeze(2).to_broadcast([128, 4, 16])

            def r_0(tt):
                tcols = slice(tt * 512, (tt + 1) * 512)
                x1k = ["x1T_%d" % (tt * 4 + j) for j in range(4)]
                lok = ["x1lo%d_%d" % (tt % 2, j) for j in range(4)]
                br_, brk = nb()
                n = 0
                for (wt, wk, xsrc, xk) in ((wr_hi, "wr_hi", "hi", x1k), (wr_hi, "wr_hi", "lo", lok), (wr_lo, "wr_lo", "hi", x1k)):
                    for dc in range(8):
                        rhs = x1T[:, dc, tcols] if xsrc == "hi" else x1loT[tt % 2][:, dc, :]
                        mm(br_[0:20, :], wt[:, dc, :], rhs, n == 0, n == 23, [wk] + xk, [brk])
                        n += 1
                act(lgT[:], br_[0:20, :], AF.Identity, [brk, "br_sb"], ["lgT"], bias=br_sb[:, 0:1])
                bt_, btk = nb()
                for j in range(4):
                    tp(bt_[:, j * 20:(j + 1) * 20], lgT[:, j * 128:(j + 1) * 128], idf[0:20, 0:20], ["lgT", "idf"], [btk])
                cp("dve", lg[:].rearrange("p a b -> p (a b)"), bt_[:, 0:80], [btk], ["lg"])

            def r_1(tt):
                ohg = ohg_all[:, tt * 4:(tt + 1) * 4, :]
                V(lambda e: e.tensor_reduce(gmax[:], lgG, AX.X, ALU.max), ["lg"], ["gmax"])
                V(lambda e: e.tensor_tensor(ohg, lgG, bc4(gmax), ALU.is_equal), ["lg", "gmax"], ["ohg"])
                V(lambda e: e.tensor_copy(osum[:], ohg[:, :, 0:1]), ["ohg"], ["osum"])
                for g_ in range(1, 4):
                    V(lambda e: e.tensor_scalar(onm[:], osum[:], -1.0, 1.0, ALU.mult, ALU.add), ["osum"], ["onm"])
                    V(lambda e, g_=g_: e.tensor_tensor(ohg[:, :, g_:g_ + 1], ohg[:, :, g_:g_ + 1], onm[:], ALU.mult), ["ohg", "onm"], ["ohg"])
                    if g_ < 3:
                        V(lambda e, g_=g_: e.tensor_tensor(osum[:], osum[:], ohg[:, :, g_:g_ + 1], ALU.add), ["osum", "ohg"], ["osum"])
                V(lambda e: e.tensor_tensor(dg[:], lgG, bc4(gmax), ALU.subtract), ["lg", "gmax"], ["dg"])
                act(exg[:], dg[:], AF.Exp, ["dg"], ["exg"])
                V(lambda e: e.tensor_scalar(pen[:], ohg, BIG, -BIG, ALU.mult, ALU.add), ["ohg"], ["pen"])
                V(lambda e: e.tensor_tensor(lem[:].rearrange("p a (g e) -> p a g e", g=4), lgE4,
                                            pen[:].unsqueeze(3).to_broadcast([128, 4, 4, 4]), ALU.add), ["lg", "pen"], ["lem"])
                V(lambda e: e.tensor_reduce(m1[:], lem[:], AX.X, ALU.max), ["lem"], ["m1"])
                V(lambda e: e.tensor_tensor(oh1[:], lem[:], bc16(m1), ALU.is_equal), ["lem", "m1"], ["oh1"])
                V(lambda e: e.scalar_tensor_tensor(lem2[:], oh1[:], -BIG, lem[:], ALU.mult, ALU.add), ["oh1", "lem"], ["lem2"])
                V(lambda e: e.tensor_reduce(m2_[:], lem2[:], AX.X, ALU.max), ["lem2"], ["m2"])
                V(lambda e: e.tensor_tensor(oh2[:], lem2[:], bc16(m2_), ALU.is_equal), ["lem2", "m2"], ["oh2"])
                V(lambda e: e.tensor_tensor(d21[:], m2_[:], m1[:], ALU.subtract), ["m1", "m2"], ["d21"])
                act(d21[:], d21[:], AF.Exp, ["d21"], ["d21"])

            def r_2(tt):
                tcols = slice(tt * 512, (tt + 1) * 512)
                V(lambda e: e.tensor_reduce(gsum[:], exg[:], AX.X, ALU.add), ["exg"], ["gsum"])
                V(lambda e: e.reciprocal(gp[:], gsum[:]), ["gsum"], ["gp"])
                V(lambda e: e.tensor_scalar(d21[:], d21[:], 1.0, None, ALU.add), ["d21"], ["d21"])
                V(lambda e: e.reciprocal(w1[:], d21[:]), ["d21"], ["w1"])
                V(lambda e: e.tensor_scalar(w2[:], w1[:], -1.0, 1.0, ALU.mult, ALU.add), ["w1"], ["w2"])
                V(lambda e: e.tensor_tensor(w1[:], w1[:], gp[:], ALU.mult), ["w1", "gp"], ["w1"])
                V(lambda e: e.tensor_tensor(w2[:], w2[:], gp[:], ALU.mult), ["w2", "gp"], ["w2"])
                V(lambda e: e.tensor_tensor(cA[:], oh1[:], bc16(w1), ALU.mult), ["oh1", "w1"], ["cA"])
                V(lambda e: e.tensor_tensor(cB[:], oh2[:], bc16(w2), ALU.mult), ["oh2", "w2"], ["cB"])
                V(lambda e: e.tensor_tensor(cA[:], cA[:], cB[:], ALU.add), ["cA", "cB"], ["cA"])
                V(lambda e: e.tensor_copy(chl[:, :, 0:16], cA[:]), ["cA"], ["chl_hi"])
                V(lambda e: e.tensor_tensor(chl[:, :, 32:48], cA[:], chl[:, :, 0:16], ALU.subtract), ["cA", "chl_hi"], ["chl_lo"])
                S.dma("sp", "d_chl", xh_dram[tt * 512:(tt + 1) * 512, 1024:XHW].rearrange("(j p) c -> p j c", p=128),
                      chl[:], reads=["chl_hi", "chl_lo", "chl_z"], writes=["chld%d" % tt])

            def grp(fn, off):
                return (off, lambda g: fn(g // 4) if g % 4 == 3 else None)

            pipeline([(0, b_dma), (1, b_xn), (2, b_mix), (3, b_r1), (4, b_sqrt), (5, b_rcp), (6, b_n1), (7, b_g1),
                      (8, b_b1), (9, b_hi), (10, b_lo), (11, b_x1T), (12, b_x1lo),
                      grp(r_0, 13), grp(r_1, 14), grp(r_2, 15)], 16)

            oflat = ohg_all[:].rearrange("p s g -> p (s g)")
            S.dma("sp", None, tri_sb[:], tri, writes=["tri_sb"])
            S.dma("sp", None, tid_sb[:], tid, writes=["tid_sb"])
            bw, bwk = nb()
            mm(bw[:, 0:64], tri_sb[:], oflat, True, True, ["tri_sb", "ohg"], [bwk])
            bt, btk = nb()
            mm(bt[:, 0:64], ones_f[:], oflat, True, True, ["ones_f", "ohg"], [btk])
            cp("dve", Wt[:].rearrange("p s g -> p (s g)"), bw[:, 0:64], [bwk], ["Wt"])
            cp("dve", Tt[:].rearrange("p s g -> p (s g)"), bt[:, 0:64], [btk], ["Tt"])
            V(lambda e: e.memset(Et[:, 0, :], 0.0), [], ["Et"])
            for s_ in range(1, 16):
                V(lambda e, s_=s_: e.tensor_tensor(Et[:, s_, :], Et[:, s_ - 1, :], Tt[:, s_ - 1, :], ALU.add), ["Et", "Tt"], ["Et"])
            V(lambda e: e.tensor_tensor(cntv[:], Et[:, 15, :], Tt[:, 15, :], ALU.add), ["Et", "Tt"], ["cntv"])
            V(lambda e: e.memset(startv[:, 0:1], 0.0), [], ["startv"])
            for g_ in range(1, 4):
                V(lambda e, g_=g_: e.tensor_tensor(startv[:, g_:g_ + 1], startv[:, g_ - 1:g_], cntv[:, g_ - 1:g_], ALU.add),
                  ["startv", "cntv"], ["startv"])
            V(lambda e: e.tensor_tensor(Wt[:], Wt[:], Et[:], ALU.add), ["Wt", "Et"], ["Wt"])
            V(lambda e: e.tensor_tensor(Wt[:], Wt[:], startv[:].unsqueeze(1).to_broadcast([128, 16, 4]), ALU.add),
              ["Wt", "startv"], ["Wt"])
            V(lambda e: e.tensor_tensor(Wt[:], Wt[:], ohg_all[:], ALU.mult), ["Wt", "ohg"], ["Wt"])
            V(lambda e: e.tensor_reduce(posf[:], Wt[:], AX.X, ALU.add), ["Wt"], ["posf"])
            V(lambda e: e.tensor_copy(posi[:], posf[:]), ["posf"], ["posi"])
            for s_ in range(16):
                S.dma_fn("pool", "d_inv", lambda eng, s_=s_: eng.indirect_dma_start(
                    out=inv_dram[:, :], out_offset=bass.IndirectOffsetOnAxis(ap=posi[:, s_:s_ + 1], axis=0),
                    in_=tid_sb[:, s_:s_ + 1], in_offset=None), reads=["posi", "tid_sb"], writes=["inv_dram%d" % s_])
            for q in range(16):
                S.dma("sp" if q % 2 == 0 else "act", "d_invl%d" % (q % 2), inv_sb[:, q:q + 1], inv_dram[q * 128:(q + 1) * 128, :],
                      reads=["inv_dram%d" % s_ for s_ in range(16)], writes=["inv_sb%d" % q])
            V(lambda e: e.tensor_tensor(hiv[:], startv[:], cntv[:], ALU.add), ["startv", "cntv"], ["hiv"])
            for tt in range(4):
                V(lambda e, tt=tt: e.tensor_scalar(f1[:], startv[:], float((tt + 1) * 512), None, ALU.is_lt), ["startv"], ["f1"])
                V(lambda e, tt=tt: e.tensor_scalar(f2[:], hiv[:], float(tt * 512), None, ALU.is_gt), ["hiv"], ["f2"])
                V(lambda e, tt=tt: e.tensor_tensor(flagsf[:, tt, :], f1[:], f2[:], ALU.mult), ["f1", "f2"], ["flagsf"])
            V(lambda e: e.memset(bitsf[:], 0.0), [], ["bitsf"])
            for k in range(16):
                V(lambda e, k=k: e.scalar_tensor_tensor(bitsf[:], flagsf[:, k // 4, k % 4:k % 4 + 1], float(2 ** k), bitsf[:],
                                                        ALU.mult, ALU.add), ["flagsf", "bitsf"], ["bitsf"])
            if os.environ.get("K_ALLFLAGS"):
                V(lambda e: e.memset(bitsf[:], 65535.0), ["bitsf"], ["bitsf"])
            V(lambda e: e.tensor_copy(flagsi[:, 0:1], bitsf[:]), ["bitsf"], ["flagsi"])
            S.dma("sp", None, flags_dram[0:1, 0:1], flagsi[0:1, 0:1], reads=["flagsi"], writes=["flags"])
            if dbg is not None:
                S.dma("sp", None, dbg[:, 0:16], flagsf[:].rearrange("p a b -> p (a b)"), reads=["flagsf"], writes=["dbg0"])
                S.dma("sp", None, dbg[:, 16:18], flagsi[:, 0:2].bitcast(F32), reads=["flagsi"], writes=["dbg1"])
                S.dma("sp", None, dbg[:, 20:24], startv[:], reads=["startv"], writes=["dbg2"])
                S.dma("sp", None, dbg[:, 24:28], cntv[:], reads=["cntv"], writes=["dbg3"])
                S.dma("sp", None, dbg[:, 32:48], posf[:], reads=["posf"], writes=["dbg4"])
            S.barrier()
            S.emit()
        if stop_after == "1b":
            return nc

        yacc = big[:, :].rearrange("p (g d) -> p g d", g=16)
        with contextlib.ExitStack() as s3:
            for i in range(1, NSLOT):
                wg[i] = sb(s3, "wg%d" % i, [128, 8, DE], BF16)
                wu[i] = sb(s3, "wu%d" % i, [128, 8, DE], BF16)
            for i in range(1, NWD):
                wd[i] = sb(s3, "wd%d" % i, [128, 2, D], BF16)
            NHID = 6
            hid_r = [sb(s3, "hidr%d" % i, [128, 2, 512], BF16) for i in range(2)]
            sel = sb(s3, "sel", [48, NE, 128], BF16)
            scr = sb(s3, "scr", [1, 512], F32)
            scrp = sb(s3, "scrp", [128, 160], F32)
            NXG = 6
            xg = [sb(s3, "xg%d" % i, [128, XHW], BF16) for i in range(NXG)]
            hid = [xg[i][:, 0:1024].rearrange("p (f t) -> p f t", f=2) for i in range(NXG)]
            hid = hid + hid_r
            hidk = lambda k: ["hid%d" % k, "xg%d" % k] if k < NXG else ["hid%d" % k]
            cbs = [sb(s3, "cbs%d" % i, [128, 512], F32) for i in range(2)]
            NSS = 3
            sbuf_s = [sb(s3, "mss%d" % i, [128, 512], F32) for i in range(NSS)]
            sbuf_t = [sb(s3, "mst%d" % i, [128, 512], F32) for i in range(NSS)]

            V(lambda e: e.memset(sel[:], 0.0), [], ["sel"])
            V(lambda e: e.tensor_copy(sel[0:16, :, :], idf[0:16, 0:16].unsqueeze(2).to_broadcast([16, NE, 128])),
              ["idf", "sel"], ["sel"])
            V(lambda e: e.tensor_copy(sel[32:48, :, :], idf[32:48, 32:48].unsqueeze(2).to_broadcast([16, NE, 128])),
              ["idf", "sel"], ["sel"])
            nbanks[0] = 7
            S.op("dve", lambda e: e.memset(scr[:], 0.0), [], ["scr"])
            S.dummy["pe"] = lambda eng, i: eng.matmul(banks[7][0:1, i:i + 1], ones_rb[0:1, 0:1], ones_rb[0:1, 0:1], start=True, stop=True)
            S.dummy["act"] = lambda eng, i: eng.copy(scr[0:1, 384 + i:385 + i], scr[0:1, 511:512])
            S.dummy["dve"] = lambda eng, i: eng.memset(scr[0:1, i:i + 1], 0.0)
            S.dummy["pool"] = lambda eng, i: eng.memset(scrp[:, i:i + 1], 0.0)

            def load_flags(e):
                def f(eng):
                    r = eng.alloc_register("fl_%s" % e)
                    eng.reg_load(r, flags_dram[0:1, 0:1])
                    S.regs[e] = eng.snap(r)
                    return None
                return f
            for e in ("pe", "act", "dve", "pool"):
                S.raw(e, load_flags(e))

            S.op("dve", lambda e: e.memset(big[:, 0:8192], 0.0), [], ["yz0"])


            def gather_q(q):
                xg_ = xg[q % NXG]
                S.dma_fn("pool", "d_xg%d" % (q % NXG), lambda eng: eng.indirect_dma_start(
                    out=xg_[:, :], out_offset=None, in_=xh_dram[:, :],
                    in_offset=bass.IndirectOffsetOnAxis(ap=inv_sb[:, q:q + 1], axis=0)), reads=[],
                    writes=["xg%d" % (q % NXG), "hid%d" % (q % NXG)])

            def trans_q(q):
                qc = slice(q * 128, (q + 1) * 128)
                xg_ = xg[q % NXG]
                xgk = "xg%d" % (q % NXG)
                bk, bkk = nb()
                bkb = bk[:].bitcast(BF16)
                for dc in range(8):
                    tp(bkb[:, dc * 128:(dc + 1) * 128], xg_[:, dc * 128:(dc + 1) * 128], idb[:], [xgk, "idb"], [bkk])
                acp(x1T[:, :, qc], bkb.rearrange("p (c t) -> p c t", c=8), [bkk], ["x1T_%d" % q])
                bk2, bkk2 = nb()
                bkb2 = bk2[:].bitcast(BF16)
                tp(bkb2[0:48, 0:128], xg_[:, 1024:XHW], idb[:], [xgk, "idb"], [bkk2])
                cp("dve", combT[:, qc], bkb2[0:48, 0:128], [bkk2], ["combT_%d" % q])

            for q in range(NXG):
                gather_q(q)
            load_expert(1)
            S.op("pool", lambda e: e.memset(big[:, 8192:16384], 0.0), [], ["yz1"])

            steps = [(e_, tt) for e_ in range(NE) for tt in range(4)]
            pend_b = {}
            uctr = 0

            def down(e_, tt, hslot):
                slot = e_ % NWD
                for j in range(4):
                    g = tt * 4 + j
                    for h in range(2):
                        bk, bkk = nb()
                        for fc in range(2):
                            mm(bk[:, :], hid[hslot][:, fc, j * 128:(j + 1) * 128], wd[slot][:, fc, h * 512:(h + 1) * 512],
                               fc == 0, fc == 1, hidk(hslot) + ["wd%d" % slot], [bkk])
                        ysl = yacc[:, g, h * 512:(h + 1) * 512]
                        yk = "y_%d_%d" % (g, h)
                        tt_("dve", ysl, ysl, bk[:], ALU.add, [bkk, yk, "yz0", "yz1"], [yk])

            for si, (e_, tt) in enumerate(steps):
                slot = e_ % NSLOT
                tcols = slice(tt * 512, (tt + 1) * 512)
                fl = tt * 4 + e_ // 4
                if e_ == 0:
                    for q in range(tt * 4, tt * 4 + 4):
                        trans_q(q)
                        if q + NXG < 16:
                            gather_q(q + NXG)
                    if tt == 1:
                        load_expert(2)
                if tt == 1 and e_ >= 1 and e_ + 2 < NE:
                    load_expert(e_ + 2)
                if tt == 2 and e_ == NE - 2:
                    S.dma("pool", "d_wob", wob[:], w_pg_v, writes=["wob"])
                S.cond_begin(fl)
                xk = ["x1T_%d" % (tt * 4 + j) for j in range(4)]
                ck = ["combT_%d" % (tt * 4 + j) for j in range(4)]
                bcb, bcbk = nb()
                mm(bcb[:, :], sel[:, e_, :], combT[:, tcols], True, True, ["sel"] + ck, [bcbk])
                pg = []
                pu = []
                for fc in range(2):
                    bk, bkk = nb()
                    for dc in range(8):
                        mm(bk[:, :], wg[slot][:, dc, fc * 128:(fc + 1) * 128], x1T[:, dc, tcols], dc == 0, dc == 7,
                           ["wg%d" % slot] + xk, [bkk])
                    pg.append((bk, bkk))
                for fc in range(2):
                    bk, bkk = nb()
                    for dc in range(8):
                        mm(bk[:, :], wu[slot][:, dc, fc * 128:(fc + 1) * 128], x1T[:, dc, tcols], dc == 0, dc == 7,
                           ["wu%d" % slot] + xk, [bkk])
                    pu.append((bk, bkk))
                cb = cbs[si % 2]
                cbk = "cbs%d" % (si % 2)
                acp(cb[:], bcb[:], [bcbk], [cbk])
                hslot = (NXG + tt % 2) if e_ == 0 else si % NHID
                for fc in range(2):
                    u = uctr % NSS
                    uctr += 1
                    act(sbuf_s[u][:], pg[fc][0][:], AF.Silu, [pg[fc][1]], ["ss%d" % u])
                    tt_("dve", sbuf_t[u][:], pu[fc][0][:], cb[:], ALU.mult, [pu[fc][1], cbk], ["st%d" % u])
                    tt_("pool", hid[hslot][:, fc, :], sbuf_s[u][:], sbuf_t[u][:], ALU.mult, ["ss%d" % u, "st%d" % u],
                        hidk(hslot))
                pend_b[(e_, tt)] = (e_, tt, hslot, fl)
                if e_ == 0:
                    prev = (0, tt - 1) if tt >= 1 else None
                elif e_ == 1:
                    prev = (0, 3) if tt == 0 else None
                else:
                    prev = (e_ - 1, tt)
                if e_ == 1 and tt == 3:
                    prev2 = None
                if prev is not None and pend_b[prev][3] == fl:
                    pb_ = pend_b.pop(prev)
                    down(*pb_[0:3])
                    S.cond_end()
                else:
                    S.cond_end()
                    if prev is not None:
                        pb_ = pend_b.pop(prev)
                        S.cond_begin(pb_[3])
                        down(*pb_[0:3])
                        S.cond_end()
            for e_r in (1,):
                pass
            for key in sorted(pend_b.keys()):
                pb_ = pend_b.pop(key)
                S.cond_begin(pb_[3])
                down(*pb_[0:3])
                S.cond_end()
            nbanks[0] = 8
            S.barrier()
            S.emit()
        sM.close()
        if stop_after == "2":
            return nc

        with contextlib.ExitStack() as s4:
            wpg = wob
            wpp = sb(s4, "wpp", [128, 2, D], BF16)
            bc_g2 = sb(s4, "bc_g2", [128, 1024], F32)
            bc_b2 = sb(s4, "bc_b2", [128, 1024], F32)
            N3 = 3
            x1r = [sb(s4, "x1r%d" % i, [128, 1024], F32) for i in range(N3)]
            pt = [sb(s4, "pt%d" % i, [128, DPLE], F32) for i in range(N3)]
            rb = [sb(s4, "rb%d" % i, [128, 1024], BF16) for i in range(N3)]
            pb16 = [sb(s4, "pb16_%d" % i, [128, DPLE], BF16) for i in range(N3)]
            rT = [sb(s4, "rT%d" % i, [128, 8, 128], BF16) for i in range(N3)]
            pT = [sb(s4, "pT%d" % i, [128, 2, 128], BF16) for i in range(N3)]
            gt = [sb(s4, "gt%d" % i, [128, 512], F32) for i in range(4)]
            gpt = [sb(s4, "gpt%d" % i, [128, 512], F32) for i in range(4)]
            NOB = 6
            ob = [sb(s4, "ob%d" % i, [128, 1024], F32) for i in range(NOB)]
            st2 = sb(s4, "st2", [128, 16, 2, 6], F32)
            mv2 = sb(s4, "mv2", [128, 16, 2], F32)
            rs2 = sb(s4, "rs2", [128, 16], F32)
            nb2 = sb(s4, "nb2", [128, 16], F32)

            S.dma("pool", "d_wpp", wpp[:], w_pp_v, writes=["wpp"])
            S.dma("sp", None, bc_g2[:], rows[:, R_L2G:R_L2G + 1024].partition_broadcast(128), writes=["bc_g2"])
            S.dma("sp", None, bc_b2[:], rows[:, R_L2B:R_L2B + 1024].partition_broadcast(128), writes=["bc_b2"])
            hctr = [0]

            pbanks = {}

            def c_dma(g):
                k = g % N3
                S.dma_fn("pool", "d_x1r%d" % k, lambda eng: eng.indirect_dma_start(
                    out=x1r[k][:, :], out_offset=None, in_=x1_dram[:, :],
                    in_offset=bass.IndirectOffsetOnAxis(ap=inv_sb[:, g:g + 1], axis=0)), reads=[], writes=["x1r%d" % k])
                S.dma_fn("pool", "d_pt%d" % k, lambda eng: eng.indirect_dma_start(
                    out=pt[k][:, :], out_offset=None, in_=ps[:, :],
                    in_offset=bass.IndirectOffsetOnAxis(ap=inv_sb[:, g:g + 1], axis=0)), reads=[], writes=["pt%d" % k])

            def c_r(g):
                k = g % N3
                yk = ["y_%d_0" % g, "y_%d_1" % g]
                stt(yacc[:, g, :], x1r[k][:], ALPHA, yacc[:, g, :], ALU.mult, ALU.add, ["x1r%d" % k] + yk, yk)
                cp("dve", pb16[k][:], pt[k][:], ["pt%d" % k], ["pb16_%d" % k])

            def c_rb(g):
                k = g % N3
                yk = ["y_%d_0" % g, "y_%d_1" % g]
                acp(rb[k][:], yacc[:, g, :], yk, ["rb%d" % k])
                bk2, bkk2 = nb()
                bkb2 = bk2[:].bitcast(BF16)
                for kc in range(2):
                    tp(bkb2[:, kc * 128:(kc + 1) * 128], pb16[k][:, kc * 128:(kc + 1) * 128], idb[:], ["pb16_%d" % k, "idb"], [bkk2])
                pbanks[("p", g)] = (bkb2, bkk2)

            def c_tr(g):
                k = g % N3
                bkb2, bkk2 = pbanks.pop(("p", g))
                cp("dve", pT[k][:], bkb2[:, 0:256].rearrange("p (c t) -> p c t", c=2), [bkk2], ["pT%d" % k])
                bk, bkk = nb()
                bkb = bk[:].bitcast(BF16)
                for dc in range(8):
                    tp(bkb[:, dc * 128:(dc + 1) * 128], rb[k][:, dc * 128:(dc + 1) * 128], idb[:], ["rb%d" % k, "idb"], [bkk])
                pbanks[("r", g)] = (bkb, bkk)

            def c_rT(g):
                k = g % N3
                bkb, bkk = pbanks.pop(("r", g))
                acp(rT[k][:], bkb.rearrange("p (c t) -> p c t", c=8), [bkk], ["rT%d" % k])

            def c_mm(g):
                k = g % N3
                for h in range(2):
                    hs = slice(h * 512, (h + 1) * 512)
                    bg_, bgk = nb()
                    for dc in range(8):
                        mm(bg_[:, :], rT[k][:, dc, :], wpg[:, dc, hs], dc == 0, False, ["rT%d" % k, "wob"], [bgk])
                    mm(bg_[:, :], ones_rb[0:1, :], brow_b[0:1, 1024 + h * 512:1024 + (h + 1) * 512], False, True,
                       ["ones_rb", "brow"], [bgk])
                    bp_, bpk = nb()
                    for kc in range(2):
                        mm(bp_[:, :], pT[k][:, kc, :], wpp[:, kc, hs], kc == 0, kc == 1, ["pT%d" % k, "wpp"], [bpk])
                    u = hctr[0] % 4
                    hctr[0] += 1
                    act(gt[u][:], bg_[:], AF.Sigmoid, [bgk], ["gt%d" % u])
                    pbanks[("g", g, h)] = (u, bp_, bpk)

            def c_gp(g):
                for h in range(2):
                    u, bp_, bpk = pbanks.pop(("g", g, h))
                    tt_("dve", gpt[u][:], gt[u][:], bp_[:], ALU.mult, ["gt%d" % u, bpk], ["gpt%d" % u])
                    pbanks[("a", g, h)] = u

            def c_add(g):
                for h in range(2):
                    hs = slice(h * 512, (h + 1) * 512)
                    u = pbanks.pop(("a", g, h))
                    tt_("pool", yacc[:, g, hs], yacc[:, g, hs], gpt[u][:], ALU.add, ["y_%d_%d" % (g, h), "gpt%d" % u],
                        ["y_%d_%d" % (g, h)])

            def c_st(g):
                V(lambda e: e.bn_stats(st2[:, g, 0, :], yacc[:, g, 0:512]), ["y_%d_0" % g], ["st2a%d" % g])
                V(lambda e: e.bn_stats(st2[:, g, 1, :], yacc[:, g, 512:1024]), ["y_%d_1" % g], ["st2b%d" % g])
                V(lambda e: e.bn_aggr(mv2[:, g, :], st2[:, g, :, :]), ["st2a%d" % g, "st2b%d" % g], ["mv2_%d" % g])

            def c_sqrt(G):
                g0 = G * 4
                act(rs2[:, g0:g0 + 4], mv2[:, g0:g0 + 4, 1], AF.Sqrt, ["mv2_%d" % (g0 + j) for j in range(4)],
                    ["rs2a%d" % g0], bias=EPS)

            def c_rcp(G):
                g0 = G * 4
                V(lambda e: e.reciprocal(rs2[:, g0:g0 + 4], rs2[:, g0:g0 + 4]), ["rs2a%d" % g0], ["rs2_%d" % g0])
                V(lambda e: e.scalar_tensor_tensor(nb2[:, g0:g0 + 4], mv2[:, g0:g0 + 4, 0], -1.0, rs2[:, g0:g0 + 4],
                                                   ALU.mult, ALU.mult),
                  ["rs2_%d" % g0] + ["mv2_%d" % (g0 + j) for j in range(4)], ["nb2_%d" % g0])

            def c_on(g):
                k = g % NOB
                g0 = g - g % 4
                yk = ["y_%d_0" % g, "y_%d_1" % g]
                act(ob[k][:], yacc[:, g, :], AF.Identity, yk + ["nb2_%d" % g0, "rs2_%d" % g0], ["ob%d" % k],
                    bias=nb2[:, g:g + 1], scale=rs2[:, g:g + 1])

            def c_og(g):
                k = g % NOB
                tt_("pool", ob[k][:], ob[k][:], bc_g2[:], ALU.mult, ["ob%d" % k, "bc_g2"], ["ob%d" % k])

            def c_ob(g):
                k = g % NOB
                tt_("dve", ob[k][:], ob[k][:], bc_b2[:], ALU.add, ["ob%d" % k, "bc_b2"], ["ob%d" % k])
                S.dma_fn("pool", "d_out%d" % k, lambda eng: eng.indirect_dma_start(
                    out=out[:, :], out_offset=bass.IndirectOffsetOnAxis(ap=inv_sb[:, g:g + 1], axis=0),
                    in_=ob[k][:, :], in_offset=None), reads=["ob%d" % k], writes=["outd%d" % g])

            def tail(off):
                return lambda g: off - (g - 12) if g >= 12 else off

            def grp3(fn, off):
                return (off, lambda g: fn(g // 4) if g % 4 == 3 else None)

            pipeline([(0, c_dma), (1, c_r), (2, c_rb), (3, c_tr), (4, c_rT), (6, c_gp), (7, c_add), (8, c_st),
                      grp3(c_sqrt, 9), grp3(c_rcp, 10), (tail(14), c_on), (tail(15), c_og), (tail(16), c_ob), (5, c_mm)], 16)
            S.final_wait("sp", ["d_out%d" % k for k in range(NOB)])
            S.emit()
    return nc


_NC_CACHE = {}


def _prep_inputs(inp):
    f = lambda a: np.ascontiguousarray(np.asarray(a, dtype=np.float32))
    x = f(inp["x"])
    p = f(inp["p"])[0]
    w_in = f(inp["w_in"])[0]
    b_in = f(inp["b_in"])[0]
    perm = []
    for i in range(4):
        perm += list(range(i * 128, (i + 1) * 128)) + list(range(512 + i * 128, 512 + (i + 1) * 128))
    for i in range(4):
        perm += list(range(1536 + i * 128, 1536 + (i + 1) * 128))
        perm += list(range(2048 + i * 128, 2048 + (i + 1) * 128))
        perm += list(range(1024 + i * 128, 1024 + (i + 1) * 128))
    perm = np.array(perm)
    w_in_p = np.ascontiguousarray(w_in[:, perm])
    b_in_p = b_in[perm].reshape(20, 128).T
    col = lambda v, n: f(v).reshape(n, 128).T
    dww = f(inp["conf_dw_w"])[0]
    dww_p = dww.T.reshape(4, 128, 31).transpose(1, 0, 2).reshape(128, 124)
    scw = f(inp["sc_w"])[0]
    scw_p = scw.T.reshape(4, 128, 3).transpose(1, 0, 2).reshape(128, 12)
    rows = np.concatenate([f(inp["ln_in_g"]), f(inp["ln_in_b"]), f(inp["ln1_g"])[0], f(inp["ln1_b"])[0],
                           f(inp["ln2_g"])[0], f(inp["ln2_b"])[0], f(inp["b_out"])[0], f(inp["b_pg"])[0]])[None, :]
    w_r = np.ascontiguousarray(np.concatenate([f(inp["w_rg"])[0], f(inp["w_re"])[0]], axis=1))
    b_r = np.concatenate([f(inp["b_rg"])[0], f(inp["b_re"])[0]])[:, None]
    shared = {
        "rows": np.ascontiguousarray(rows), "w_in": w_in_p, "w_out": f(inp["w_out"])[0], "w_r": w_r,
        "b_r": np.ascontiguousarray(b_r), "w_gate": f(inp["w_gate"])[0], "w_up": f(inp["w_up"])[0],
        "w_down": f(inp["w_down"])[0], "w_pg": f(inp["w_pg"])[0], "w_pp": f(inp["w_pp"])[0],
        "ident": np.eye(128, dtype=np.float32),
        "tri": np.triu(np.ones((128, 128), np.float32), k=1),
        "tid": (np.arange(16, dtype=np.int32)[None, :] * 128 + np.arange(128, dtype=np.int32)[:, None]).astype(np.int32),
    }
    maps = []
    for c in range(NCORES):
        b, q = divmod(c, 4)
        lo = q * NT
        if q == 0:
            halo = np.zeros((HALO, D), np.float32)
            mask = 0.0
        else:
            halo = x[b, lo - HALO:lo]
            mask = 1.0
        xs = np.ascontiguousarray(np.concatenate([halo, x[b, lo:lo + NT]], axis=0))
        pp = np.concatenate([b_in_p, col(inp["ln_in_g"], 8), col(inp["ln_in_b"], 8), dww_p,
                             col(f(inp["conf_dw_b"])[0], 4), col(f(inp["conf_ln_g"])[0], 4), col(f(inp["conf_ln_b"])[0], 4),
                             scw_p, col(f(inp["sc_b"])[0], 4), np.full((128, 1), mask, np.float32)], axis=1)
        m = dict(shared)
        m["xs"] = xs
        m["ps"] = np.ascontiguousarray(p[b, lo:lo + NT])
        m["pp"] = np.ascontiguousarray(pp.astype(np.float32))
        maps.append(m)
    return maps


def kernel(**inputs):
    if "nc" not in _NC_CACHE:
        _NC_CACHE["nc"] = build_nc()
    nc = _NC_CACHE["nc"]
    maps = _prep_inputs(inputs)
    res = run_bass_kernel_spmd(nc, maps, core_ids=list(range(NCORES)))
    outs = [np.asarray(r["out"], dtype=np.float32) for r in res.results]
    full = np.stack(outs, axis=0).reshape(2, 4 * NT, D)
    return full
```

```python
import contextlib
import os
import numpy as np
import concourse.bass as bass
import concourse.mybir as mybir
from concourse.bass_utils import run_bass_kernel_spmd

F32 = mybir.dt.float32
I32 = mybir.dt.int32
BF16 = mybir.dt.bfloat16
AF = mybir.ActivationFunctionType
ALU = mybir.AluOpType
AX = mybir.AxisListType

NCORES = 8
NT = 2048
HALO = 32
D = 1024
DIN = 2560
NE = 16
DE = 256
DPLE = 256
XHW = 1072
EPS = 1e-5
ALPHA = 2.0 ** 0.25
BIG = 1.0e4
ENGS = ["pe", "act", "dve", "pool", "sp"]

PP_BIN = 0
PP_LNG = 20
PP_LNB = 28
PP_DWW = 36
PP_DWB = 160
PP_CLG = 164
PP_CLB = 168
PP_SCW = 172
PP_SCB = 184
PP_MASK = 188
PP_N = 189
R_LNIG, R_LNIB, R_L1G, R_L1B, R_L2G, R_L2B, R_BOUT, R_BPG = [i * 1024 for i in range(8)]


class Sched:
    def __init__(self, nc, stack):
        self.nc = nc
        self.stack = stack
        self.prog = {e: [] for e in ENGS}
        self.sem = {}
        self.cnt = {}
        self.seen = {e: {} for e in ENGS}
        self.res = {}
        self.in_cond = False
        self.regs = {}
        self.dummy = {}
        self.dummy_ctr = {}
        for e in ENGS:
            self.new_sem(e)

    def new_sem(self, key):
        if key not in self.sem:
            self.sem[key] = self.stack.enter_context(self.nc.semaphore("s_" + key))
            self.cnt[key] = 0
        return key

    def _deps(self, e, reads, writes):
        deps = {}

        def add(k, v):
            if deps.get(k, 0) < v:
                deps[k] = v

        for r in reads:
            st = self.res.get(r)
            if st:
                for k, v in st["w"].items():
                    add(k, v)
        for w in writes:
            st = self.res.get(w)
            if st:
                for k, v in st["w"].items():
                    add(k, v)
                for k, v in st["r"].items():
                    add(k, v)
        waits = []
        for k, v in deps.items():
            if k == e and e in ("pe", "sp"):
                continue
            if self.seen[e].get(k, 0) >= v:
                continue
            self.seen[e][k] = v
            waits.append((k, v))
        return waits

    def _commit(self, me, reads, writes):
        for r in reads:
            st = self.res.setdefault(r, {"w": {}, "r": {}})
            if st["r"].get(me[0], 0) < me[1]:
                st["r"][me[0]] = me[1]
        for w in writes:
            if self.in_cond:
                st = self.res.setdefault(w, {"w": {}, "r": {}})
                if st["w"].get(me[0], 0) < me[1]:
                    st["w"][me[0]] = me[1]
            else:
                self.res[w] = {"w": {me[0]: me[1]}, "r": {}}

    def op(self, e, fn, reads=(), writes=()):
        waits = self._deps(e, reads, writes)
        self.cnt[e] += 1
        me = (e, self.cnt[e])
        self.prog[e].append((waits, fn, (e, 1)))
        self._commit(me, reads, writes)
        return me

    def dma(self, q, semkey, out, in_, reads=(), writes=()):
        if semkey is None:
            self.uniq = getattr(self, "uniq", 0) + 1
            semkey = "du%d" % self.uniq
        self.new_sem(semkey)
        waits = self._deps(q, reads, writes)
        self.cnt[semkey] += 16
        me = (semkey, self.cnt[semkey])
        self.prog[q].append((waits, lambda eng: eng.dma_start(out=out, in_=in_), (semkey, 16)))
        self._commit(me, reads, writes)
        return me

    def dma_fn(self, q, semkey, fn, reads=(), writes=()):
        self.new_sem(semkey)
        waits = self._deps(q, reads, writes)
        self.cnt[semkey] += 16
        me = (semkey, self.cnt[semkey])
        self.prog[q].append((waits, fn, (semkey, 16)))
        self._commit(me, reads, writes)
        return me

    def raw(self, e, fn, reads=()):
        waits = self._deps(e, reads, ())
        self.prog[e].append((waits, fn, None))

    def cond_begin(self, flag):
        assert not self.in_cond
        self.in_cond = True
        self.seen_saved = {e: dict(d) for e, d in self.seen.items()}
        self.cond_info = {"snap": dict(self.cnt), "ext": {}}
        for e in ENGS:
            self.prog[e].append(("cond_begin", flag, self.cond_info))

    def cond_end(self):
        self.in_cond = False
        self.seen = self.seen_saved
        info = self.cond_info
        snap = info["snap"]
        for e in ENGS:
            prog = self.prog[e]
            i = len(prog) - 1
            while prog[i][0] != "cond_begin":
                for k, v in prog[i][0]:
                    if v <= snap.get(k, 0) and info["ext"].get(k, 0) < v:
                        info["ext"][k] = v
                i -= 1
            self.prog[e].append(("cond_end", None, None))

    def barrier(self):
        for e in ENGS:
            waits = []
            for k, v in self.cnt.items():
                if v == 0 or k == e:
                    continue
                if self.seen[e].get(k, 0) >= v:
                    continue
                self.seen[e][k] = v
                waits.append((k, v))
            self.prog[e].append((waits, None, None))
        self.res = {}

    def final_wait(self, e, semkeys):
        waits = [(k, self.cnt[k]) for k in semkeys if self.cnt.get(k, 0) > 0]
        self.prog[e].append((waits, None, None))

    def emit(self):
        nc = self.nc
        with nc.Block() as block:
            def replay(e, eng):
                prog = self.prog[e]
                i = 0
                while i < len(prog):
                    waits, fn, inc = prog[i]
                    if waits == "cond_begin":
                        j = i + 1
                        while prog[j][0] != "cond_end":
                            j += 1
                        body = prog[i + 1:j]
                        n_inc = sum(1 for b in body if b[2] is not None and b[2][0] == e)
                        if body:
                            cm = eng.If(((self.regs[e] >> fn) & 1) != 0)
                            cm.__enter__()
                            for w_, f_, inc_ in body:
                                for k, v in w_:
                                    eng.wait_ge(self.sem[k], v)
                                if f_ is not None:
                                    ins = f_(eng)
                                    if inc_ is not None:
                                        ins.then_inc(self.sem[inc_[0]], inc_[1])
                            cm.__exit__(None, None, None)
                            if n_inc > 0:
                                info = inc
                                cm2 = eng.Else()
                                cm2.__enter__()
                                self.dummy_ctr[e] = self.dummy_ctr.get(e, 0) + 1
                                self.dummy[e](eng, self.dummy_ctr[e]).then_inc(self.sem[e], n_inc)
                                cm2.__exit__(None, None, None)
                        i = j + 1
                        continue
                    for k, v in waits:
                        eng.wait_ge(self.sem[k], v)
                    if fn is not None:
                        ins = fn(eng)
                        if inc is not None:
                            ins.then_inc(self.sem[inc[0]], inc[1])
                    i += 1
                self.prog[e] = []

            @block.tensor
            def _(eng):
                replay("pe", eng)

            @block.scalar
            def _(eng):
                replay("act", eng)

            @block.vector
            def _(eng):
                replay("dve", eng)

            @block.gpsimd
            def _(eng):
                replay("pool", eng)

            @block.sync
            def _(eng):
                replay("sp", eng)


def build_nc(stop_after=None):
    nc = bass.Bass("TRN2", target_bir_lowering=False)
    dt_in = lambda n, s: nc.dram_tensor(n, s, F32, kind="ExternalInput").ap()
    xs = dt_in("xs", [NT + HALO, D])
    ps = dt_in("ps", [NT, DPLE])
    pp = dt_in("pp", [128, PP_N])
    rows = dt_in("rows", [1, 8192])
    w_in = dt_in("w_in", [D, DIN])
    w_out = dt_in("w_out", [D, D])
    w_r = dt_in("w_r", [D, 20])
    b_r = dt_in("b_r", [20, 1])
    w_gate = dt_in("w_gate", [NE, D, DE])
    w_up = dt_in("w_up", [NE, D, DE])
    w_down = dt_in("w_down", [NE, DE, D])
    w_pg = dt_in("w_pg", [D, D])
    w_pp = dt_in("w_pp", [DPLE, D])
    ident = dt_in("ident", [128, 128])
    tri = dt_in("tri", [128, 128])
    tid = nc.dram_tensor("tid", [128, 16], I32, kind="ExternalInput").ap()
    out = nc.dram_tensor("out", [NT, D], F32, kind="ExternalOutput").ap()
    x1_dram = nc.dram_tensor("x1_spill", [NT, D], F32).ap()
    xh_dram = nc.dram_tensor("xh_spill", [NT, XHW], BF16).ap()
    inv_dram = nc.dram_tensor("inv_spill", [NT, 1], I32).ap()
    flags_dram = nc.dram_tensor("flags_spill", [1, 16], I32).ap()
    dbg = nc.dram_tensor("dbg", [128, 64], F32, kind="ExternalOutput").ap() if os.environ.get("K_DBG") else None

    w_in_v = w_in.rearrange("(dc p) f -> p dc f", p=128)
    w_out_v = w_out.rearrange("(dc p) f -> p dc f", p=128)
    w_pg_v = w_pg.rearrange("(dc p) f -> p dc f", p=128)
    w_pp_v = w_pp.rearrange("(dc p) f -> p dc f", p=128)
    w_r_v = w_r.rearrange("(dc p) f -> p dc f", p=128)

    with contextlib.ExitStack() as st:
        S = Sched(nc, st)

        def sb(stack, n, s, d):
            return stack.enter_context(nc.sbuf_tensor(n, s, d))

        banks = [st.enter_context(nc.psum_tensor("pb%d" % i, [128, 512], F32)) for i in range(8)]
        bank_ctr = [0]

        nbanks = [8]

        def nb():
            i = bank_ctr[0] % nbanks[0]
            bank_ctr[0] += 1
            return banks[i], "pb%d" % i

        def mm(o, l, r, start, stop, reads, writes):
            S.op("pe", lambda e: e.matmul(o, l, r, start=start, stop=stop), reads, writes)

        def tp(o, i_, idn, reads, writes):
            S.op("pe", lambda e: e.transpose(o, i_, idn), reads, writes)

        def act(o, i_, f, reads, writes, bias=0.0, scale=1.0):
            S.op("act", lambda e: e.activation(o, i_, f, bias=bias, scale=scale), reads, writes)

        def acp(o, i_, reads, writes):
            S.op("act", lambda e: e.copy(o, i_), reads, writes)

        def tt_(eng, o, a, b, op, reads, writes):
            S.op(eng, lambda e: e.tensor_tensor(o, a, b, op), reads, writes)

        def ts_(o, a, s1, s2, op0, op1, reads, writes):
            if s2 is None:
                S.op("dve", lambda e: e.tensor_scalar(o, a, s1, None, op0), reads, writes)
            else:
                S.op("dve", lambda e: e.tensor_scalar(o, a, s1, s2, op0, op1), reads, writes)

        def stt(o, a, sc, b, op0, op1, reads, writes):
            S.op("dve", lambda e: e.scalar_tensor_tensor(o, a, sc, b, op0, op1), reads, writes)

        def cp(eng, o, i_, reads, writes):
            S.op(eng, lambda e: e.tensor_copy(o, i_), reads, writes)

        def V(f, r, w):
            S.op("dve", f, r, w)

        def pipeline(stages, n):
            offs = [(off if callable(off) else (lambda i, off=off: off), fn) for off, fn in stages]
            last = max(i + off(i) for off, _ in offs for i in range(n))
            for t in range(last + 1):
                for off, fn in offs:
                    for i in range(n):
                        if i + off(i) == t:
                            fn(i)

        idf = sb(st, "idf", [128, 128], F32)
        idb = sb(st, "idb", [128, 128], BF16)
        ones_f = sb(st, "ones_f", [128, 128], F32)
        ones_rb = sb(st, "ones_rb", [1, 128], BF16)
        pps = sb(st, "pps", [128, PP_N], F32)
        brow_b = sb(st, "brow_b", [1, 2048], BF16)
        mv_in = sb(st, "mv_in", [128, 17, 2], F32)
        rs_in = sb(st, "rs_in", [128, 17], F32)
        ohg_all = sb(st, "ohg_all", [128, 16, 4], F32)
        inv_sb = sb(st, "inv_sb", [128, 16], I32)
        big = sb(st, "big", [128, 16384], F32)
        x1T = sb(st, "x1T", [128, 8, NT], BF16)
        wob = sb(st, "wob", [128, 8, 1024], BF16)

        def ppc(c, n=1):
            return pps[:, c:c + n]

        S.dma("sp", None, idf[:], ident, writes=["idf"])
        S.dma("sp", None, pps[:], pp, writes=["pps"])
        S.dma("pool", None, brow_b[:], rows[:, R_BOUT:R_BOUT + 2048], writes=["brow"])
        cp("dve", idb[:], idf[:], ["idf"], ["idb"])
        S.op("dve", lambda e: e.memset(ones_f[:], 1.0), (), ["ones_f"])
        S.op("dve", lambda e: e.memset(ones_rb[:], 1.0), (), ["ones_rb"])

        catT = big[:, 0:8192].bitcast(BF16).rearrange("p (c t) -> p c t", c=8)

        with contextlib.ExitStack() as s1:
            diag = x1T[:, :, :].rearrange("p c t -> p (c t)")[:, 0:4 * 31 * 128].rearrange(
                "p (c k j) -> p c k j", c=4, k=31)
            NWS = 3
            wslab = [sb(s1, "wslab%d" % i, [128, 8, 384], BF16) for i in range(NWS)]
            xcb = sb(s1, "xcb", [128, 4, 1024], BF16)
            xnT = [sb(s1, "xnT%d" % i, [128, 8, 512], BF16) for i in range(2)]
            xnTh = sb(s1, "xnTh", [128, 8, 32], BF16)
            xth = xcb[0:32, 0:2, :].rearrange("p a b -> p (a b)").bitcast(F32)
            xcbh = xcb[0:32, 2, :]
            sth = sb(s1, "sth", [128, 5, 2, 6], F32)
            glu = sb(s1, "glu", [128, 4, 544], BF16)
            cv = sb(s1, "cv", [128, 4, 514], F32)
            NTMP = 8
            tmp = [sb(s1, "tmp%d" % i, [128, 512], F32) for i in range(NTMP)]
            htmp = [sb(s1, "htmp%d" % i, [128, 32], F32) for i in range(4)]
            c_mean = sb(s1, "c_mean", [128, 512], F32)
            c_var = sb(s1, "c_var", [128, 512], F32)
            xt4 = big[:, 8192:12288].rearrange("p (j d) -> p j d", j=4)
            confy = big[:, 12288:14336].rearrange("p (c t) -> p c t", c=4)
            sq = big[:, 14336:16384].rearrange("p (c t) -> p c t", c=4)
            tctr = [0]

            def ntmp():
                i = tctr[0] % NTMP
                tctr[0] += 1
                return tmp[i], "tmp%d" % i

            def maybe_stop(tag):
                if stop_after == tag:
                    S.barrier()
                    S.emit()
                    return True
                return False

            def build_diag(c):
                S.op("dve", lambda e: e.tensor_tensor(
                    diag[:, c, :, :], idf[:].unsqueeze(1).to_broadcast([128, 31, 128]),
                    pps[:, PP_DWW + c * 31:PP_DWW + (c + 1) * 31].unsqueeze(2).to_broadcast([128, 31, 128]),
                    ALU.mult), ["idf", "pps"], ["diag%d" % c])

            S.dma("sp", "d_xh", xth, xs[0:HALO, :], writes=["xcb0", "xcb1"])
            V(lambda e: e.bn_stats(sth[0:32, 4, 0, :], xth[:, 0:512]), ["xcb0", "xcb1"], ["sth4a"])
            V(lambda e: e.bn_stats(sth[0:32, 4, 1, :], xth[:, 512:1024]), ["xcb0", "xcb1"], ["sth4b"])
            V(lambda e: e.bn_aggr(mv_in[0:32, 16, :], sth[0:32, 4, :, :]), ["sth4a", "sth4b"], ["mvh"])
            act(rs_in[0:32, 16:17], mv_in[0:32, 16, 1:2], AF.Sqrt, ["mvh"], ["rsh0"], bias=EPS)
            V(lambda e: e.reciprocal(rs_in[0:32, 16:17], rs_in[0:32, 16:17]), ["rsh0"], ["rsh"])
            ts_(xcbh, xth, mv_in[0:32, 16, 0:1], rs_in[0:32, 16:17], ALU.subtract, ALU.mult,
                ["xcb0", "xcb1", "mvh", "rsh"], ["xcb2"])
            bk, bkk = nb()
            bkb = bk[:].bitcast(BF16)
            for dc in range(8):
                tp(bkb[:, dc * 32:(dc + 1) * 32], xcbh[:, dc * 128:(dc + 1) * 128], idb[0:32, 0:32],
                   ["xcb2", "idb"], [bkk])
            for dc in range(8):
                act(xnTh[:, dc, :], bkb[:, dc * 32:(dc + 1) * 32], AF.Identity, [bkk, "pps"], ["xnTh"],
                    bias=ppc(PP_LNB + dc), scale=ppc(PP_LNG + dc))

            def stepA_load(tt, j):
                g = tt * 4 + j
                S.dma("sp", "d_xt%d" % j, xt4[:, j, :], xs[HALO + g * 128:HALO + (g + 1) * 128, :],
                      writes=["xt%d" % j])
                V(lambda e: e.bn_stats(sth[:, j, 0, :], xt4[:, j, 0:512]), ["xt%d" % j], ["sth%da" % j])
                V(lambda e: e.bn_stats(sth[:, j, 1, :], xt4[:, j, 512:1024]), ["xt%d" % j], ["sth%db" % j])
                V(lambda e: e.bn_aggr(mv_in[:, g, :], sth[:, j, :, :]), ["sth%da" % j, "sth%db" % j], ["mv%d" % g])

            def stepA_norm(tt):
                g0 = tt * 4
                act(rs_in[:, g0:g0 + 4], mv_in[:, g0:g0 + 4, 1], AF.Sqrt, ["mv%d" % (g0 + j) for j in range(4)],
                    ["rs0_%d" % tt], bias=EPS)
                V(lambda e: e.reciprocal(rs_in[:, g0:g0 + 4], rs_in[:, g0:g0 + 4]), ["rs0_%d" % tt], ["rs_%d" % tt])
                for j in range(4):
                    g = g0 + j
                    ts_(xcb[:, j, :], xt4[:, j, :], mv_in[:, g, 0:1], rs_in[:, g:g + 1], ALU.subtract, ALU.mult,
                        ["xt%d" % j, "mv%d" % g, "rs_%d" % tt], ["xcb%d" % j])

            def stepA_tr(tt, dcs):
                xn = xnT[tt % 2]
                xnk = "xnT%d" % (tt % 2)
                for dc in dcs:
                    bk, bkk = nb()
                    bkb = bk[:].bitcast(BF16)
                    for j in range(4):
                        tp(bkb[:, j * 128:(j + 1) * 128], xcb[:, j, dc * 128:(dc + 1) * 128], idb[:],
                           ["xcb%d" % j, "idb"], [bkk])
                    act(xn[:, dc, :], bkb[:, 0:512], AF.Identity, [bkk, "pps"], [xnk + "_%d" % dc],
                        bias=ppc(PP_LNB + dc), scale=ppc(PP_LNG + dc))

            def load_slab(s):
                if s >= 32:
                    return
                slot = s % NWS
                if s % 8 < 4:
                    c0, ncol = (s % 8) * 256, 256
                else:
                    c0, ncol = 1024 + (s % 8 - 4) * 384, 384
                S.dma("pool", "d_ws%d" % slot, wslab[slot][:, :, 0:ncol], w_in_v[:, :, c0:c0 + ncol],
                      writes=["ws%d" % slot])

            def conv_job(tt, i):
                bc_, bck = nb()
                for k in range(31):
                    mm(bc_[:, :], diag[:, i, k, :], glu[:, i, k + 2:k + 2 + 512], k == 0, k == 30,
                       ["diag%d" % i, "glu%d" % i], [bck])
                act(confy[:, i, :], bc_[:], AF.Identity, [bck, "pps"], ["confy%d" % i], bias=ppc(PP_DWB + i))
                act(sq[:, i, :], bc_[:], AF.Square, [bck, "pps"], ["sq%d" % i], bias=ppc(PP_DWB + i))
                if tt < 3:
                    cp("pool", glu[:, i, 0:32], glu[:, i, 512:544], ["glu%d" % i], ["glu%d" % i])

            def slab(tt, sl):
                s = tt * 8 + sl
                slot = s % NWS
                ws = wslab[slot]
                wsk = "ws%d" % slot
                xn = xnT[tt % 2]
                xn_reads = ["xnT%d_%d" % (tt % 2, dc) for dc in range(8)]
                tcols = slice(tt * 512, (tt + 1) * 512)

                def proj(coff):
                    bk_, bkk_ = nb()
                    for dc in range(8):
                        mm(bk_[:, :], ws[:, dc, coff:coff + 128], xn[:, dc, :], dc == 0, dc == 7,
                           [wsk] + xn_reads, [bkk_])
                    return bk_, bkk_

                def proj_h(coff, bank, bcol):
                    bk_, bkk_ = bank
                    for dc in range(8):
                        mm(bk_[:, bcol:bcol + 32], ws[:, dc, coff:coff + 128], xnTh[:, dc, :],
                           dc == 0, dc == 7, [wsk, "xnTh"], [bkk_])

                if sl < 4:
                    i = sl
                    bv, bvk = proj(0)
                    bg, bgk = proj(128)
                    if tt == 0:
                        hb = nb()
                        proj_h(0, hb, 0)
                        proj_h(128, hb, 32)
                    sig, sigk = ntmp()
                    act(sig[:], bg[:], AF.Sigmoid, [bgk, "pps"], [sigk], bias=ppc(PP_BIN + 2 * i + 1))
                    stt(glu[:, i, 32:544], bv[:], ppc(PP_BIN + 2 * i), sig[:], ALU.add, ALU.mult,
                        [bvk, sigk, "pps"], ["glu%d" % i])
                    if tt == 0:
                        act(htmp[0][:], hb[0][:, 32:64], AF.Sigmoid, [hb[1], "pps"], ["ht0"],
                            bias=ppc(PP_BIN + 2 * i + 1))
                        stt(htmp[1][:], hb[0][:, 0:32], ppc(PP_BIN + 2 * i), htmp[0][:], ALU.add, ALU.mult,
                            [hb[1], "ht0", "pps"], ["ht1"])
                        ts_(glu[:, i, 0:32], htmp[1][:], ppc(PP_MASK), None, ALU.mult, ALU.bypass,
                            ["ht1", "pps"], ["glu%d" % i])
                else:
                    i = sl - 4
                    cb_ = PP_BIN + 8 + 3 * i
                    bC, bCk = proj(0)
                    bV, bVk = proj(128)
                    bB, bBk = proj(256)
                    if tt == 0:
                        hb = nb()
                        proj_h(0, hb, 0)
                        proj_h(128, hb, 32)
                    vv, vvk = ntmp()
                    act(vv[:], bV[:], AF.Identity, [bVk, "pps"], [vvk], bias=ppc(cb_ + 1))
                    stt(cv[:, i, 2:514], bC[:], ppc(cb_), vv[:], ALU.add, ALU.mult, [bCk, vvk, "pps"], ["cv%d" % i])
                    if tt == 0:
                        act(htmp[2][:], hb[0][:, 32:64], AF.Identity, [hb[1], "pps"], ["ht2"], bias=ppc(cb_ + 1))
                        stt(htmp[3][:], hb[0][:, 0:32], ppc(cb_), htmp[2][:], ALU.add, ALU.mult,
                            [hb[1], "ht2", "pps"], ["ht3"])
                        ts_(cv[:, i, 0:2], htmp[3][:, 30:32], ppc(PP_MASK), None, ALU.mult, ALU.bypass,
                            ["ht3", "pps"], ["cv%d" % i])
                    tc, tck = ntmp()
                    w0 = PP_SCW + i * 3
                    ts_(tc[:], cv[:, i, 2:514], ppc(w0 + 2), ppc(PP_SCB + i), ALU.mult, ALU.add,
                        ["cv%d" % i, "pps"], [tck])
                    stt(tc[:], cv[:, i, 1:513], ppc(w0 + 1), tc[:], ALU.mult, ALU.add, ["cv%d" % i, "pps", tck], [tck])
                    stt(tc[:], cv[:, i, 0:512], ppc(w0), tc[:], ALU.mult, ALU.add, ["cv%d" % i, "pps", tck], [tck])
                    stt(catT[:, 4 + i, tcols], bB[:], ppc(cb_ + 2), tc[:], ALU.add, ALU.mult,
                        [bBk, tck, "pps"], ["cat%d_%d" % (4 + i, tt)])
                    if tt < 3:
                        cp("pool", cv[:, i, 0:2], cv[:, i, 512:514], ["cv%d" % i], ["cv%d" % i])

            cst = {}

            def stepC1(tt):
                b1_, b1k = nb()
                b2_, b2k = nb()
                for i in range(4):
                    mm(b1_[:, :], ones_f[:], confy[:, i, :], i == 0, i == 3, ["ones_f", "confy%d" % i], [b1k])
                for i in range(4):
                    mm(b2_[:, :], ones_f[:], sq[:, i, :], i == 0, i == 3, ["ones_f", "sq%d" % i], [b2k])
                mean, meank = c_mean, "c_mean"
                m2, m2k = ntmp()
                var, vark = c_var, "c_var"
                ts_(mean[:], b1_[:], 1.0 / 512, None, ALU.mult, ALU.bypass, [b1k], [meank])
                tt_("pool", m2[:], mean[:], mean[:], ALU.mult, [meank], [m2k])
                stt(var[:], b2_[:], 1.0 / 512, m2[:], ALU.mult, ALU.subtract, [b2k, m2k], [vark])
                act(var[:], var[:], AF.Sqrt, [vark], [vark], bias=EPS)
                V(lambda e: e.reciprocal(var[:], var[:]), [vark], [vark])
                cst[tt] = (mean, meank, var, vark, [(confy[:, i, :], "confy%d" % i) for i in range(4)],
                           [(sq[:, i, :], "sq%d" % i) for i in range(4)])

            def stepC2(tt):
                mean, meank, var, vark, tas, sgs = cst[tt]
                for i in range(4):
                    ta, tak = tas[i]
                    tt_("pool", ta, confy[:, i, :], mean[:], ALU.subtract, ["confy%d" % i, meank], [tak])
                for i in range(4):
                    ta, tak = tas[i]
                    tt_("dve", ta, ta, var[:], ALU.mult, [tak, vark], [tak])

            def stepC3(tt):
                tcols = slice(tt * 512, (tt + 1) * 512)
                mean, meank, var, vark, tas, sgs = cst[tt]
                for i in range(4):
                    ta, tak = tas[i]
                    sg, sgk = sgs[i]
                    act(sg, ta, AF.Sigmoid, [tak, "pps"], [sgk], bias=ppc(PP_CLB + i), scale=ppc(PP_CLG + i))
                for i in range(4):
                    ta, tak = tas[i]
                    sg, sgk = sgs[i]
                    ts_(ta, ta, ppc(PP_CLG + i), ppc(PP_CLB + i), ALU.mult, ALU.add, [tak, "pps", sgk], [tak])
                for i in range(4):
                    ta, tak = tas[i]
                    sg, sgk = sgs[i]
                    tt_("pool", catT[:, i, tcols], ta, sg, ALU.mult, [tak, sgk], ["cat%d_%d" % (i, tt)])

            load_slab(0)
            load_slab(1)
            for j in range(4):
                stepA_load(0, j)
            stepA_norm(0)
            stepA_tr(0, range(8))
            for tt in range(4):
                pend = None
                for sl in range(8):
                    load_slab(tt * 8 + sl + 2)
                    if tt == 0 and sl < 4:
                        build_diag(sl)
                    slab(tt, sl)
                    if pend is not None:
                        conv_job(*pend)
                    pend = (tt, sl) if sl < 4 else None
                    if tt == 1 and sl == 0:
                        S.dma("pool", "d_wob", wob[:], w_out_v, writes=["wob"])
                    if tt < 3:
                        if sl < 4:
                            stepA_load(tt + 1, sl)
                        elif sl == 4:
                            stepA_norm(tt + 1)
                        elif sl == 6:
                            stepA_tr(tt + 1, range(0, 4))
                        elif sl == 7:
                            stepA_tr(tt + 1, range(4, 8))
                    if sl == 5:
                        stepC1(tt)
                    elif sl == 6:
                        stepC2(tt)
                    elif sl == 7:
                        stepC3(tt)
            S.barrier()
            S.emit()
        if stop_after == "1a":
            return nc

        combT = sb(st, "combT", [48, NT], BF16)
        sM = st.enter_context(contextlib.ExitStack())
        NSLOT = 3
        wg = [None] * NSLOT
        wu = [None] * NSLOT
        NWD = 4
        wd = [None] * NWD
        wg[0] = sb(sM, "wg0", [128, 8, DE], BF16)
        wu[0] = sb(sM, "wu0", [128, 8, DE], BF16)
        wd[0] = sb(sM, "wd0", [128, 2, D], BF16)

        def load_expert(e_):
            slot = e_ % NSLOT
            S.dma("pool", "d_mwg%d" % slot, wg[slot][:], w_gate[e_].rearrange("(dc p) f -> p dc f", p=128), writes=["wg%d" % slot])
            S.dma("pool", "d_mwu%d" % slot, wu[slot][:], w_up[e_].rearrange("(dc p) f -> p dc f", p=128), writes=["wu%d" % slot])
            S.dma("pool", "d_mwd%d" % (e_ % NWD), wd[e_ % NWD][:], w_down[e_].rearrange("(fc p) d -> p fc d", p=128),
                  writes=["wd%d" % (e_ % NWD)])

        with contextlib.ExitStack() as s2:
            bc_ag = sb(s2, "bc_ag", [128, 1024], F32)
            bc_g1 = sb(s2, "bc_g1", [128, 1024], F32)
            bc_b1 = sb(s2, "bc_b1", [128, 1024], F32)
            NH = 3
            hib = [sb(s2, "hib%d" % i, [128, 1024], BF16) for i in range(NH)]
            lob = [sb(s2, "lob%d" % i, [128, 1024], BF16) for i in range(NH)]
            x1loT = [sb(s2, "x1loT%d" % i, [128, 8, 512], BF16) for i in range(2)]
            r1x = sb(s2, "r1x", [128, 4, 1024], F32)
            wr_f = sb(s2, "wr_f", [128, 8, 20], F32)
            wr_hi = sb(s2, "wr_hi", [128, 8, 20], BF16)
            wr_lo = sb(s2, "wr_lo", [128, 8, 20], BF16)
            br_sb = sb(s2, "br_sb", [20, 1], F32)
            lgT = sb(s2, "lgT", [20, 512], F32)
            lg = sb(s2, "lg", [128, 4, 20], F32)
            chl = sb(s2, "chl", [128, 4, 48], BF16)
            osum = sb(s2, "osum", [128, 4, 1], F32)
            onm = sb(s2, "onm", [128, 4, 1], F32)
            tri_sb = sb(s2, "tri_sb", [128, 128], F32)
            tid_sb = sb(s2, "tid_sb", [128, 16], I32)
            Wt = sb(s2, "Wt", [128, 16, 4], F32)
            Tt = sb(s2, "Tt", [128, 16, 4], F32)
            Et = sb(s2, "Et", [128, 16, 4], F32)
            cntv = sb(s2, "cntv", [128, 4], F32)
            startv = sb(s2, "startv", [128, 4], F32)
            hiv = sb(s2, "hiv", [128, 4], F32)
            f1 = sb(s2, "f1", [128, 4], F32)
            f2 = sb(s2, "f2", [128, 4], F32)
            posf = sb(s2, "posf", [128, 16], F32)
            posi = sb(s2, "posi", [128, 16], I32)
            flagsf = sb(s2, "flagsf", [128, 4, 4], F32)
            flagsi = sb(s2, "flagsi", [128, 16], I32)
            bitsf = sb(s2, "bitsf", [128, 1], F32)
            st1 = sb(s2, "st1", [128, 16, 2, 6], F32)
            mv1 = sb(s2, "mv1", [128, 16, 2], F32)
            rs1 = sb(s2, "rs1", [128, 16], F32)
            nb1 = sb(s2, "nb1", [128, 16], F32)
            nb_in = sb(s2, "nb_in", [128, 16], F32)
            brow_hl = sb(s2, "brow_hl", [64, 1024], BF16)
            ones64 = sb(s2, "ones64", [64, 128], BF16)
            r_sm = [sb(s2, "rsm%d" % i, [128, 4, 16], F32) for i in range(6)]
            r_s4 = [sb(s2, "rs4_%d" % i, [128, 4, 4], F32) for i in range(4)]
            r_s1 = [sb(s2, "rs1_%d" % i, [128, 4], F32) for i in range(8)]
            r1b = big[:, 8192:12288].rearrange("p (j d) -> p j d", j=4)
            xrb = big[:, 12288:16384].rearrange("p (j d) -> p j d", j=4)

            def r1v(g):
                s_ = g % 8
                return (r1b[:, s_, :] if s_ < 4 else r1x[:, s_ - 4, :]), "r1s%d" % s_

            S.dma("sp", None, bc_ag[:], rows[:, R_LNIG:R_LNIG + 1024].partition_broadcast(128), writes=["bc_ag"])
            S.dma("sp", None, bc_g1[:], rows[:, R_L1G:R_L1G + 1024].partition_broadcast(128), writes=["bc_g1"])
            S.dma("sp", None, bc_b1[:], rows[:, R_L1B:R_L1B + 1024].partition_broadcast(128), writes=["bc_b1"])
            S.dma("sp", None, wr_f[:], w_r_v, writes=["wr_f"])
            S.dma("sp", None, br_sb[:], b_r, writes=["br_sb"])
            ts_(bc_ag[:], bc_ag[:], ALPHA, None, ALU.mult, ALU.bypass, ["bc_ag"], ["bc_ag"])
            cp("dve", wr_hi[:], wr_f[:], ["wr_f"], ["wr_hi"])
            tt_("dve", wr_lo[:], wr_f[:], wr_hi[:], ALU.subtract, ["wr_f", "wr_hi"], ["wr_lo"])
            load_expert(0)
            V(lambda e: e.memset(chl[:], 0.0), [], ["chl_z"])
            V(lambda e: e.scalar_tensor_tensor(nb_in[:], mv_in[:, 0:16, 0], -1.0, rs_in[:, 0:16], ALU.mult, ALU.mult),
              [], ["nb_in"])

            mixb = {}
            V(lambda e: e.memset(brow_hl[:], 0.0), [], ["brow_hl"])
            V(lambda e: e.memset(ones64[:], 1.0), [], ["ones64"])
            for pb_ in (0, 32):
                f_ = xrb[pb_:pb_ + 1, 2, :]
                f2_ = xrb[pb_:pb_ + 1, 3, :]
                S.dma("sp", None, f_, rows[:, R_BOUT:R_BOUT + 1024], writes=["xr2"])
                S.dma("sp", None, f2_, rows[:, R_LNIB:R_LNIB + 1024], writes=["xr3"])
                stt(f_, f2_, ALPHA, f_, ALU.mult, ALU.add, ["xr2", "xr3"], ["xr2"])
                cp("dve", brow_hl[pb_:pb_ + 1, :], f_, ["xr2", "brow_hl"], ["brow_hl"])
                if pb_ == 32:
                    tt_("dve", brow_hl[32:33, :], f_, brow_hl[32:33, :], ALU.subtract, ["xr2", "brow_hl"], ["brow_hl"])

            def b_dma(g):
                S.dma("sp", "d_xr%d" % (g % 4), xrb[:, g % 4, :], xs[HALO + g * 128:HALO + (g + 1) * 128, :],
                      writes=["xr%d" % (g % 4)])

            def b_xn(g):
                xk = "xr%d" % (g % 4)
                xr = xrb[:, g % 4, :]
                act(xr, xr, AF.Identity, [xk, "nb_in"], [xk], bias=nb_in[:, g:g + 1], scale=rs_in[:, g:g + 1])

            def b_mix(g):
                tt, j = divmod(g, 4)
                gc = slice(g * 128, (g + 1) * 128)
                xk = "xr%d" % (g % 4)
                xr = xrb[:, g % 4, :]
                tt_("pool", xr, xr, bc_ag[:], ALU.mult, [xk, "bc_ag"], [xk])
                pbk = []
                for h in range(2):
                    hs = slice(h * 512, (h + 1) * 512)
                    bk, bkk = nb()
                    for c in range(8):
                        mm(bk[:, :], catT[:, c, gc], wob[:, c, hs], c == 0, False, ["cat%d_%d" % (c, tt), "wob"], [bkk])
                    mm(bk[:, :], ones64[:, :], brow_hl[:, hs], False, True, ["ones64", "brow_hl"], [bkk])
                    pbk.append((bk, bkk))
                mixb[g] = pbk

            def b_r1(g):
                xk = "xr%d" % (g % 4)
                xr = xrb[:, g % 4, :]
                r1, r1k = r1v(g)
                pbk = mixb.pop(g)
                for h in range(2):
                    hs = slice(h * 512, (h + 1) * 512)
                    tt_("dve", r1[:, hs], xr[:, hs], pbk[h][0][:], ALU.add, [xk, pbk[h][1]], [r1k + "_%d" % h])
                V(lambda e: e.bn_stats(st1[:, g, 0, :], r1[:, 0:512]), [r1k + "_0"], ["st1a%d" % g])
                V(lambda e: e.bn_stats(st1[:, g, 1, :], r1[:, 512:1024]), [r1k + "_1"], ["st1b%d" % g])
                V(lambda e: e.bn_aggr(mv1[:, g, :], st1[:, g, :, :]), ["st1a%d" % g, "st1b%d" % g], ["mv1_%d" % g])

            def b_sqrt(g):
                act(rs1[:, g:g + 1], mv1[:, g, 1:2], AF.Sqrt, ["mv1_%d" % g], ["rs1a%d" % g], bias=EPS)

            def b_rcp(g):
                V(lambda e: e.reciprocal(rs1[:, g:g + 1], rs1[:, g:g + 1]), ["rs1a%d" % g], ["rs1_%d" % g])
                V(lambda e: e.scalar_tensor_tensor(nb1[:, g:g + 1], mv1[:, g, 0:1], -1.0, rs1[:, g:g + 1],
                                                   ALU.mult, ALU.mult), ["rs1_%d" % g, "mv1_%d" % g], ["nb1_%d" % g])

            def b_n1(g):
                r1, r1k = r1v(g)
                rk = [r1k + "_0", r1k + "_1"]
                act(r1, r1, AF.Identity, rk + ["nb1_%d" % g, "rs1_%d" % g], rk, bias=nb1[:, g:g + 1], scale=rs1[:, g:g + 1])

            def b_g1(g):
                r1, r1k = r1v(g)
                rk = [r1k + "_0", r1k + "_1"]
                tt_("pool", r1, r1, bc_g1[:], ALU.mult, rk + ["bc_g1"], rk)

            def b_b1(g):
                r1, r1k = r1v(g)
                rk = [r1k + "_0", r1k + "_1"]
                tt_("dve", r1, r1, bc_b1[:], ALU.add, rk + ["bc_b1"], rk)
                S.dma("sp", "d_x1s%d" % (g % 8), x1_dram[g * 128:(g + 1) * 128, :], r1, reads=rk, writes=["x1d%d" % g])

            def b_hi(g):
                r1, r1k = r1v(g)
                rk = [r1k + "_0", r1k + "_1"]
                acp(hib[g % NH][:], r1, rk, ["hib%d" % (g % NH)])
                S.dma("sp", "d_xh%d" % (g % NH), xh_dram[g * 128:(g + 1) * 128, 0:1024], hib[g % NH][:],
                      reads=["hib%d" % (g % NH)], writes=["xhd%d" % g])

            def b_lo(g):
                r1, r1k = r1v(g)
                rk = [r1k + "_0", r1k + "_1"]
                hb_, lb_ = hib[g % NH], lob[g % NH]
                hk, lk = "hib%d" % (g % NH), "lob%d" % (g % NH)
                tt_("dve", lb_[:], r1, hb_[:], ALU.subtract, rk + [hk], [lk])
                bk, bkk = nb()
                bkb = bk[:].bitcast(BF16)
                for dc in range(8):
                    tp(bkb[:, dc * 128:(dc + 1) * 128], hb_[:, dc * 128:(dc + 1) * 128], idb[:], [hk, "idb"], [bkk])
                mixb[("h", g)] = (bkb, bkk)

            def b_x1T(g):
                gc = slice(g * 128, (g + 1) * 128)
                lb_ = lob[g % NH]
                lk = "lob%d" % (g % NH)
                bkb, bkk = mixb.pop(("h", g))
                acp(x1T[:, :, gc], bkb.rearrange("p (c t) -> p c t", c=8), [bkk], ["x1T_%d" % g])
                bk2, bkk2 = nb()
                bkb2 = bk2[:].bitcast(BF16)
                for dc in range(8):
                    tp(bkb2[:, dc * 128:(dc + 1) * 128], lb_[:, dc * 128:(dc + 1) * 128], idb[:], [lk, "idb"], [bkk2])
                mixb[("l", g)] = (bkb2, bkk2)

            def b_x1lo(g):
                tt, j = divmod(g, 4)
                bkb2, bkk2 = mixb.pop(("l", g))
                acp(x1loT[tt % 2][:, :, j * 128:(j + 1) * 128], bkb2.rearrange("p (c t) -> p c t", c=8),
                    [bkk2], ["x1lo%d_%d" % (tt % 2, j)])

            gmax, gsum, gp, m1, m2_, d21, w1, w2 = [r_s1[i] for i in range(8)]
            _unused, dg, exg, pen = [r_s4[i] for i in range(4)]
            lem, oh1, lem2, oh2, cA, cB = [r_sm[i] for i in range(6)]
            lgG = lg[:, :, 0:4]
            lgE4 = lg[:, :, 4:20].rearrange("p a (g e) -> p a g e", g=4)
            bc4 = lambda a: a[:].unsqueeze(2).to_broadcast([128, 4, 4])
            bc16 = lambda a: a[:].unsqueeze(2).to_broadcast([128, 4, 16])

            def r_0(tt):
                tcols = slice(tt * 512, (tt + 1) * 512)
                x1k = ["x1T_%d" % (tt * 4 + j) for j in range(4)]
                lok = ["x1lo%d_%d" % (tt % 2, j) for j in range(4)]
                br_, brk = nb()
                n = 0
                for (wt, wk, xsrc, xk) in ((wr_hi, "wr_hi", "hi", x1k), (wr_hi, "wr_hi", "lo", lok), (wr_lo, "wr_lo", "hi", x1k)):
                    for dc in range(8):
                        rhs = x1T[:, dc, tcols] if xsrc == "hi" else x1loT[tt % 2][:, dc, :]
                        mm(br_[0:20, :], wt[:, dc, :], rhs, n == 0, n == 23, [wk] + xk, [brk])
                        n += 1
                act(lgT[:], br_[0:20, :], AF.Identity, [brk, "br_sb"], ["lgT"], bias=br_sb[:, 0:1])
                bt_, btk = nb()
                for j in range(4):
                    tp(bt_[:, j * 20:(j + 1) * 20], lgT[:, j * 128:(j + 1) * 128], idf[0:20, 0:20], ["lgT", "idf"], [btk])
                cp("dve", lg[:].rearrange("p a b -> p (a b)"), bt_[:, 0:80], [btk], ["lg"])

            def r_1(tt):
                ohg = ohg_all[:, tt * 4:(tt + 1) * 4, :]
                V(lambda e: e.tensor_reduce(gmax[:], lgG, AX.X, ALU.max), ["lg"], ["gmax"])
                V(lambda e: e.tensor_tensor(ohg, lgG, bc4(gmax), ALU.is_equal), ["lg", "gmax"], ["ohg"])
                V(lambda e: e.tensor_copy(osum[:], ohg[:, :, 0:1]), ["ohg"], ["osum"])
                for g_ in range(1, 4):
                    V(lambda e: e.tensor_scalar(onm[:], osum[:], -1.0, 1.0, ALU.mult, ALU.add), ["osum"], ["onm"])
                    V(lambda e, g_=g_: e.tensor_tensor(ohg[:, :, g_:g_ + 1], ohg[:, :, g_:g_ + 1], onm[:], ALU.mult), ["ohg", "onm"], ["ohg"])
                    if g_ < 3:
                        V(lambda e, g_=g_: e.tensor_tensor(osum[:], osum[:], ohg[:, :, g_:g_ + 1], ALU.add), ["osum", "ohg"], ["osum"])
                V(lambda e: e.tensor_tensor(dg[:], lgG, bc4(gmax), ALU.subtract), ["lg", "gmax"], ["dg"])
                act(exg[:], dg[:], AF.Exp, ["dg"], ["exg"])
                V(lambda e: e.tensor_scalar(pen[:], ohg, BIG, -BIG, ALU.mult, ALU.add), ["ohg"], ["pen"])
                V(lambda e: e.tensor_tensor(lem[:].rearrange("p a (g e) -> p a g e", g=4), lgE4,
                                            pen[:].unsqueeze(3).to_broadcast([128, 4, 4, 4]), ALU.add), ["lg", "pen"], ["lem"])
                V(lambda e: e.tensor_reduce(m1[:], lem[:], AX.X, ALU.max), ["lem"], ["m1"])
                V(lambda e: e.tensor_tensor(oh1[:], lem[:], bc16(m1), ALU.is_equal), ["lem", "m1"], ["oh1"])
                V(lambda e: e.scalar_tensor_tensor(lem2[:], oh1[:], -BIG, lem[:], ALU.mult, ALU.add), ["oh1", "lem"], ["lem2"])
                V(lambda e: e.tensor_reduce(m2_[:], lem2[:], AX.X, ALU.max), ["lem2"], ["m2"])
                V(lambda e: e.tensor_tensor(oh2[:], lem2[:], bc16(m2_), ALU.is_equal), ["lem2", "m2"], ["oh2"])
                V(lambda e: e.tensor_tensor(d21[:], m2_[:], m1[:], ALU.subtract), ["m1", "m2"], ["d21"])
                act(d21[:], d21[:], AF.Exp, ["d21"], ["d21"])

            def r_2(tt):
                tcols = slice(tt * 512, (tt + 1) * 512)
                V(lambda e: e.tensor_reduce(gsum[:], exg[:], AX.X, ALU.add), ["exg"], ["gsum"])
                V(lambda e: e.reciprocal(gp[:], gsum[:]), ["gsum"], ["gp"])
                V(lambda e: e.tensor_scalar(d21[:], d21[:], 1.0, None, ALU.add), ["d21"], ["d21"])
                V(lambda e: e.reciprocal(w1[:], d21[:]), ["d21"], ["w1"])
                V(lambda e: e.tensor_scalar(w2[:], w1[:], -1.0, 1.0, ALU.mult, ALU.add), ["w1"], ["w2"])
                V(lambda e: e.tensor_tensor(w1[:], w1[:], gp[:], ALU.mult), ["w1", "gp"], ["w1"])
                V(lambda e: e.tensor_tensor(w2[:], w2[:], gp[:], ALU.mult), ["w2", "gp"], ["w2"])
                V(lambda e: e.tensor_tensor(cA[:], oh1[:], bc16(w1), ALU.mult), ["oh1", "w1"], ["cA"])
                V(lambda e: e.tensor_tensor(cB[:], oh2[:], bc16(w2), ALU.mult), ["oh2", "w2"], ["cB"])
                V(lambda e: e.tensor_tensor(cA[:], cA[:], cB[:], ALU.add), ["cA", "cB"], ["cA"])
                V(lambda e: e.tensor_copy(chl[:, :, 0:16], cA[:]), ["cA"], ["chl_hi"])
                V(lambda e: e.tensor_tensor(chl[:, :, 32:48], cA[:], chl[:, :, 0:16], ALU.subtract), ["cA", "chl_hi"], ["chl_lo"])
                S.dma("sp", "d_chl", xh_dram[tt * 512:(tt + 1) * 512, 1024:XHW].rearrange("(j p) c -> p j c", p=128),
                      chl[:], reads=["chl_hi", "chl_lo", "chl_z"], writes=["chld%d" % tt])

            def grp(fn, off):
                return (off, lambda g: fn(g // 4) if g % 4 == 3 else None)

            pipeline([(0, b_dma), (1, b_xn), (2, b_mix), (3, b_r1), (4, b_sqrt), (5, b_rcp), (6, b_n1), (7, b_g1),
                      (8, b_b1), (9, b_hi), (10, b_lo), (11, b_x1T), (12, b_x1lo),
                      grp(r_0, 13), grp(r_1, 14), grp(r_2, 15)], 16)

            oflat = ohg_all[:].rearrange("p s g -> p (s g)")
            S.dma("sp", None, tri_sb[:], tri, writes=["tri_sb"])
            S.dma("sp", None, tid_sb[:], tid, writes=["tid_sb"])
            bw, bwk = nb()
            mm(bw[:, 0:64], tri_sb[:], oflat, True, True, ["tri_sb", "ohg"], [bwk])
            bt, btk = nb()
            mm(bt[:, 0:64], ones_f[:], oflat, True, True, ["ones_f", "ohg"], [btk])
            cp("dve", Wt[:].rearrange("p s g -> p (s g)"), bw[:, 0:64], [bwk], ["Wt"])
            cp("dve", Tt[:].rearrange("p s g -> p (s g)"), bt[:, 0:64], [btk], ["Tt"])
            V(lambda e: e.memset(Et[:, 0, :], 0.0), [], ["Et"])
            for s_ in range(1, 16):
                V(lambda e, s_=s_: e.tensor_tensor(Et[:, s_, :], Et[:, s_ - 1, :], Tt[:, s_ - 1, :], ALU.add), ["Et", "Tt"], ["Et"])
            V(lambda e: e.tensor_tensor(cntv[:], Et[:, 15, :], Tt[:, 15, :], ALU.add), ["Et", "Tt"], ["cntv"])
            V(lambda e: e.memset(startv[:, 0:1], 0.0), [], ["startv"])
            for g_ in range(1, 4):
                V(lambda e, g_=g_: e.tensor_tensor(startv[:, g_:g_ + 1], startv[:, g_ - 1:g_], cntv[:, g_ - 1:g_], ALU.add),
                  ["startv", "cntv"], ["startv"])
            V(lambda e: e.tensor_tensor(Wt[:], Wt[:], Et[:], ALU.add), ["Wt", "Et"], ["Wt"])
            V(lambda e: e.tensor_tensor(Wt[:], Wt[:], startv[:].unsqueeze(1).to_broadcast([128, 16, 4]), ALU.add),
              ["Wt", "startv"], ["Wt"])
            V(lambda e: e.tensor_tensor(Wt[:], Wt[:], ohg_all[:], ALU.mult), ["Wt", "ohg"], ["Wt"])
            V(lambda e: e.tensor_reduce(posf[:], Wt[:], AX.X, ALU.add), ["Wt"], ["posf"])
            V(lambda e: e.tensor_copy(posi[:], posf[:]), ["posf"], ["posi"])
            for s_ in range(16):
                S.dma_fn("pool", "d_inv", lambda eng, s_=s_: eng.indirect_dma_start(
                    out=inv_dram[:, :], out_offset=bass.IndirectOffsetOnAxis(ap=posi[:, s_:s_ + 1], axis=0),
                    in_=tid_sb[:, s_:s_ + 1], in_offset=None), reads=["posi", "tid_sb"], writes=["inv_dram%d" % s_])
            for q in range(16):
                S.dma("sp" if q % 2 == 0 else "act", "d_invl%d" % (q % 2), inv_sb[:, q:q + 1], inv_dram[q * 128:(q + 1) * 128, :],
                      reads=["inv_dram%d" % s_ for s_ in range(16)], writes=["inv_sb%d" % q])
            V(lambda e: e.tensor_tensor(hiv[:], startv[:], cntv[:], ALU.add), ["startv", "cntv"], ["hiv"])
            for tt in range(4):
                V(lambda e, tt=tt: e.tensor_scalar(f1[:], startv[:], float((tt + 1) * 512), None, ALU.is_lt), ["startv"], ["f1"])
                V(lambda e, tt=tt: e.tensor_scalar(f2[:], hiv[:], float(tt * 512), None, ALU.is_gt), ["hiv"], ["f2"])
                V(lambda e, tt=tt: e.tensor_tensor(flagsf[:, tt, :], f1[:], f2[:], ALU.mult), ["f1", "f2"], ["flagsf"])
            V(lambda e: e.memset(bitsf[:], 0.0), [], ["bitsf"])
            for k in range(16):
                V(lambda e, k=k: e.scalar_tensor_tensor(bitsf[:], flagsf[:, k // 4, k % 4:k % 4 + 1], float(2 ** k), bitsf[:],
                                                        ALU.mult, ALU.add), ["flagsf", "bitsf"], ["bitsf"])
            if os.environ.get("K_ALLFLAGS"):
                V(lambda e: e.memset(bitsf[:], 65535.0), ["bitsf"], ["bitsf"])
            V(lambda e: e.tensor_copy(flagsi[:, 0:1], bitsf[:]), ["bitsf"], ["flagsi"])
            S.dma("sp", None, flags_dram[0:1, 0:1], flagsi[0:1, 0:1], reads=["flagsi"], writes=["flags"])
            if dbg is not None:
                S.dma("sp", None, dbg[:, 0:16], flagsf[:].rearrange("p a b -> p (a b)"), reads=["flagsf"], writes=["dbg0"])
                S.dma("sp", None, dbg[:, 16:18], flagsi[:, 0:2].bitcast(F32), reads=["flagsi"], writes=["dbg1"])
                S.dma("sp", None, dbg[:, 20:24], startv[:], reads=["startv"], writes=["dbg2"])
                S.dma("sp", None, dbg[:, 24:28], cntv[:], reads=["cntv"], writes=["dbg3"])
                S.dma("sp", None, dbg[:, 32:48], posf[:], reads=["posf"], writes=["dbg4"])
            S.barrier()
            S.emit()
        if stop_after == "1b":
            return nc

        yacc = big[:, :].rearrange("p (g d) -> p g d", g=16)
        with contextlib.ExitStack() as s3:
            for i in range(1, NSLOT):
                wg[i] = sb(s3, "wg%d" % i, [128, 8, DE], BF16)
                wu[i] = sb(s3, "wu%d" % i, [128, 8, DE], BF16)
            for i in range(1, NWD):
                wd[i] = sb(s3, "wd%d" % i, [128, 2, D], BF16)
            NHID = 6
            hid_r = [sb(s3, "hidr%d" % i, [128, 2, 512], BF16) for i in range(2)]
            sel = sb(s3, "sel", [48, NE, 128], BF16)
            scr = sb(s3, "scr", [1, 512], F32)
            scrp = sb(s3, "scrp", [128, 160], F32)
            NXG = 6
            xg = [sb(s3, "xg%d" % i, [128, XHW], BF16) for i in range(NXG)]
            hid = [xg[i][:, 0:1024].rearrange("p (f t) -> p f t", f=2) for i in range(NXG)]
            hid = hid + hid_r
            hidk = lambda k: ["hid%d" % k, "xg%d" % k] if k < NXG else ["hid%d" % k]
            cbs = [sb(s3, "cbs%d" % i, [128, 512], F32) for i in range(2)]
            NSS = 3
            sbuf_s = [sb(s3, "mss%d" % i, [128, 512], F32) for i in range(NSS)]
            sbuf_t = [sb(s3, "mst%d" % i, [128, 512], F32) for i in range(NSS)]

            V(lambda e: e.memset(sel[:], 0.0), [], ["sel"])
            V(lambda e: e.tensor_copy(sel[0:16, :, :], idf[0:16, 0:16].unsqueeze(2).to_broadcast([16, NE, 128])),
              ["idf", "sel"], ["sel"])
            V(lambda e: e.tensor_copy(sel[32:48, :, :], idf[32:48, 32:48].unsqueeze(2).to_broadcast([16, NE, 128])),
              ["idf", "sel"], ["sel"])
            nbanks[0] = 7
            S.op("dve", lambda e: e.memset(scr[:], 0.0), [], ["scr"])
            S.dummy["pe"] = lambda eng, i: eng.matmul(banks[7][0:1, i:i + 1], ones_rb[0:1, 0:1], ones_rb[0:1, 0:1], start=True, stop=True)
            S.dummy["act"] = lambda eng, i: eng.copy(scr[0:1, 384 + i:385 + i], scr[0:1, 511:512])
            S.dummy["dve"] = lambda eng, i: eng.memset(scr[0:1, i:i + 1], 0.0)
            S.dummy["pool"] = lambda eng, i: eng.memset(scrp[:, i:i + 1], 0.0)

            def load_flags(e):
                def f(eng):
                    r = eng.alloc_register("fl_%s" % e)
                    eng.reg_load(r, flags_dram[0:1, 0:1])
                    S.regs[e] = eng.snap(r)
                    return None
                return f
            for e in ("pe", "act", "dve", "pool"):
                S.raw(e, load_flags(e))

            S.op("dve", lambda e: e.memset(big[:, 0:8192], 0.0), [], ["yz0"])


            def gather_q(q):
                xg_ = xg[q % NXG]
                S.dma_fn("pool", "d_xg%d" % (q % NXG), lambda eng: eng.indirect_dma_start(
                    out=xg_[:, :], out_offset=None, in_=xh_dram[:, :],
                    in_offset=bass.IndirectOffsetOnAxis(ap=inv_sb[:, q:q + 1], axis=0)), reads=[],
                    writes=["xg%d" % (q % NXG), "hid%d" % (q % NXG)])

            def trans_q(q):
                qc = slice(q * 128, (q + 1) * 128)
                xg_ = xg[q % NXG]
                xgk = "xg%d" % (q % NXG)
                bk, bkk = nb()
                bkb = bk[:].bitcast(BF16)
                for dc in range(8):
                    tp(bkb[:, dc * 128:(dc + 1) * 128], xg_[:, dc * 128:(dc + 1) * 128], idb[:], [xgk, "idb"], [bkk])
                acp(x1T[:, :, qc], bkb.rearrange("p (c t) -> p c t", c=8), [bkk], ["x1T_%d" % q])
                bk2, bkk2 = nb()
                bkb2 = bk2[:].bitcast(BF16)
                tp(bkb2[0:48, 0:128], xg_[:, 1024:XHW], idb[:], [xgk, "idb"], [bkk2])
                cp("dve", combT[:, qc], bkb2[0:48, 0:128], [bkk2], ["combT_%d" % q])

            for q in range(NXG):
                gather_q(q)
            load_expert(1)
            S.op("pool", lambda e: e.memset(big[:, 8192:16384], 0.0), [], ["yz1"])

            steps = [(e_, tt) for e_ in range(NE) for tt in range(4)]
            pend_b = {}
            uctr = 0

            def down(e_, tt, hslot):
                slot = e_ % NWD
                for j in range(4):
                    g = tt * 4 + j
                    for h in range(2):
                        bk, bkk = nb()
                        for fc in range(2):
                            mm(bk[:, :], hid[hslot][:, fc, j * 128:(j + 1) * 128], wd[slot][:, fc, h * 512:(h + 1) * 512],
                               fc == 0, fc == 1, hidk(hslot) + ["wd%d" % slot], [bkk])
                        ysl = yacc[:, g, h * 512:(h + 1) * 512]
                        yk = "y_%d_%d" % (g, h)
                        tt_("dve", ysl, ysl, bk[:], ALU.add, [bkk, yk, "yz0", "yz1"], [yk])

            for si, (e_, tt) in enumerate(steps):
                slot = e_ % NSLOT
                tcols = slice(tt * 512, (tt + 1) * 512)
                fl = tt * 4 + e_ // 4
                if e_ == 0:
                    for q in range(tt * 4, tt * 4 + 4):
                        trans_q(q)
                        if q + NXG < 16:
                            gather_q(q + NXG)
                    if tt == 1:
                        load_expert(2)
                if tt == 1 and e_ >= 1 and e_ + 2 < NE:
                    load_expert(e_ + 2)
                if tt == 2 and e_ == NE - 2:
                    S.dma("pool", "d_wob", wob[:], w_pg_v, writes=["wob"])
                S.cond_begin(fl)
                xk = ["x1T_%d" % (tt * 4 + j) for j in range(4)]
                ck = ["combT_%d" % (tt * 4 + j) for j in range(4)]
                bcb, bcbk = nb()
                mm(bcb[:, :], sel[:, e_, :], combT[:, tcols], True, True, ["sel"] + ck, [bcbk])
                pg = []
                pu = []
                for fc in range(2):
                    bk, bkk = nb()
                    for dc in range(8):
                        mm(bk[:, :], wg[slot][:, dc, fc * 128:(fc + 1) * 128], x1T[:, dc, tcols], dc == 0, dc == 7,
                           ["wg%d" % slot] + xk, [bkk])
                    pg.append((bk, bkk))
                for fc in range(2):
                    bk, bkk = nb()
                    for dc in range(8):
                        mm(bk[:, :], wu[slot][:, dc, fc * 128:(fc + 1) * 128], x1T[:, dc, tcols], dc == 0, dc == 7,
                           ["wu%d" % slot] + xk, [bkk])
                    pu.append((bk, bkk))
                cb = cbs[si % 2]
                cbk = "cbs%d" % (si % 2)
                cp("dve", cb[:], bcb[:], [bcbk], [cbk])
                hslot = (NXG + tt % 2) if e_ == 0 else si % NHID
                for fc in range(2):
                    u = uctr % NSS
                    uctr += 1
                    act(sbuf_s[u][:], pg[fc][0][:], AF.Silu, [pg[fc][1]], ["ss%d" % u])
                    tt_("dve", sbuf_t[u][:], pu[fc][0][:], cb[:], ALU.mult, [pu[fc][1], cbk], ["st%d" % u])
                    tt_("pool", hid[hslot][:, fc, :], sbuf_s[u][:], sbuf_t[u][:], ALU.mult, ["ss%d" % u, "st%d" % u],
                        hidk(hslot))
                pend_b[(e_, tt)] = (e_, tt, hslot, fl)
                if e_ == 0:
                    prev = (0, tt - 1) if tt >= 1 else None
                elif e_ == 1:
                    prev = (0, 3) if tt == 0 else None
                else:
                    prev = (e_ - 1, tt)
                if e_ == 1 and tt == 3:
                    prev2 = None
                if prev is not None and pend_b[prev][3] == fl:
                    pb_ = pend_b.pop(prev)
                    down(*pb_[0:3])
                    S.cond_end()
                else:
                    S.cond_end()
                    if prev is not None:
                        pb_ = pend_b.pop(prev)
                        S.cond_begin(pb_[3])
                        down(*pb_[0:3])
                        S.cond_end()
            for e_r in (1,):
                pass
            for key in sorted(pend_b.keys()):
                pb_ = pend_b.pop(key)
                S.cond_begin(pb_[3])
                down(*pb_[0:3])
                S.cond_end()
            nbanks[0] = 8
            S.barrier()
            S.emit()
        sM.close()
        if stop_after == "2":
            return nc

        with contextlib.ExitStack() as s4:
            wpg = wob
            wpp = sb(s4, "wpp", [128, 2, D], BF16)
            bc_g2 = sb(s4, "bc_g2", [128, 1024], F32)
            bc_b2 = sb(s4, "bc_b2", [128, 1024], F32)
            N3 = 3
            x1r = [sb(s4, "x1r%d" % i, [128, 1024], F32) for i in range(N3)]
            pt = [sb(s4, "pt%d" % i, [128, DPLE], F32) for i in range(N3)]
            rb = [sb(s4, "rb%d" % i, [128, 1024], BF16) for i in range(N3)]
            pb16 = [sb(s4, "pb16_%d" % i, [128, DPLE], BF16) for i in range(N3)]
            rT = [sb(s4, "rT%d" % i, [128, 8, 128], BF16) for i in range(N3)]
            pT = [sb(s4, "pT%d" % i, [128, 2, 128], BF16) for i in range(N3)]
            gt = [sb(s4, "gt%d" % i, [128, 512], F32) for i in range(4)]
            gpt = [sb(s4, "gpt%d" % i, [128, 512], F32) for i in range(4)]
            NOB = 6
            ob = [sb(s4, "ob%d" % i, [128, 1024], F32) for i in range(NOB)]
            st2 = sb(s4, "st2", [128, 16, 2, 6], F32)
            mv2 = sb(s4, "mv2", [128, 16, 2], F32)
            rs2 = sb(s4, "rs2", [128, 16], F32)
            nb2 = sb(s4, "nb2", [128, 16], F32)

            S.dma("pool", "d_wpp", wpp[:], w_pp_v, writes=["wpp"])
            S.dma("sp", None, bc_g2[:], rows[:, R_L2G:R_L2G + 1024].partition_broadcast(128), writes=["bc_g2"])
            S.dma("sp", None, bc_b2[:], rows[:, R_L2B:R_L2B + 1024].partition_broadcast(128), writes=["bc_b2"])
            hctr = [0]

            pbanks = {}

            def c_dma(g):
                k = g % N3
                S.dma_fn("pool", "d_x1r%d" % k, lambda eng: eng.indirect_dma_start(
                    out=x1r[k][:, :], out_offset=None, in_=x1_dram[:, :],
                    in_offset=bass.IndirectOffsetOnAxis(ap=inv_sb[:, g:g + 1], axis=0)), reads=[], writes=["x1r%d" % k])
                S.dma_fn("pool", "d_pt%d" % k, lambda eng: eng.indirect_dma_start(
                    out=pt[k][:, :], out_offset=None, in_=ps[:, :],
                    in_offset=bass.IndirectOffsetOnAxis(ap=inv_sb[:, g:g + 1], axis=0)), reads=[], writes=["pt%d" % k])

            def c_r(g):
                k = g % N3
                yk = ["y_%d_0" % g, "y_%d_1" % g]
                stt(yacc[:, g, :], x1r[k][:], ALPHA, yacc[:, g, :], ALU.mult, ALU.add, ["x1r%d" % k] + yk, yk)
                cp("dve", pb16[k][:], pt[k][:], ["pt%d" % k], ["pb16_%d" % k])

            def c_rb(g):
                k = g % N3
                yk = ["y_%d_0" % g, "y_%d_1" % g]
                acp(rb[k][:], yacc[:, g, :], yk, ["rb%d" % k])
                bk2, bkk2 = nb()
                bkb2 = bk2[:].bitcast(BF16)
                for kc in range(2):
                    tp(bkb2[:, kc * 128:(kc + 1) * 128], pb16[k][:, kc * 128:(kc + 1) * 128], idb[:], ["pb16_%d" % k, "idb"], [bkk2])
                pbanks[("p", g)] = (bkb2, bkk2)

            def c_tr(g):
                k = g % N3
                bkb2, bkk2 = pbanks.pop(("p", g))
                cp("dve", pT[k][:], bkb2[:, 0:256].rearrange("p (c t) -> p c t", c=2), [bkk2], ["pT%d" % k])
                bk, bkk = nb()
                bkb = bk[:].bitcast(BF16)
                for dc in range(8):
                    tp(bkb[:, dc * 128:(dc + 1) * 128], rb[k][:, dc * 128:(dc + 1) * 128], idb[:], ["rb%d" % k, "idb"], [bkk])
                pbanks[("r", g)] = (bkb, bkk)

            def c_rT(g):
                k = g % N3
                bkb, bkk = pbanks.pop(("r", g))
                acp(rT[k][:], bkb.rearrange("p (c t) -> p c t", c=8), [bkk], ["rT%d" % k])

            def c_mm(g):
                k = g % N3
                for h in range(2):
                    hs = slice(h * 512, (h + 1) * 512)
                    bg_, bgk = nb()
                    for dc in range(8):
                        mm(bg_[:, :], rT[k][:, dc, :], wpg[:, dc, hs], dc == 0, False, ["rT%d" % k, "wob"], [bgk])
                    mm(bg_[:, :], ones_rb[0:1, :], brow_b[0:1, 1024 + h * 512:1024 + (h + 1) * 512], False, True,
                       ["ones_rb", "brow"], [bgk])
                    bp_, bpk = nb()
                    for kc in range(2):
                        mm(bp_[:, :], pT[k][:, kc, :], wpp[:, kc, hs], kc == 0, kc == 1, ["pT%d" % k, "wpp"], [bpk])
                    u = hctr[0] % 4
                    hctr[0] += 1
                    act(gt[u][:], bg_[:], AF.Sigmoid, [bgk], ["gt%d" % u])
                    pbanks[("g", g, h)] = (u, bp_, bpk)

            def c_gp(g):
                for h in range(2):
                    u, bp_, bpk = pbanks.pop(("g", g, h))
                    tt_("dve", gpt[u][:], gt[u][:], bp_[:], ALU.mult, ["gt%d" % u, bpk], ["gpt%d" % u])
                    pbanks[("a", g, h)] = u

            def c_add(g):
                for h in range(2):
                    hs = slice(h * 512, (h + 1) * 512)
                    u = pbanks.pop(("a", g, h))
                    tt_("pool", yacc[:, g, hs], yacc[:, g, hs], gpt[u][:], ALU.add, ["y_%d_%d" % (g, h), "gpt%d" % u],
                        ["y_%d_%d" % (g, h)])

            def c_st(g):
                V(lambda e: e.bn_stats(st2[:, g, 0, :], yacc[:, g, 0:512]), ["y_%d_0" % g], ["st2a%d" % g])
                V(lambda e: e.bn_stats(st2[:, g, 1, :], yacc[:, g, 512:1024]), ["y_%d_1" % g], ["st2b%d" % g])
                V(lambda e: e.bn_aggr(mv2[:, g, :], st2[:, g, :, :]), ["st2a%d" % g, "st2b%d" % g], ["mv2_%d" % g])

            def c_sqrt(G):
                g0 = G * 4
                act(rs2[:, g0:g0 + 4], mv2[:, g0:g0 + 4, 1], AF.Sqrt, ["mv2_%d" % (g0 + j) for j in range(4)],
                    ["rs2a%d" % g0], bias=EPS)

            def c_rcp(G):
                g0 = G * 4
                V(lambda e: e.reciprocal(rs2[:, g0:g0 + 4], rs2[:, g0:g0 + 4]), ["rs2a%d" % g0], ["rs2_%d" % g0])
                V(lambda e: e.scalar_tensor_tensor(nb2[:, g0:g0 + 4], mv2[:, g0:g0 + 4, 0], -1.0, rs2[:, g0:g0 + 4],
                                                   ALU.mult, ALU.mult),
                  ["rs2_%d" % g0] + ["mv2_%d" % (g0 + j) for j in range(4)], ["nb2_%d" % g0])

            def c_on(g):
                k = g % NOB
                g0 = g - g % 4
                yk = ["y_%d_0" % g, "y_%d_1" % g]
                act(ob[k][:], yacc[:, g, :], AF.Identity, yk + ["nb2_%d" % g0, "rs2_%d" % g0], ["ob%d" % k],
                    bias=nb2[:, g:g + 1], scale=rs2[:, g:g + 1])

            def c_og(g):
                k = g % NOB
                tt_("pool", ob[k][:], ob[k][:], bc_g2[:], ALU.mult, ["ob%d" % k, "bc_g2"], ["ob%d" % k])

            def c_ob(g):
                k = g % NOB
                tt_("dve", ob[k][:], ob[k][:], bc_b2[:], ALU.add, ["ob%d" % k, "bc_b2"], ["ob%d" % k])
                S.dma_fn("pool", "d_out%d" % k, lambda eng: eng.indirect_dma_start(
                    out=out[:, :], out_offset=bass.IndirectOffsetOnAxis(ap=inv_sb[:, g:g + 1], axis=0),
                    in_=ob[k][:, :], in_offset=None), reads=["ob%d" % k], writes=["outd%d" % g])

            def tail(off):
                return lambda g: off - (g - 12) if g >= 12 else off

            def grp3(fn, off):
                return (off, lambda g: fn(g // 4) if g % 4 == 3 else None)

            pipeline([(0, c_dma), (1, c_r), (2, c_rb), (3, c_tr), (4, c_rT), (6, c_gp), (7, c_add), (8, c_st),
                      grp3(c_sqrt, 9), grp3(c_rcp, 10), (tail(14), c_on), (tail(15), c_og), (tail(16), c_ob), (5, c_mm)], 16)
            S.final_wait("sp", ["d_out%d" % k for k in range(NOB)])
            S.emit()
    return nc


_NC_CACHE = {}


def _prep_inputs(inp):
    f = lambda a: np.ascontiguousarray(np.asarray(a, dtype=np.float32))
    x = f(inp["x"])
    p = f(inp["p"])[0]
    w_in = f(inp["w_in"])[0]
    b_in = f(inp["b_in"])[0]
    perm = []
    for i in range(4):
        perm += list(range(i * 128, (i + 1) * 128)) + list(range(512 + i * 128, 512 + (i + 1) * 128))
    for i in range(4):
        perm += list(range(1536 + i * 128, 1536 + (i + 1) * 128))
        perm += list(range(2048 + i * 128, 2048 + (i + 1) * 128))
        perm += list(range(1024 + i * 128, 1024 + (i + 1) * 128))
    perm = np.array(perm)
    w_in_p = np.ascontiguousarray(w_in[:, perm])
    b_in_p = b_in[perm].reshape(20, 128).T
    col = lambda v, n: f(v).reshape(n, 128).T
    dww = f(inp["conf_dw_w"])[0]
    dww_p = dww.T.reshape(4, 128, 31).transpose(1, 0, 2).reshape(128, 124)
    scw = f(inp["sc_w"])[0]
    scw_p = scw.T.reshape(4, 128, 3).transpose(1, 0, 2).reshape(128, 12)
    rows = np.concatenate([f(inp["ln_in_g"]), f(inp["ln_in_b"]), f(inp["ln1_g"])[0], f(inp["ln1_b"])[0],
                           f(inp["ln2_g"])[0], f(inp["ln2_b"])[0], f(inp["b_out"])[0], f(inp["b_pg"])[0]])[None, :]
    w_r = np.ascontiguousarray(np.concatenate([f(inp["w_rg"])[0], f(inp["w_re"])[0]], axis=1))
    b_r = np.concatenate([f(inp["b_rg"])[0], f(inp["b_re"])[0]])[:, None]
    shared = {
        "rows": np.ascontiguousarray(rows), "w_in": w_in_p, "w_out": f(inp["w_out"])[0], "w_r": w_r,
        "b_r": np.ascontiguousarray(b_r), "w_gate": f(inp["w_gate"])[0], "w_up": f(inp["w_up"])[0],
        "w_down": f(inp["w_down"])[0], "w_pg": f(inp["w_pg"])[0], "w_pp": f(inp["w_pp"])[0],
        "ident": np.eye(128, dtype=np.float32),
        "tri": np.triu(np.ones((128, 128), np.float32), k=1),
        "tid": (np.arange(16, dtype=np.int32)[None, :] * 128 + np.arange(128, dtype=np.int32)[:, None]).astype(np.int32),
    }
    maps = []
    for c in range(NCORES):
        b, q = divmod(c, 4)
        lo = q * NT
        if q == 0:
            halo = np.zeros((HALO, D), np.float32)
            mask = 0.0
        else:
            halo = x[b, lo - HALO:lo]
            mask = 1.0
        xs = np.ascontiguousarray(np.concatenate([halo, x[b, lo:lo + NT]], axis=0))
        pp = np.concatenate([b_in_p, col(inp["ln_in_g"], 8), col(inp["ln_in_b"], 8), dww_p,
                             col(f(inp["conf_dw_b"])[0], 4), col(f(inp["conf_ln_g"])[0], 4), col(f(inp["conf_ln_b"])[0], 4),
                             scw_p, col(f(inp["sc_b"])[0], 4), np.full((128, 1), mask, np.float32)], axis=1)
        m = dict(shared)
        m["xs"] = xs
        m["ps"] = np.ascontiguousarray(p[b, lo:lo + NT])
        m["pp"] = np.ascontiguousarray(pp.astype(np.float32))
        maps.append(m)
    return maps


def kernel(**inputs):
    if "nc" not in _NC_CACHE:
        _NC_CACHE["nc"] = build_nc()
    nc = _NC_CACHE["nc"]
    maps = _prep_inputs(inputs)
    res = run_bass_kernel_spmd(nc, maps, core_ids=list(range(NCORES)))
    outs = [np.asarray(r["out"], dtype=np.float32) for r in res.results]
    full = np.stack(outs, axis=0).reshape(2, 4 * NT, D)
    return full
```

```python
import contextlib
import os
import numpy as np
import concourse.bass as bass
import concourse.mybir as mybir
from concourse.bass_utils import run_bass_kernel_spmd

F32 = mybir.dt.float32
I32 = mybir.dt.int32
BF16 = mybir.dt.bfloat16
AF = mybir.ActivationFunctionType
ALU = mybir.AluOpType
AX = mybir.AxisListType

NCORES = 8
NT = 2048
HALO = 32
D = 1024
DIN = 2560
NE = 16
DE = 256
DPLE = 256
XHW = 1072
EPS = 1e-5
ALPHA = 2.0 ** 0.25
BIG = 1.0e4
ENGS = ["pe", "act", "dve", "pool", "sp"]

PP_BIN = 0
PP_LNG = 20
PP_LNB = 28
PP_DWW = 36
PP_DWB = 160
PP_CLG = 164
PP_CLB = 168
PP_SCW = 172
PP_SCB = 184
PP_MASK = 188
PP_N = 189
R_LNIG, R_LNIB, R_L1G, R_L1B, R_L2G, R_L2B, R_BOUT, R_BPG = [i * 1024 for i in range(8)]


class Sched:
    def __init__(self, nc, stack):
        self.nc = nc
        self.stack = stack
        self.prog = {e: [] for e in ENGS}
        self.sem = {}
        self.cnt = {}
        self.seen = {e: {} for e in ENGS}
        self.res = {}
        self.in_cond = False
        self.regs = {}
        self.dummy = {}
        self.dummy_ctr = {}
        for e in ENGS:
            self.new_sem(e)

    def new_sem(self, key):
        if key not in self.sem:
            self.sem[key] = self.stack.enter_context(self.nc.semaphore("s_" + key))
            self.cnt[key] = 0
        return key

    def _deps(self, e, reads, writes):
        deps = {}

        def add(k, v):
            if deps.get(k, 0) < v:
                deps[k] = v

        for r in reads:
            st = self.res.get(r)
            if st:
                for k, v in st["w"].items():
                    add(k, v)
        for w in writes:
            st = self.res.get(w)
            if st:
                for k, v in st["w"].items():
                    add(k, v)
                for k, v in st["r"].items():
                    add(k, v)
        waits = []
        for k, v in deps.items():
            if k == e and e in ("pe", "sp"):
                continue
            if self.seen[e].get(k, 0) >= v:
                continue
            self.seen[e][k] = v
            waits.append((k, v))
        return waits

    def _commit(self, me, reads, writes):
        for r in reads:
            st = self.res.setdefault(r, {"w": {}, "r": {}})
            if st["r"].get(me[0], 0) < me[1]:
                st["r"][me[0]] = me[1]
        for w in writes:
            if self.in_cond:
                st = self.res.setdefault(w, {"w": {}, "r": {}})
                if st["w"].get(me[0], 0) < me[1]:
                    st["w"][me[0]] = me[1]
            else:
                self.res[w] = {"w": {me[0]: me[1]}, "r": {}}

    def op(self, e, fn, reads=(), writes=()):
        waits = self._deps(e, reads, writes)
        self.cnt[e] += 1
        me = (e, self.cnt[e])
        self.prog[e].append((waits, fn, (e, 1)))
        self._commit(me, reads, writes)
        return me

    def dma(self, q, semkey, out, in_, reads=(), writes=()):
        if semkey is None:
            self.uniq = getattr(self, "uniq", 0) + 1
            semkey = "du%d" % self.uniq
        self.new_sem(semkey)
        waits = self._deps(q, reads, writes)
        self.cnt[semkey] += 16
        me = (semkey, self.cnt[semkey])
        self.prog[q].append((waits, lambda eng: eng.dma_start(out=out, in_=in_), (semkey, 16)))
        self._commit(me, reads, writes)
        return me

    def dma_fn(self, q, semkey, fn, reads=(), writes=()):
        self.new_sem(semkey)
        waits = self._deps(q, reads, writes)
        self.cnt[semkey] += 16
        me = (semkey, self.cnt[semkey])
        self.prog[q].append((waits, fn, (semkey, 16)))
        self._commit(me, reads, writes)
        return me

    def raw(self, e, fn, reads=()):
        waits = self._deps(e, reads, ())
        self.prog[e].append((waits, fn, None))

    def cond_begin(self, flag):
        assert not self.in_cond
        self.in_cond = True
        self.seen_saved = {e: dict(d) for e, d in self.seen.items()}
        self.cond_info = {"snap": dict(self.cnt), "ext": {}}
        for e in ENGS:
            self.prog[e].append(("cond_begin", flag, self.cond_info))

    def cond_end(self):
        self.in_cond = False
        self.seen = self.seen_saved
        info = self.cond_info
        snap = info["snap"]
        for e in ENGS:
            prog = self.prog[e]
            i = len(prog) - 1
            while prog[i][0] != "cond_begin":
                for k, v in prog[i][0]:
                    if v <= snap.get(k, 0) and info["ext"].get(k, 0) < v:
                        info["ext"][k] = v
                i -= 1
            self.prog[e].append(("cond_end", None, None))

    def barrier(self):
        for e in ENGS:
            waits = []
            for k, v in self.cnt.items():
                if v == 0 or k == e:
                    continue
                if self.seen[e].get(k, 0) >= v:
                    continue
                self.seen[e][k] = v
                waits.append((k, v))
            self.prog[e].append((waits, None, None))
        self.res = {}

    def final_wait(self, e, semkeys):
        waits = [(k, self.cnt[k]) for k in semkeys if self.cnt.get(k, 0) > 0]
        self.prog[e].append((waits, None, None))

    def emit(self):
        nc = self.nc
        with nc.Block() as block:
            def replay(e, eng):
                prog = self.prog[e]
                i = 0
                while i < len(prog):
                    waits, fn, inc = prog[i]
                    if waits == "cond_begin":
                        j = i + 1
                        while prog[j][0] != "cond_end":
                            j += 1
                        body = prog[i + 1:j]
                        n_inc = sum(1 for b in body if b[2] is not None and b[2][0] == e)
                        if body:
                            cm = eng.If(((self.regs[e] >> fn) & 1) != 0)
                            cm.__enter__()
                            for w_, f_, inc_ in body:
                                for k, v in w_:
                                    eng.wait_ge(self.sem[k], v)
                                if f_ is not None:
                                    ins = f_(eng)
                                    if inc_ is not None:
                                        ins.then_inc(self.sem[inc_[0]], inc_[1])
                            cm.__exit__(None, None, None)
                            if n_inc > 0:
                                info = inc
                                cm2 = eng.Else()
                                cm2.__enter__()
                                self.dummy_ctr[e] = self.dummy_ctr.get(e, 0) + 1
                                self.dummy[e](eng, self.dummy_ctr[e]).then_inc(self.sem[e], n_inc)
                                cm2.__exit__(None, None, None)
                        i = j + 1
                        continue
                    for k, v in waits:
                        eng.wait_ge(self.sem[k], v)
                    if fn is not None:
                        ins = fn(eng)
                        if inc is not None:
                            ins.then_inc(self.sem[inc[0]], inc[1])
                    i += 1
                self.prog[e] = []

            @block.tensor
            def _(eng):
                replay("pe", eng)

            @block.scalar
            def _(eng):
                replay("act", eng)

            @block.vector
            def _(eng):
                replay("dve", eng)

            @block.gpsimd
            def _(eng):
                replay("pool", eng)

            @block.sync
            def _(eng):
                replay("sp", eng)


def build_nc(stop_after=None):
    nc = bass.Bass("TRN2", target_bir_lowering=False)
    dt_in = lambda n, s: nc.dram_tensor(n, s, F32, kind="ExternalInput").ap()
    xs = dt_in("xs", [NT + HALO, D])
    ps = dt_in("ps", [NT, DPLE])
    pp = dt_in("pp", [128, PP_N])
    rows = dt_in("rows", [1, 8192])
    w_in = dt_in("w_in", [D, DIN])
    w_out = dt_in("w_out", [D, D])
    w_r = dt_in("w_r", [D, 20])
    b_r = dt_in("b_r", [20, 1])
    w_gu = dt_in("w_gu", [NE, D, 2 * DE])
    w_down = dt_in("w_down", [NE, DE, D])
    w_pg = dt_in("w_pg", [D, D])
    w_pp = dt_in("w_pp", [DPLE, D])
    ident = dt_in("ident", [128, 128])
    tri = dt_in("tri", [128, 128])
    tid = nc.dram_tensor("tid", [128, 16], I32, kind="ExternalInput").ap()
    out = nc.dram_tensor("out", [NT, D], F32, kind="ExternalOutput").ap()
    x1_dram = nc.dram_tensor("x1_spill", [NT, D], F32).ap()
    xh_dram = nc.dram_tensor("xh_spill", [NT, XHW], BF16).ap()
    inv_dram = nc.dram_tensor("inv_spill", [NT, 1], I32).ap()
    flags_dram = nc.dram_tensor("flags_spill", [1, 16], I32).ap()
    dbg = nc.dram_tensor("dbg", [128, 64], F32, kind="ExternalOutput").ap() if os.environ.get("K_DBG") else None

    w_in_v = w_in.rearrange("(dc p) f -> p dc f", p=128)
    w_out_v = w_out.rearrange("(dc p) f -> p dc f", p=128)
    w_pg_v = w_pg.rearrange("(dc p) f -> p dc f", p=128)
    w_pp_v = w_pp.rearrange("(dc p) f -> p dc f", p=128)
    w_r_v = w_r.rearrange("(dc p) f -> p dc f", p=128)

    with contextlib.ExitStack() as st:
        S = Sched(nc, st)

        def sb(stack, n, s, d):
            return stack.enter_context(nc.sbuf_tensor(n, s, d))

        banks = [st.enter_context(nc.psum_tensor("pb%d" % i, [128, 512], F32)) for i in range(8)]
        bank_ctr = [0]

        nbanks = [8]

        def nb():
            i = bank_ctr[0] % nbanks[0]
            bank_ctr[0] += 1
            return banks[i], "pb%d" % i

        def mm(o, l, r, start, stop, reads, writes):
            S.op("pe", lambda e: e.matmul(o, l, r, start=start, stop=stop), reads, writes)

        def tp(o, i_, idn, reads, writes):
            S.op("pe", lambda e: e.transpose(o, i_, idn), reads, writes)

        def act(o, i_, f, reads, writes, bias=0.0, scale=1.0):
            S.op("act", lambda e: e.activation(o, i_, f, bias=bias, scale=scale), reads, writes)

        def acp(o, i_, reads, writes):
            S.op("act", lambda e: e.copy(o, i_), reads, writes)

        def tt_(eng, o, a, b, op, reads, writes):
            S.op(eng, lambda e: e.tensor_tensor(o, a, b, op), reads, writes)

        def ts_(o, a, s1, s2, op0, op1, reads, writes):
            if s2 is None:
                S.op("dve", lambda e: e.tensor_scalar(o, a, s1, None, op0), reads, writes)
            else:
                S.op("dve", lambda e: e.tensor_scalar(o, a, s1, s2, op0, op1), reads, writes)

        def stt(o, a, sc, b, op0, op1, reads, writes):
            S.op("dve", lambda e: e.scalar_tensor_tensor(o, a, sc, b, op0, op1), reads, writes)

        def cp(eng, o, i_, reads, writes):
            S.op(eng, lambda e: e.tensor_copy(o, i_), reads, writes)

        def V(f, r, w):
            S.op("dve", f, r, w)

        def pipeline(stages, n):
            offs = [(off if callable(off) else (lambda i, off=off: off), fn) for off, fn in stages]
            last = max(i + off(i) for off, _ in offs for i in range(n))
            for t in range(last + 1):
                for off, fn in offs:
                    for i in range(n):
                        if i + off(i) == t:
                            fn(i)

        idf = sb(st, "idf", [128, 128], F32)
        idb = sb(st, "idb", [128, 128], BF16)
        ones_f = sb(st, "ones_f", [128, 128], F32)
        ones_rb = sb(st, "ones_rb", [1, 128], BF16)
        pps = sb(st, "pps", [128, PP_N], F32)
        brow_b = sb(st, "brow_b", [1, 2048], BF16)
        mv_in = sb(st, "mv_in", [128, 17, 2], F32)
        rs_in = sb(st, "rs_in", [128, 17], F32)
        ohg_all = sb(st, "ohg_all", [128, 16, 4], F32)
        inv_sb = sb(st, "inv_sb", [128, 16], I32)
        big = sb(st, "big", [128, 16384], F32)
        x1T = sb(st, "x1T", [128, 8, NT], BF16)
        wob = sb(st, "wob", [128, 8, 1024], BF16)

        def ppc(c, n=1):
            return pps[:, c:c + n]

        S.dma("sp", None, idf[:], ident, writes=["idf"])
        S.dma("sp", None, pps[:], pp, writes=["pps"])
        S.dma("pool", None, brow_b[:], rows[:, R_BOUT:R_BOUT + 2048], writes=["brow"])
        cp("dve", idb[:], idf[:], ["idf"], ["idb"])
        S.op("dve", lambda e: e.memset(ones_f[:], 1.0), (), ["ones_f"])
        S.op("dve", lambda e: e.memset(ones_rb[:], 1.0), (), ["ones_rb"])

        catT = big[:, 0:8192].bitcast(BF16).rearrange("p (c t) -> p c t", c=8)

        with contextlib.ExitStack() as s1:
            diag = x1T[:, :, :].rearrange("p c t -> p (c t)")[:, 0:4 * 31 * 128].rearrange(
                "p (c k j) -> p c k j", c=4, k=31)
            NWS = 3
            wslab = [sb(s1, "wslab%d" % i, [128, 8, 384], BF16) for i in range(NWS)]
            xcb = sb(s1, "xcb", [128, 4, 1024], BF16)
            xnT = [sb(s1, "xnT%d" % i, [128, 8, 512], BF16) for i in range(2)]
            xnTh = sb(s1, "xnTh", [128, 8, 32], BF16)
            xth = xcb[0:32, 0:2, :].rearrange("p a b -> p (a b)").bitcast(F32)
            xcbh = xcb[0:32, 2, :]
            sth = sb(s1, "sth", [128, 5, 2, 6], F32)
            glu = sb(s1, "glu", [128, 4, 544], BF16)
            cv = sb(s1, "cv", [128, 4, 514], F32)
            NTMP = 8
            tmp = [sb(s1, "tmp%d" % i, [128, 512], F32) for i in range(NTMP)]
            htmp = [sb(s1, "htmp%d" % i, [128, 32], F32) for i in range(4)]
            c_mean = sb(s1, "c_mean", [128, 512], F32)
            c_var = sb(s1, "c_var", [128, 512], F32)
            xt4 = big[:, 8192:12288].rearrange("p (j d) -> p j d", j=4)
            confy = big[:, 12288:14336].rearrange("p (c t) -> p c t", c=4)
            sq = big[:, 14336:16384].rearrange("p (c t) -> p c t", c=4)
            tctr = [0]

            def ntmp():
                i = tctr[0] % NTMP
                tctr[0] += 1
                return tmp[i], "tmp%d" % i

            def maybe_stop(tag):
                if stop_after == tag:
                    S.barrier()
                    S.emit()
                    return True
                return False

            def build_diag(c):
                S.op("dve", lambda e: e.tensor_tensor(
                    diag[:, c, :, :], idf[:].unsqueeze(1).to_broadcast([128, 31, 128]),
                    pps[:, PP_DWW + c * 31:PP_DWW + (c + 1) * 31].unsqueeze(2).to_broadcast([128, 31, 128]),
                    ALU.mult), ["idf", "pps"], ["diag%d" % c])

            S.dma("sp", "d_xh", xth, xs[0:HALO, :], writes=["xcb0", "xcb1"])
            V(lambda e: e.bn_stats(sth[0:32, 4, 0, :], xth[:, 0:512]), ["xcb0", "xcb1"], ["sth4a"])
            V(lambda e: e.bn_stats(sth[0:32, 4, 1, :], xth[:, 512:1024]), ["xcb0", "xcb1"], ["sth4b"])
            V(lambda e: e.bn_aggr(mv_in[0:32, 16, :], sth[0:32, 4, :, :]), ["sth4a", "sth4b"], ["mvh"])
            act(rs_in[0:32, 16:17], mv_in[0:32, 16, 1:2], AF.Sqrt, ["mvh"], ["rsh0"], bias=EPS)
            V(lambda e: e.reciprocal(rs_in[0:32, 16:17], rs_in[0:32, 16:17]), ["rsh0"], ["rsh"])
            ts_(xcbh, xth, mv_in[0:32, 16, 0:1], rs_in[0:32, 16:17], ALU.subtract, ALU.mult,
                ["xcb0", "xcb1", "mvh", "rsh"], ["xcb2"])
            bk, bkk = nb()
            bkb = bk[:].bitcast(BF16)
            for dc in range(8):
                tp(bkb[:, dc * 32:(dc + 1) * 32], xcbh[:, dc * 128:(dc + 1) * 128], idb[0:32, 0:32],
                   ["xcb2", "idb"], [bkk])
            for dc in range(8):
                act(xnTh[:, dc, :], bkb[:, dc * 32:(dc + 1) * 32], AF.Identity, [bkk, "pps"], ["xnTh"],
                    bias=ppc(PP_LNB + dc), scale=ppc(PP_LNG + dc))

            def stepA_load(tt, j):
                g = tt * 4 + j
                S.dma("sp", "d_xt%d" % j, xt4[:, j, :], xs[HALO + g * 128:HALO + (g + 1) * 128, :],
                      writes=["xt%d" % j])
                V(lambda e: e.bn_stats(sth[:, j, 0, :], xt4[:, j, 0:512]), ["xt%d" % j], ["sth%da" % j])
                V(lambda e: e.bn_stats(sth[:, j, 1, :], xt4[:, j, 512:1024]), ["xt%d" % j], ["sth%db" % j])
                V(lambda e: e.bn_aggr(mv_in[:, g, :], sth[:, j, :, :]), ["sth%da" % j, "sth%db" % j], ["mv%d" % g])

            def stepA_norm(tt):
                g0 = tt * 4
                act(rs_in[:, g0:g0 + 4], mv_in[:, g0:g0 + 4, 1], AF.Sqrt, ["mv%d" % (g0 + j) for j in range(4)],
                    ["rs0_%d" % tt], bias=EPS)
                V(lambda e: e.reciprocal(rs_in[:, g0:g0 + 4], rs_in[:, g0:g0 + 4]), ["rs0_%d" % tt], ["rs_%d" % tt])
                for j in range(4):
                    g = g0 + j
                    ts_(xcb[:, j, :], xt4[:, j, :], mv_in[:, g, 0:1], rs_in[:, g:g + 1], ALU.subtract, ALU.mult,
                        ["xt%d" % j, "mv%d" % g, "rs_%d" % tt], ["xcb%d" % j])

            def stepA_tr(tt, dcs):
                xn = xnT[tt % 2]
                xnk = "xnT%d" % (tt % 2)
                for dc in dcs:
                    bk, bkk = nb()
                    bkb = bk[:].bitcast(BF16)
                    for j in range(4):
                        tp(bkb[:, j * 128:(j + 1) * 128], xcb[:, j, dc * 128:(dc + 1) * 128], idb[:],
                           ["xcb%d" % j, "idb"], [bkk])
                    act(xn[:, dc, :], bkb[:, 0:512], AF.Identity, [bkk, "pps"], [xnk + "_%d" % dc],
                        bias=ppc(PP_LNB + dc), scale=ppc(PP_LNG + dc))

            def load_slab(s):
                if s >= 32:
                    return
                slot = s % NWS
                if s % 8 < 4:
                    c0, ncol = (s % 8) * 256, 256
                else:
                    c0, ncol = 1024 + (s % 8 - 4) * 384, 384
                S.dma("pool", "d_ws%d" % slot, wslab[slot][:, :, 0:ncol], w_in_v[:, :, c0:c0 + ncol],
                      writes=["ws%d" % slot])

            def conv_job(tt, i):
                bc_, bck = nb()
                for k in range(31):
                    mm(bc_[:, :], diag[:, i, k, :], glu[:, i, k + 2:k + 2 + 512], k == 0, k == 30,
                       ["diag%d" % i, "glu%d" % i], [bck])
                act(confy[:, i, :], bc_[:], AF.Identity, [bck, "pps"], ["confy%d" % i], bias=ppc(PP_DWB + i))
                act(sq[:, i, :], bc_[:], AF.Square, [bck, "pps"], ["sq%d" % i], bias=ppc(PP_DWB + i))
                if tt < 3:
                    cp("pool", glu[:, i, 0:32], glu[:, i, 512:544], ["glu%d" % i], ["glu%d" % i])

            def slab(tt, sl):
                s = tt * 8 + sl
                slot = s % NWS
                ws = wslab[slot]
                wsk = "ws%d" % slot
                xn = xnT[tt % 2]
                xn_reads = ["xnT%d_%d" % (tt % 2, dc) for dc in range(8)]
                tcols = slice(tt * 512, (tt + 1) * 512)

                def proj(coff):
                    bk_, bkk_ = nb()
                    for dc in range(8):
                        mm(bk_[:, :], ws[:, dc, coff:coff + 128], xn[:, dc, :], dc == 0, dc == 7,
                           [wsk] + xn_reads, [bkk_])
                    return bk_, bkk_

                def proj_h(coff, bank, bcol):
                    bk_, bkk_ = bank
                    for dc in range(8):
                        mm(bk_[:, bcol:bcol + 32], ws[:, dc, coff:coff + 128], xnTh[:, dc, :],
                           dc == 0, dc == 7, [wsk, "xnTh"], [bkk_])

                if sl < 4:
                    i = sl
                    bv, bvk = proj(0)
                    bg, bgk = proj(128)
                    if tt == 0:
                        hb = nb()
                        proj_h(0, hb, 0)
                        proj_h(128, hb, 32)
                    sig, sigk = ntmp()
                    act(sig[:], bg[:], AF.Sigmoid, [bgk, "pps"], [sigk], bias=ppc(PP_BIN + 2 * i + 1))
                    stt(glu[:, i, 32:544], bv[:], ppc(PP_BIN + 2 * i), sig[:], ALU.add, ALU.mult,
                        [bvk, sigk, "pps"], ["glu%d" % i])
                    if tt == 0:
                        act(htmp[0][:], hb[0][:, 32:64], AF.Sigmoid, [hb[1], "pps"], ["ht0"],
                            bias=ppc(PP_BIN + 2 * i + 1))
                        stt(htmp[1][:], hb[0][:, 0:32], ppc(PP_BIN + 2 * i), htmp[0][:], ALU.add, ALU.mult,
                            [hb[1], "ht0", "pps"], ["ht1"])
                        ts_(glu[:, i, 0:32], htmp[1][:], ppc(PP_MASK), None, ALU.mult, ALU.bypass,
                            ["ht1", "pps"], ["glu%d" % i])
                else:
                    i = sl - 4
                    cb_ = PP_BIN + 8 + 3 * i
                    bC, bCk = proj(0)
                    bV, bVk = proj(128)
                    bB, bBk = proj(256)
                    if tt == 0:
                        hb = nb()
                        proj_h(0, hb, 0)
                        proj_h(128, hb, 32)
                    vv, vvk = ntmp()
                    act(vv[:], bV[:], AF.Identity, [bVk, "pps"], [vvk], bias=ppc(cb_ + 1))
                    stt(cv[:, i, 2:514], bC[:], ppc(cb_), vv[:], ALU.add, ALU.mult, [bCk, vvk, "pps"], ["cv%d" % i])
                    if tt == 0:
                        act(htmp[2][:], hb[0][:, 32:64], AF.Identity, [hb[1], "pps"], ["ht2"], bias=ppc(cb_ + 1))
                        stt(htmp[3][:], hb[0][:, 0:32], ppc(cb_), htmp[2][:], ALU.add, ALU.mult,
                            [hb[1], "ht2", "pps"], ["ht3"])
                        ts_(cv[:, i, 0:2], htmp[3][:, 30:32], ppc(PP_MASK), None, ALU.mult, ALU.bypass,
                            ["ht3", "pps"], ["cv%d" % i])
                    tc, tck = ntmp()
                    w0 = PP_SCW + i * 3
                    ts_(tc[:], cv[:, i, 2:514], ppc(w0 + 2), ppc(PP_SCB + i), ALU.mult, ALU.add,
                        ["cv%d" % i, "pps"], [tck])
                    stt(tc[:], cv[:, i, 1:513], ppc(w0 + 1), tc[:], ALU.mult, ALU.add, ["cv%d" % i, "pps", tck], [tck])
                    stt(tc[:], cv[:, i, 0:512], ppc(w0), tc[:], ALU.mult, ALU.add, ["cv%d" % i, "pps", tck], [tck])
                    stt(catT[:, 4 + i, tcols], bB[:], ppc(cb_ + 2), tc[:], ALU.add, ALU.mult,
                        [bBk, tck, "pps"], ["cat%d_%d" % (4 + i, tt)])
                    if tt < 3:
                        cp("pool", cv[:, i, 0:2], cv[:, i, 512:514], ["cv%d" % i], ["cv%d" % i])

            cst = {}

            def stepC1(tt):
                b1_, b1k = nb()
                b2_, b2k = nb()
                for i in range(4):
                    mm(b1_[:, :], ones_f[:], confy[:, i, :], i == 0, i == 3, ["ones_f", "confy%d" % i], [b1k])
                for i in range(4):
                    mm(b2_[:, :], ones_f[:], sq[:, i, :], i == 0, i == 3, ["ones_f", "sq%d" % i], [b2k])
                mean, meank = c_mean, "c_mean"
                m2, m2k = ntmp()
                var, vark = c_var, "c_var"
                ts_(mean[:], b1_[:], 1.0 / 512, None, ALU.mult, ALU.bypass, [b1k], [meank])
                tt_("pool", m2[:], mean[:], mean[:], ALU.mult, [meank], [m2k])
                stt(var[:], b2_[:], 1.0 / 512, m2[:], ALU.mult, ALU.subtract, [b2k, m2k], [vark])
                act(var[:], var[:], AF.Sqrt, [vark], [vark], bias=EPS)
                V(lambda e: e.reciprocal(var[:], var[:]), [vark], [vark])
                cst[tt] = (mean, meank, var, vark, [(confy[:, i, :], "confy%d" % i) for i in range(4)],
                           [(sq[:, i, :], "sq%d" % i) for i in range(4)])

            def stepC2(tt):
                mean, meank, var, vark, tas, sgs = cst[tt]
                for i in range(4):
                    ta, tak = tas[i]
                    tt_("pool", ta, confy[:, i, :], mean[:], ALU.subtract, ["confy%d" % i, meank], [tak])
                for i in range(4):
                    ta, tak = tas[i]
                    tt_("dve", ta, ta, var[:], ALU.mult, [tak, vark], [tak])

            def stepC3(tt):
                tcols = slice(tt * 512, (tt + 1) * 512)
                mean, meank, var, vark, tas, sgs = cst[tt]
                for i in range(4):
                    ta, tak = tas[i]
                    sg, sgk = sgs[i]
                    act(sg, ta, AF.Sigmoid, [tak, "pps"], [sgk], bias=ppc(PP_CLB + i), scale=ppc(PP_CLG + i))
                for i in range(4):
                    ta, tak = tas[i]
                    sg, sgk = sgs[i]
                    ts_(ta, ta, ppc(PP_CLG + i), ppc(PP_CLB + i), ALU.mult, ALU.add, [tak, "pps", sgk], [tak])
                for i in range(4):
                    ta, tak = tas[i]
                    sg, sgk = sgs[i]
                    tt_("pool", catT[:, i, tcols], ta, sg, ALU.mult, [tak, sgk], ["cat%d_%d" % (i, tt)])

            load_slab(0)
            load_slab(1)
            for j in range(4):
                stepA_load(0, j)
            stepA_norm(0)
            stepA_tr(0, range(8))
            for tt in range(4):
                pend = None
                for sl in range(8):
                    load_slab(tt * 8 + sl + 2)
                    if tt == 0 and sl < 4:
                        build_diag(sl)
                    slab(tt, sl)
                    if pend is not None:
                        conv_job(*pend)
                    pend = (tt, sl) if sl < 4 else None
                    if tt == 1 and sl == 0:
                        S.dma("pool", "d_wob", wob[:], w_out_v, writes=["wob"])
                    if tt < 3:
                        if sl < 4:
                            stepA_load(tt + 1, sl)
                        elif sl == 4:
                            stepA_norm(tt + 1)
                        elif sl == 6:
                            stepA_tr(tt + 1, range(0, 4))
                        elif sl == 7:
                            stepA_tr(tt + 1, range(4, 8))
                    if sl == 5:
                        stepC1(tt)
                    elif sl == 6:
                        stepC2(tt)
                    elif sl == 7:
                        stepC3(tt)
            S.barrier()
            S.emit()
        if stop_after == "1a":
            return nc

        combT = sb(st, "combT", [48, NT], BF16)
        sM = st.enter_context(contextlib.ExitStack())
        NSLOT = 3
        wgu = [None] * NSLOT
        NWD = 4
        wd = [None] * NWD
        wgu[0] = sb(sM, "wgu0", [128, 8, 2 * DE], BF16)
        wd[0] = sb(sM, "wd0", [128, 2, D], BF16)

        def load_expert(e_):
            slot = e_ % NSLOT
            S.dma("pool", "d_mwgu%d" % slot, wgu[slot][:], w_gu[e_].rearrange("(dc p) f -> p dc f", p=128), writes=["wgu%d" % slot])
            S.dma("pool", "d_mwd%d" % (e_ % NWD), wd[e_ % NWD][:], w_down[e_].rearrange("(fc p) d -> p fc d", p=128),
                  writes=["wd%d" % (e_ % NWD)])

        with contextlib.ExitStack() as s2:
            bc_ag = sb(s2, "bc_ag", [128, 1024], F32)
            bc_g1 = sb(s2, "bc_g1", [128, 1024], F32)
            bc_b1 = sb(s2, "bc_b1", [128, 1024], F32)
            NH = 3
            hib = [sb(s2, "hib%d" % i, [128, 1024], BF16) for i in range(NH)]
            lob = [sb(s2, "lob%d" % i, [128, 1024], BF16) for i in range(NH)]
            x1loT = [sb(s2, "x1loT%d" % i, [128, 8, 512], BF16) for i in range(2)]
            r1x = sb(s2, "r1x", [128, 4, 1024], F32)
            wr_f = sb(s2, "wr_f", [128, 8, 20], F32)
            wr_hi = sb(s2, "wr_hi", [128, 8, 20], BF16)
            wr_lo = sb(s2, "wr_lo", [128, 8, 20], BF16)
            br_sb = sb(s2, "br_sb", [20, 1], F32)
            lgT = sb(s2, "lgT", [20, 512], F32)
            lg = sb(s2, "lg", [128, 4, 20], F32)
            chl = sb(s2, "chl", [128, 4, 48], BF16)
            osum = sb(s2, "osum", [128, 4, 1], F32)
            onm = sb(s2, "onm", [128, 4, 1], F32)
            tri_sb = sb(s2, "tri_sb", [128, 128], F32)
            tid_sb = sb(s2, "tid_sb", [128, 16], I32)
            Wt = sb(s2, "Wt", [128, 16, 4], F32)
            Tt = sb(s2, "Tt", [128, 16, 4], F32)
            Et = sb(s2, "Et", [128, 16, 4], F32)
            cntv = sb(s2, "cntv", [128, 4], F32)
            startv = sb(s2, "startv", [128, 4], F32)
            hiv = sb(s2, "hiv", [128, 4], F32)
            f1 = sb(s2, "f1", [128, 4], F32)
            f2 = sb(s2, "f2", [128, 4], F32)
            posf = sb(s2, "posf", [128, 16], F32)
            posi = sb(s2, "posi", [128, 16], I32)
            flagsf = sb(s2, "flagsf", [128, 4, 4], F32)
            flagsi = sb(s2, "flagsi", [128, 16], I32)
            bitsf = sb(s2, "bitsf", [128, 1], F32)
            st1 = sb(s2, "st1", [128, 16, 2, 6], F32)
            mv1 = sb(s2, "mv1", [128, 16, 2], F32)
            rs1 = sb(s2, "rs1", [128, 16], F32)
            nb1 = sb(s2, "nb1", [128, 16], F32)
            nb_in = sb(s2, "nb_in", [128, 16], F32)
            brow_hl = sb(s2, "brow_hl", [64, 1024], BF16)
            ones64 = sb(s2, "ones64", [64, 128], BF16)
            r_sm = [sb(s2, "rsm%d" % i, [128, 4, 16], F32) for i in range(6)]
            r_s4 = [sb(s2, "rs4_%d" % i, [128, 4, 4], F32) for i in range(4)]
            r_s1 = [sb(s2, "rs1_%d" % i, [128, 4], F32) for i in range(8)]
            r1b = big[:, 8192:12288].rearrange("p (j d) -> p j d", j=4)
            xrb = big[:, 12288:16384].rearrange("p (j d) -> p j d", j=4)

            def r1v(g):
                s_ = g % 8
                return (r1b[:, s_, :] if s_ < 4 else r1x[:, s_ - 4, :]), "r1s%d" % s_

            S.dma("sp", None, bc_ag[:], rows[:, R_LNIG:R_LNIG + 1024].partition_broadcast(128), writes=["bc_ag"])
            S.dma("sp", None, bc_g1[:], rows[:, R_L1G:R_L1G + 1024].partition_broadcast(128), writes=["bc_g1"])
            S.dma("sp", None, bc_b1[:], rows[:, R_L1B:R_L1B + 1024].partition_broadcast(128), writes=["bc_b1"])
            S.dma("sp", None, wr_f[:], w_r_v, writes=["wr_f"])
            S.dma("sp", None, br_sb[:], b_r, writes=["br_sb"])
            ts_(bc_ag[:], bc_ag[:], ALPHA, None, ALU.mult, ALU.bypass, ["bc_ag"], ["bc_ag"])
            cp("dve", wr_hi[:], wr_f[:], ["wr_f"], ["wr_hi"])
            tt_("dve", wr_lo[:], wr_f[:], wr_hi[:], ALU.subtract, ["wr_f", "wr_hi"], ["wr_lo"])
            load_expert(0)
            V(lambda e: e.memset(chl[:], 0.0), [], ["chl_z"])
            V(lambda e: e.scalar_tensor_tensor(nb_in[:], mv_in[:, 0:16, 0], -1.0, rs_in[:, 0:16], ALU.mult, ALU.mult),
              [], ["nb_in"])

            mixb = {}
            V(lambda e: e.memset(brow_hl[:], 0.0), [], ["brow_hl"])
            V(lambda e: e.memset(ones64[:], 1.0), [], ["ones64"])
            for pb_ in (0, 32):
                f_ = xrb[pb_:pb_ + 1, 2, :]
                f2_ = xrb[pb_:pb_ + 1, 3, :]
                S.dma("sp", None, f_, rows[:, R_BOUT:R_BOUT + 1024], writes=["xr2"])
                S.dma("sp", None, f2_, rows[:, R_LNIB:R_LNIB + 1024], writes=["xr3"])
                stt(f_, f2_, ALPHA, f_, ALU.mult, ALU.add, ["xr2", "xr3"], ["xr2"])
                cp("dve", brow_hl[pb_:pb_ + 1, :], f_, ["xr2", "brow_hl"], ["brow_hl"])
                if pb_ == 32:
                    tt_("dve", brow_hl[32:33, :], f_, brow_hl[32:33, :], ALU.subtract, ["xr2", "brow_hl"], ["brow_hl"])

            def b_dma(g):
                S.dma("sp", "d_xr%d" % (g % 4), xrb[:, g % 4, :], xs[HALO + g * 128:HALO + (g + 1) * 128, :],
                      writes=["xr%d" % (g % 4)])

            def b_xn(g):
                xk = "xr%d" % (g % 4)
                xr = xrb[:, g % 4, :]
                act(xr, xr, AF.Identity, [xk, "nb_in"], [xk], bias=nb_in[:, g:g + 1], scale=rs_in[:, g:g + 1])

            def b_mix(g):
                tt, j = divmod(g, 4)
                gc = slice(g * 128, (g + 1) * 128)
                xk = "xr%d" % (g % 4)
                xr = xrb[:, g % 4, :]
                tt_("pool", xr, xr, bc_ag[:], ALU.mult, [xk, "bc_ag"], [xk])
                pbk = []
                for h in range(2):
                    hs = slice(h * 512, (h + 1) * 512)
                    bk, bkk = nb()
                    for c in range(8):
                        mm(bk[:, :], catT[:, c, gc], wob[:, c, hs], c == 0, False, ["cat%d_%d" % (c, tt), "wob"], [bkk])
                    mm(bk[:, :], ones64[:, :], brow_hl[:, hs], False, True, ["ones64", "brow_hl"], [bkk])
                    pbk.append((bk, bkk))
                mixb[g] = pbk

            def b_r1(g):
                xk = "xr%d" % (g % 4)
                xr = xrb[:, g % 4, :]
                r1, r1k = r1v(g)
                pbk = mixb.pop(g)
                for h in range(2):
                    hs = slice(h * 512, (h + 1) * 512)
                    tt_("dve", r1[:, hs], xr[:, hs], pbk[h][0][:], ALU.add, [xk, pbk[h][1]], [r1k + "_%d" % h])
                V(lambda e: e.bn_stats(st1[:, g, 0, :], r1[:, 0:512]), [r1k + "_0"], ["st1a%d" % g])
                V(lambda e: e.bn_stats(st1[:, g, 1, :], r1[:, 512:1024]), [r1k + "_1"], ["st1b%d" % g])
                V(lambda e: e.bn_aggr(mv1[:, g, :], st1[:, g, :, :]), ["st1a%d" % g, "st1b%d" % g], ["mv1_%d" % g])

            def b_sqrt(g):
                act(rs1[:, g:g + 1], mv1[:, g, 1:2], AF.Sqrt, ["mv1_%d" % g], ["rs1a%d" % g], bias=EPS)

            def b_rcp(g):
                V(lambda e: e.reciprocal(rs1[:, g:g + 1], rs1[:, g:g + 1]), ["rs1a%d" % g], ["rs1_%d" % g])
                V(lambda e: e.scalar_tensor_tensor(nb1[:, g:g + 1], mv1[:, g, 0:1], -1.0, rs1[:, g:g + 1],
                                                   ALU.mult, ALU.mult), ["rs1_%d" % g, "mv1_%d" % g], ["nb1_%d" % g])

            def b_n1(g):
                r1, r1k = r1v(g)
                rk = [r1k + "_0", r1k + "_1"]
                act(r1, r1, AF.Identity, rk + ["nb1_%d" % g, "rs1_%d" % g], rk, bias=nb1[:, g:g + 1], scale=rs1[:, g:g + 1])

            def b_g1(g):
                r1, r1k = r1v(g)
                rk = [r1k + "_0", r1k + "_1"]
                tt_("pool", r1, r1, bc_g1[:], ALU.mult, rk + ["bc_g1"], rk)

            def b_b1(g):
                r1, r1k = r1v(g)
                rk = [r1k + "_0", r1k + "_1"]
                tt_("dve", r1, r1, bc_b1[:], ALU.add, rk + ["bc_b1"], rk)
                S.dma("sp", "d_x1s%d" % (g % 8), x1_dram[g * 128:(g + 1) * 128, :], r1, reads=rk, writes=["x1d%d" % g])

            def b_hi(g):
                r1, r1k = r1v(g)
                rk = [r1k + "_0", r1k + "_1"]
                acp(hib[g % NH][:], r1, rk, ["hib%d" % (g % NH)])
                S.dma("sp", "d_xh%d" % (g % NH), xh_dram[g * 128:(g + 1) * 128, 0:1024], hib[g % NH][:],
                      reads=["hib%d" % (g % NH)], writes=["xhd%d" % g])

            def b_lo(g):
                r1, r1k = r1v(g)
                rk = [r1k + "_0", r1k + "_1"]
                hb_, lb_ = hib[g % NH], lob[g % NH]
                hk, lk = "hib%d" % (g % NH), "lob%d" % (g % NH)
                tt_("dve", lb_[:], r1, hb_[:], ALU.subtract, rk + [hk], [lk])
                bk, bkk = nb()
                bkb = bk[:].bitcast(BF16)
                for dc in range(8):
                    tp(bkb[:, dc * 128:(dc + 1) * 128], hb_[:, dc * 128:(dc + 1) * 128], idb[:], [hk, "idb"], [bkk])
                mixb[("h", g)] = (bkb, bkk)

            def b_x1T(g):
                gc = slice(g * 128, (g + 1) * 128)
                lb_ = lob[g % NH]
                lk = "lob%d" % (g % NH)
                bkb, bkk = mixb.pop(("h", g))
                acp(x1T[:, :, gc], bkb.rearrange("p (c t) -> p c t", c=8), [bkk], ["x1T_%d" % g])
                bk2, bkk2 = nb()
                bkb2 = bk2[:].bitcast(BF16)
                for dc in range(8):
                    tp(bkb2[:, dc * 128:(dc + 1) * 128], lb_[:, dc * 128:(dc + 1) * 128], idb[:], [lk, "idb"], [bkk2])
                mixb[("l", g)] = (bkb2, bkk2)

            def b_x1lo(g):
                tt, j = divmod(g, 4)
                bkb2, bkk2 = mixb.pop(("l", g))
                acp(x1loT[tt % 2][:, :, j * 128:(j + 1) * 128], bkb2.rearrange("p (c t) -> p c t", c=8),
                    [bkk2], ["x1lo%d_%d" % (tt % 2, j)])

            gmax, gsum, gp, m1, m2_, d21, w1, w2 = [r_s1[i] for i in range(8)]
            _unused, dg, exg, pen = [r_s4[i] for i in range(4)]
            lem, oh1, lem2, oh2, cA, cB = [r_sm[i] for i in range(6)]
            lgG = lg[:, :, 0:4]
            lgE4 = lg[:, :, 4:20].rearrange("p a (g e) -> p a g e", g=4)
            bc4 = lambda a: a[:].unsqueeze(2).to_broadcast([128, 4, 4])
            bc16 = lambda a: a[:].unsqueeze(2).to_broadcast([128, 4, 16])

            def r_0(tt):
                tcols = slice(tt * 512, (tt + 1) * 512)
                x1k = ["x1T_%d" % (tt * 4 + j) for j in range(4)]
                lok = ["x1lo%d_%d" % (tt % 2, j) for j in range(4)]
                br_, brk = nb()
                n = 0
                for (wt, wk, xsrc, xk) in ((wr_hi, "wr_hi", "hi", x1k), (wr_hi, "wr_hi", "lo", lok), (wr_lo, "wr_lo", "hi", x1k)):
                    for dc in range(8):
                        rhs = x1T[:, dc, tcols] if xsrc == "hi" else x1loT[tt % 2][:, dc, :]
                        mm(br_[0:20, :], wt[:, dc, :], rhs, n == 0, n == 23, [wk] + xk, [brk])
                        n += 1
                act(lgT[:], br_[0:20, :], AF.Identity, [brk, "br_sb"], ["lgT"], bias=br_sb[:, 0:1])
                bt_, btk = nb()
                for j in range(4):
                    tp(bt_[:, j * 20:(j + 1) * 20], lgT[:, j * 128:(j + 1) * 128], idf[0:20, 0:20], ["lgT", "idf"], [btk])
                cp("dve", lg[:].rearrange("p a b -> p (a b)"), bt_[:, 0:80], [btk], ["lg"])

            def r_1(tt):
                ohg = ohg_all[:, tt * 4:(tt + 1) * 4, :]
                V(lambda e: e.tensor_reduce(gmax[:], lgG, AX.X, ALU.max), ["lg"], ["gmax"])
                V(lambda e: e.tensor_tensor(ohg, lgG, bc4(gmax), ALU.is_equal), ["lg", "gmax"], ["ohg"])
                V(lambda e: e.tensor_copy(osum[:], ohg[:, :, 0:1]), ["ohg"], ["osum"])
                for g_ in range(1, 4):
                    V(lambda e: e.tensor_scalar(onm[:], osum[:], -1.0, 1.0, ALU.mult, ALU.add), ["osum"], ["onm"])
                    V(lambda e, g_=g_: e.tensor_tensor(ohg[:, :, g_:g_ + 1], ohg[:, :, g_:g_ + 1], onm[:], ALU.mult), ["ohg", "onm"], ["ohg"])
                    if g_ < 3:
                        V(lambda e, g_=g_: e.tensor_tensor(osum[:], osum[:], ohg[:, :, g_:g_ + 1], ALU.add), ["osum", "ohg"], ["osum"])
                V(lambda e: e.tensor_tensor(dg[:], lgG, bc4(gmax), ALU.subtract), ["lg", "gmax"], ["dg"])
                act(exg[:], dg[:], AF.Exp, ["dg"], ["exg"])
                V(lambda e: e.tensor_scalar(pen[:], ohg, BIG, -BIG, ALU.mult, ALU.add), ["ohg"], ["pen"])
                V(lambda e: e.tensor_tensor(lem[:].rearrange("p a (g e) -> p a g e", g=4), lgE4,
                                            pen[:].unsqueeze(3).to_broadcast([128, 4, 4, 4]), ALU.add), ["lg", "pen"], ["lem"])
                V(lambda e: e.tensor_reduce(m1[:], lem[:], AX.X, ALU.max), ["lem"], ["m1"])
                V(lambda e: e.tensor_tensor(oh1[:], lem[:], bc16(m1), ALU.is_equal), ["lem", "m1"], ["oh1"])
                V(lambda e: e.scalar_tensor_tensor(lem2[:], oh1[:], -BIG, lem[:], ALU.mult, ALU.add), ["oh1", "lem"], ["lem2"])
                V(lambda e: e.tensor_reduce(m2_[:], lem2[:], AX.X, ALU.max), ["lem2"], ["m2"])
                V(lambda e: e.tensor_tensor(oh2[:], lem2[:], bc16(m2_), ALU.is_equal), ["lem2", "m2"], ["oh2"])
                V(lambda e: e.tensor_tensor(d21[:], m2_[:], m1[:], ALU.subtract), ["m1", "m2"], ["d21"])
                act(d21[:], d21[:], AF.Exp, ["d21"], ["d21"])

            def r_2(tt):
                tcols = slice(tt * 512, (tt + 1) * 512)
                V(lambda e: e.tensor_reduce(gsum[:], exg[:], AX.X, ALU.add), ["exg"], ["gsum"])
                V(lambda e: e.reciprocal(gp[:], gsum[:]), ["gsum"], ["gp"])
                V(lambda e: e.tensor_scalar(d21[:], d21[:], 1.0, None, ALU.add), ["d21"], ["d21"])
                V(lambda e: e.reciprocal(w1[:], d21[:]), ["d21"], ["w1"])
                V(lambda e: e.tensor_scalar(w2[:], w1[:], -1.0, 1.0, ALU.mult, ALU.add), ["w1"], ["w2"])
                V(lambda e: e.tensor_tensor(w1[:], w1[:], gp[:], ALU.mult), ["w1", "gp"], ["w1"])
                V(lambda e: e.tensor_tensor(w2[:], w2[:], gp[:], ALU.mult), ["w2", "gp"], ["w2"])
                V(lambda e: e.tensor_tensor(cA[:], oh1[:], bc16(w1), ALU.mult), ["oh1", "w1"], ["cA"])
                V(lambda e: e.tensor_tensor(cB[:], oh2[:], bc16(w2), ALU.mult), ["oh2", "w2"], ["cB"])
                V(lambda e: e.tensor_tensor(cA[:], cA[:], cB[:], ALU.add), ["cA", "cB"], ["cA"])
                V(lambda e: e.tensor_copy(chl[:, :, 0:16], cA[:]), ["cA"], ["chl_hi"])
                V(lambda e: e.tensor_tensor(chl[:, :, 32:48], cA[:], chl[:, :, 0:16], ALU.subtract), ["cA", "chl_hi"], ["chl_lo"])
                S.dma("sp", "d_chl", xh_dram[tt * 512:(tt + 1) * 512, 1024:XHW].rearrange("(j p) c -> p j c", p=128),
                      chl[:], reads=["chl_hi", "chl_lo", "chl_z"], writes=["chld%d" % tt])

            def grp(fn, off):
                return (off, lambda g: fn(g // 4) if g % 4 == 3 else None)

            pipeline([(0, b_dma), (1, b_xn), (2, b_mix), (3, b_r1), (4, b_sqrt), (5, b_rcp), (6, b_n1), (7, b_g1),
                      (8, b_b1), (9, b_hi), (10, b_lo), (11, b_x1T), (12, b_x1lo),
                      grp(r_0, 13), grp(r_1, 14), grp(r_2, 15)], 16)

            oflat = ohg_all[:].rearrange("p s g -> p (s g)")
            S.dma("sp", None, tri_sb[:], tri, writes=["tri_sb"])
            S.dma("sp", None, tid_sb[:], tid, writes=["tid_sb"])
            bw, bwk = nb()
            mm(bw[:, 0:64], tri_sb[:], oflat, True, True, ["tri_sb", "ohg"], [bwk])
            bt, btk = nb()
            mm(bt[:, 0:64], ones_f[:], oflat, True, True, ["ones_f", "ohg"], [btk])
            cp("dve", Wt[:].rearrange("p s g -> p (s g)"), bw[:, 0:64], [bwk], ["Wt"])
            cp("dve", Tt[:].rearrange("p s g -> p (s g)"), bt[:, 0:64], [btk], ["Tt"])
            V(lambda e: e.memset(Et[:, 0, :], 0.0), [], ["Et"])
            for s_ in range(1, 16):
                V(lambda e, s_=s_: e.tensor_tensor(Et[:, s_, :], Et[:, s_ - 1, :], Tt[:, s_ - 1, :], ALU.add), ["Et", "Tt"], ["Et"])
            V(lambda e: e.tensor_tensor(cntv[:], Et[:, 15, :], Tt[:, 15, :], ALU.add), ["Et", "Tt"], ["cntv"])
            V(lambda e: e.memset(startv[:, 0:1], 0.0), [], ["startv"])
            for g_ in range(1, 4):
                V(lambda e, g_=g_: e.tensor_tensor(startv[:, g_:g_ + 1], startv[:, g_ - 1:g_], cntv[:, g_ - 1:g_], ALU.add),
                  ["startv", "cntv"], ["startv"])
            V(lambda e: e.tensor_tensor(Wt[:], Wt[:], Et[:], ALU.add), ["Wt", "Et"], ["Wt"])
            V(lambda e: e.tensor_tensor(Wt[:], Wt[:], startv[:].unsqueeze(1).to_broadcast([128, 16, 4]), ALU.add),
              ["Wt", "startv"], ["Wt"])
            V(lambda e: e.tensor_tensor(Wt[:], Wt[:], ohg_all[:], ALU.mult), ["Wt", "ohg"], ["Wt"])
            V(lambda e: e.tensor_reduce(posf[:], Wt[:], AX.X, ALU.add), ["Wt"], ["posf"])
            V(lambda e: e.tensor_copy(posi[:], posf[:]), ["posf"], ["posi"])
            for s_ in range(16):
                S.dma_fn("pool", "d_inv", lambda eng, s_=s_: eng.indirect_dma_start(
                    out=inv_dram[:, :], out_offset=bass.IndirectOffsetOnAxis(ap=posi[:, s_:s_ + 1], axis=0),
                    in_=tid_sb[:, s_:s_ + 1], in_offset=None), reads=["posi", "tid_sb"], writes=["inv_dram%d" % s_])
            for q in range(16):
                S.dma("sp" if q % 2 == 0 else "act", "d_invl%d" % (q % 2), inv_sb[:, q:q + 1], inv_dram[q * 128:(q + 1) * 128, :],
                      reads=["inv_dram%d" % s_ for s_ in range(16)], writes=["inv_sb%d" % q])
            V(lambda e: e.tensor_tensor(hiv[:], startv[:], cntv[:], ALU.add), ["startv", "cntv"], ["hiv"])
            for tt in range(4):
                V(lambda e, tt=tt: e.tensor_scalar(f1[:], startv[:], float((tt + 1) * 512), None, ALU.is_lt), ["startv"], ["f1"])
                V(lambda e, tt=tt: e.tensor_scalar(f2[:], hiv[:], float(tt * 512), None, ALU.is_gt), ["hiv"], ["f2"])
                V(lambda e, tt=tt: e.tensor_tensor(flagsf[:, tt, :], f1[:], f2[:], ALU.mult), ["f1", "f2"], ["flagsf"])
            V(lambda e: e.memset(bitsf[:], 0.0), [], ["bitsf"])
            for k in range(16):
                V(lambda e, k=k: e.scalar_tensor_tensor(bitsf[:], flagsf[:, k // 4, k % 4:k % 4 + 1], float(2 ** k), bitsf[:],
                                                        ALU.mult, ALU.add), ["flagsf", "bitsf"], ["bitsf"])
            if os.environ.get("K_ALLFLAGS"):
                V(lambda e: e.memset(bitsf[:], 65535.0), ["bitsf"], ["bitsf"])
            V(lambda e: e.tensor_copy(flagsi[:, 0:1], bitsf[:]), ["bitsf"], ["flagsi"])
            S.dma("sp", None, flags_dram[0:1, 0:1], flagsi[0:1, 0:1], reads=["flagsi"], writes=["flags"])
            if dbg is not None:
                S.dma("sp", None, dbg[:, 0:16], flagsf[:].rearrange("p a b -> p (a b)"), reads=["flagsf"], writes=["dbg0"])
                S.dma("sp", None, dbg[:, 16:18], flagsi[:, 0:2].bitcast(F32), reads=["flagsi"], writes=["dbg1"])
                S.dma("sp", None, dbg[:, 20:24], startv[:], reads=["startv"], writes=["dbg2"])
                S.dma("sp", None, dbg[:, 24:28], cntv[:], reads=["cntv"], writes=["dbg3"])
                S.dma("sp", None, dbg[:, 32:48], posf[:], reads=["posf"], writes=["dbg4"])
            S.barrier()
            S.emit()
        if stop_after == "1b":
            return nc

        yacc = big[:, :].rearrange("p (g d) -> p g d", g=16)
        with contextlib.ExitStack() as s3:
            for i in range(1, NSLOT):
                wgu[i] = sb(s3, "wgu%d" % i, [128, 8, 2 * DE], BF16)
            for i in range(1, NWD):
                wd[i] = sb(s3, "wd%d" % i, [128, 2, D], BF16)
            NHID = 6
            hid_r = [sb(s3, "hidr%d" % i, [128, 2, 512], BF16) for i in range(2)]
            sel = sb(s3, "sel", [48, NE, 128], BF16)
            scr = sb(s3, "scr", [1, 512], F32)
            scrp = sb(s3, "scrp", [128, 160], F32)
            NXG = 6
            xg = [sb(s3, "xg%d" % i, [128, XHW], BF16) for i in range(NXG)]
            hid = [xg[i][:, 0:1024].rearrange("p (f t) -> p f t", f=2) for i in range(NXG)]
            hid = hid + hid_r
            hidk = lambda k: ["hid%d" % k, "xg%d" % k] if k < NXG else ["hid%d" % k]
            cbs = [sb(s3, "cbs%d" % i, [128, 512], F32) for i in range(2)]
            NSS = 3
            sbuf_s = [sb(s3, "mss%d" % i, [128, 512], F32) for i in range(NSS)]
            sbuf_t = [sb(s3, "mst%d" % i, [128, 512], F32) for i in range(NSS)]

            V(lambda e: e.memset(sel[:], 0.0), [], ["sel"])
            V(lambda e: e.tensor_copy(sel[0:16, :, :], idf[0:16, 0:16].unsqueeze(2).to_broadcast([16, NE, 128])),
              ["idf", "sel"], ["sel"])
            V(lambda e: e.tensor_copy(sel[32:48, :, :], idf[32:48, 32:48].unsqueeze(2).to_broadcast([16, NE, 128])),
              ["idf", "sel"], ["sel"])
            nbanks[0] = 7
            S.op("dve", lambda e: e.memset(scr[:], 0.0), [], ["scr"])
            S.dummy["pe"] = lambda eng, i: eng.matmul(banks[7][0:1, i:i + 1], ones_rb[0:1, 0:1], ones_rb[0:1, 0:1], start=True, stop=True)
            S.dummy["act"] = lambda eng, i: eng.copy(scr[0:1, 384 + i:385 + i], scr[0:1, 511:512])
            S.dummy["dve"] = lambda eng, i: eng.memset(scr[0:1, i:i + 1], 0.0)
            S.dummy["pool"] = lambda eng, i: eng.memset(scrp[:, i:i + 1], 0.0)

            def load_flags(e):
                def f(eng):
                    r = eng.alloc_register("fl_%s" % e)
                    eng.reg_load(r, flags_dram[0:1, 0:1])
                    S.regs[e] = eng.snap(r)
                    return None
                return f
            for e in ("pe", "act", "dve", "pool"):
                S.raw(e, load_flags(e))

            S.op("dve", lambda e: e.memset(big[:, 0:8192], 0.0), [], ["yz0"])


            def gather_q(q):
                xg_ = xg[q % NXG]
                S.dma_fn("pool", "d_xg%d" % (q % NXG), lambda eng: eng.indirect_dma_start(
                    out=xg_[:, :], out_offset=None, in_=xh_dram[:, :],
                    in_offset=bass.IndirectOffsetOnAxis(ap=inv_sb[:, q:q + 1], axis=0)), reads=[],
                    writes=["xg%d" % (q % NXG), "hid%d" % (q % NXG)])

            def trans_q(q):
                qc = slice(q * 128, (q + 1) * 128)
                xg_ = xg[q % NXG]
                xgk = "xg%d" % (q % NXG)
                bk, bkk = nb()
                bkb = bk[:].bitcast(BF16)
                for dc in range(8):
                    tp(bkb[:, dc * 128:(dc + 1) * 128], xg_[:, dc * 128:(dc + 1) * 128], idb[:], [xgk, "idb"], [bkk])
                acp(x1T[:, :, qc], bkb.rearrange("p (c t) -> p c t", c=8), [bkk], ["x1T_%d" % q])
                bk2, bkk2 = nb()
                bkb2 = bk2[:].bitcast(BF16)
                tp(bkb2[0:48, 0:128], xg_[:, 1024:XHW], idb[:], [xgk, "idb"], [bkk2])
                cp("dve", combT[:, qc], bkb2[0:48, 0:128], [bkk2], ["combT_%d" % q])

            for q in range(NXG):
                gather_q(q)
            load_expert(1)
            S.op("pool", lambda e: e.memset(big[:, 8192:16384], 0.0), [], ["yz1"])

            steps = [(e_, tt) for e_ in range(NE) for tt in range(4)]
            pend_b = {}
            uctr = 0

            def down(e_, tt, hslot):
                slot = e_ % NWD
                for j in range(4):
                    g = tt * 4 + j
                    for h in range(2):
                        bk, bkk = nb()
                        for fc in range(2):
                            mm(bk[:, :], hid[hslot][:, fc, j * 128:(j + 1) * 128], wd[slot][:, fc, h * 512:(h + 1) * 512],
                               fc == 0, fc == 1, hidk(hslot) + ["wd%d" % slot], [bkk])
                        ysl = yacc[:, g, h * 512:(h + 1) * 512]
                        yk = "y_%d_%d" % (g, h)
                        tt_("dve", ysl, ysl, bk[:], ALU.add, [bkk, yk, "yz0", "yz1"], [yk])

            for si, (e_, tt) in enumerate(steps):
                slot = e_ % NSLOT
                tcols = slice(tt * 512, (tt + 1) * 512)
                fl = tt * 4 + e_ // 4
                if e_ == 0:
                    for q in range(tt * 4, tt * 4 + 4):
                        trans_q(q)
                        if q + NXG < 16:
                            gather_q(q + NXG)
                    if tt == 1:
                        load_expert(2)
                if tt == 1 and e_ >= 1 and e_ + 2 < NE:
                    load_expert(e_ + 2)
                if tt == 2 and e_ == NE - 2:
                    S.dma("pool", "d_wob", wob[:], w_pg_v, writes=["wob"])
                S.cond_begin(fl)
                xk = ["x1T_%d" % (tt * 4 + j) for j in range(4)]
                ck = ["combT_%d" % (tt * 4 + j) for j in range(4)]
                bcb, bcbk = nb()
                mm(bcb[:, :], sel[:, e_, :], combT[:, tcols], True, True, ["sel"] + ck, [bcbk])
                pg = []
                pu = []
                for fc in range(2):
                    bk, bkk = nb()
                    for dc in range(8):
                        mm(bk[:, :], wgu[slot][:, dc, fc * 128:(fc + 1) * 128], x1T[:, dc, tcols], dc == 0, dc == 7,
                           ["wgu%d" % slot] + xk, [bkk])
                    pg.append((bk, bkk))
                for fc in range(2):
                    bk, bkk = nb()
                    for dc in range(8):
                        mm(bk[:, :], wgu[slot][:, dc, DE + fc * 128:DE + (fc + 1) * 128], x1T[:, dc, tcols], dc == 0, dc == 7,
                           ["wgu%d" % slot] + xk, [bkk])
                    pu.append((bk, bkk))
                cb = cbs[si % 2]
                cbk = "cbs%d" % (si % 2)
                cp("dve", cb[:], bcb[:], [bcbk], [cbk])
                hslot = (NXG + tt % 2) if e_ == 0 else si % NHID
                for fc in range(2):
                    u = uctr % NSS
                    uctr += 1
                    act(sbuf_s[u][:], pg[fc][0][:], AF.Silu, [pg[fc][1]], ["ss%d" % u])
                    tt_("dve", sbuf_t[u][:], pu[fc][0][:], cb[:], ALU.mult, [pu[fc][1], cbk], ["st%d" % u])
                    tt_("pool", hid[hslot][:, fc, :], sbuf_s[u][:], sbuf_t[u][:], ALU.mult, ["ss%d" % u, "st%d" % u],
                        hidk(hslot))
                pend_b[(e_, tt)] = (e_, tt, hslot, fl)
                if e_ == 0:
                    prev = (0, tt - 1) if tt >= 1 else None
                elif e_ == 1:
                    prev = (0, 3) if tt == 0 else None
                else:
                    prev = (e_ - 1, tt)
                if e_ == 1 and tt == 3:
                    prev2 = None
                if prev is not None and pend_b[prev][3] == fl:
                    pb_ = pend_b.pop(prev)
                    down(*pb_[0:3])
                    S.cond_end()
                else:
                    S.cond_end()
                    if prev is not None:
                        pb_ = pend_b.pop(prev)
                        S.cond_begin(pb_[3])
                        down(*pb_[0:3])
                        S.cond_end()
            for e_r in (1,):
                pass
            for key in sorted(pend_b.keys()):
                pb_ = pend_b.pop(key)
                S.cond_begin(pb_[3])
                down(*pb_[0:3])
                S.cond_end()
            nbanks[0] = 8
            S.barrier()
            S.emit()
        sM.close()
        if stop_after == "2":
            return nc

        with contextlib.ExitStack() as s4:
            wpg = wob
            wpp = sb(s4, "wpp", [128, 2, D], BF16)
            bc_g2 = sb(s4, "bc_g2", [128, 1024], F32)
            bc_b2 = sb(s4, "bc_b2", [128, 1024], F32)
            N3 = 3
            x1r = [sb(s4, "x1r%d" % i, [128, 1024], F32) for i in range(N3)]
            pt = [sb(s4, "pt%d" % i, [128, DPLE], F32) for i in range(N3)]
            rb = [sb(s4, "rb%d" % i, [128, 1024], BF16) for i in range(N3)]
            pb16 = [sb(s4, "pb16_%d" % i, [128, DPLE], BF16) for i in range(N3)]
            rT = [sb(s4, "rT%d" % i, [128, 8, 128], BF16) for i in range(N3)]
            pT = [sb(s4, "pT%d" % i, [128, 2, 128], BF16) for i in range(N3)]
            gt = [sb(s4, "gt%d" % i, [128, 512], F32) for i in range(4)]
            gpt = [sb(s4, "gpt%d" % i, [128, 512], F32) for i in range(4)]
            NOB = 6
            ob = [sb(s4, "ob%d" % i, [128, 1024], F32) for i in range(NOB)]
            st2 = sb(s4, "st2", [128, 16, 2, 6], F32)
            mv2 = sb(s4, "mv2", [128, 16, 2], F32)
            rs2 = sb(s4, "rs2", [128, 16], F32)
            nb2 = sb(s4, "nb2", [128, 16], F32)

            S.dma("pool", "d_wpp", wpp[:], w_pp_v, writes=["wpp"])
            S.dma("sp", None, bc_g2[:], rows[:, R_L2G:R_L2G + 1024].partition_broadcast(128), writes=["bc_g2"])
            S.dma("sp", None, bc_b2[:], rows[:, R_L2B:R_L2B + 1024].partition_broadcast(128), writes=["bc_b2"])
            hctr = [0]

            pbanks = {}

            def c_dma(g):
                k = g % N3
                S.dma_fn("pool", "d_x1r%d" % k, lambda eng: eng.indirect_dma_start(
                    out=x1r[k][:, :], out_offset=None, in_=x1_dram[:, :],
                    in_offset=bass.IndirectOffsetOnAxis(ap=inv_sb[:, g:g + 1], axis=0)), reads=[], writes=["x1r%d" % k])
                S.dma_fn("pool", "d_pt%d" % k, lambda eng: eng.indirect_dma_start(
                    out=pt[k][:, :], out_offset=None, in_=ps[:, :],
                    in_offset=bass.IndirectOffsetOnAxis(ap=inv_sb[:, g:g + 1], axis=0)), reads=[], writes=["pt%d" % k])

            def c_r(g):
                k = g % N3
                yk = ["y_%d_0" % g, "y_%d_1" % g]
                stt(yacc[:, g, :], x1r[k][:], ALPHA, yacc[:, g, :], ALU.mult, ALU.add, ["x1r%d" % k] + yk, yk)
                cp("dve", pb16[k][:], pt[k][:], ["pt%d" % k], ["pb16_%d" % k])

            def c_rb(g):
                k = g % N3
                yk = ["y_%d_0" % g, "y_%d_1" % g]
                acp(rb[k][:], yacc[:, g, :], yk, ["rb%d" % k])
                bk2, bkk2 = nb()
                bkb2 = bk2[:].bitcast(BF16)
                for kc in range(2):
                    tp(bkb2[:, kc * 128:(kc + 1) * 128], pb16[k][:, kc * 128:(kc + 1) * 128], idb[:], ["pb16_%d" % k, "idb"], [bkk2])
                pbanks[("p", g)] = (bkb2, bkk2)

            def c_tr(g):
                k = g % N3
                bkb2, bkk2 = pbanks.pop(("p", g))
                cp("dve", pT[k][:], bkb2[:, 0:256].rearrange("p (c t) -> p c t", c=2), [bkk2], ["pT%d" % k])
                bk, bkk = nb()
                bkb = bk[:].bitcast(BF16)
                for dc in range(8):
                    tp(bkb[:, dc * 128:(dc + 1) * 128], rb[k][:, dc * 128:(dc + 1) * 128], idb[:], ["rb%d" % k, "idb"], [bkk])
                pbanks[("r", g)] = (bkb, bkk)

            def c_rT(g):
                k = g % N3
                bkb, bkk = pbanks.pop(("r", g))
                acp(rT[k][:], bkb.rearrange("p (c t) -> p c t", c=8), [bkk], ["rT%d" % k])

            def c_mm(g):
                k = g % N3
                for h in range(2):
                    hs = slice(h * 512, (h + 1) * 512)
                    bg_, bgk = nb()
                    for dc in range(8):
                        mm(bg_[:, :], rT[k][:, dc, :], wpg[:, dc, hs], dc == 0, False, ["rT%d" % k, "wob"], [bgk])
                    mm(bg_[:, :], ones_rb[0:1, :], brow_b[0:1, 1024 + h * 512:1024 + (h + 1) * 512], False, True,
                       ["ones_rb", "brow"], [bgk])
                    bp_, bpk = nb()
                    for kc in range(2):
                        mm(bp_[:, :], pT[k][:, kc, :], wpp[:, kc, hs], kc == 0, kc == 1, ["pT%d" % k, "wpp"], [bpk])
                    u = hctr[0] % 4
                    hctr[0] += 1
                    act(gt[u][:], bg_[:], AF.Sigmoid, [bgk], ["gt%d" % u])
                    pbanks[("g", g, h)] = (u, bp_, bpk)

            def c_gp(g):
                for h in range(2):
                    u, bp_, bpk = pbanks.pop(("g", g, h))
                    tt_("dve", gpt[u][:], gt[u][:], bp_[:], ALU.mult, ["gt%d" % u, bpk], ["gpt%d" % u])
                    pbanks[("a", g, h)] = u

            def c_add(g):
                for h in range(2):
                    hs = slice(h * 512, (h + 1) * 512)
                    u = pbanks.pop(("a", g, h))
                    tt_("pool", yacc[:, g, hs], yacc[:, g, hs], gpt[u][:], ALU.add, ["y_%d_%d" % (g, h), "gpt%d" % u],
                        ["y_%d_%d" % (g, h)])

            def c_st(g):
                V(lambda e: e.bn_stats(st2[:, g, 0, :], yacc[:, g, 0:512]), ["y_%d_0" % g], ["st2a%d" % g])
                V(lambda e: e.bn_stats(st2[:, g, 1, :], yacc[:, g, 512:1024]), ["y_%d_1" % g], ["st2b%d" % g])
                V(lambda e: e.bn_aggr(mv2[:, g, :], st2[:, g, :, :]), ["st2a%d" % g, "st2b%d" % g], ["mv2_%d" % g])

            def c_sqrt(G):
                g0 = G * 4
                act(rs2[:, g0:g0 + 4], mv2[:, g0:g0 + 4, 1], AF.Sqrt, ["mv2_%d" % (g0 + j) for j in range(4)],
                    ["rs2a%d" % g0], bias=EPS)

            def c_rcp(G):
                g0 = G * 4
                V(lambda e: e.reciprocal(rs2[:, g0:g0 + 4], rs2[:, g0:g0 + 4]), ["rs2a%d" % g0], ["rs2_%d" % g0])
                V(lambda e: e.scalar_tensor_tensor(nb2[:, g0:g0 + 4], mv2[:, g0:g0 + 4, 0], -1.0, rs2[:, g0:g0 + 4],
                                                   ALU.mult, ALU.mult),
                  ["rs2_%d" % g0] + ["mv2_%d" % (g0 + j) for j in range(4)], ["nb2_%d" % g0])

            def c_on(g):
                k = g % NOB
                g0 = g - g % 4
                yk = ["y_%d_0" % g, "y_%d_1" % g]
                act(ob[k][:], yacc[:, g, :], AF.Identity, yk + ["nb2_%d" % g0, "rs2_%d" % g0], ["ob%d" % k],
                    bias=nb2[:, g:g + 1], scale=rs2[:, g:g + 1])

            def c_og(g):
                k = g % NOB
                tt_("pool", ob[k][:], ob[k][:], bc_g2[:], ALU.mult, ["ob%d" % k, "bc_g2"], ["ob%d" % k])

            def c_ob(g):
                k = g % NOB
                tt_("dve", ob[k][:], ob[k][:], bc_b2[:], ALU.add, ["ob%d" % k, "bc_b2"], ["ob%d" % k])
                S.dma_fn("pool", "d_out%d" % k, lambda eng: eng.indirect_dma_start(
                    out=out[:, :], out_offset=bass.IndirectOffsetOnAxis(ap=inv_sb[:, g:g + 1], axis=0),
                    in_=ob[k][:, :], in_offset=None), reads=["ob%d" % k], writes=["outd%d" % g])

            def tail(off):
                return lambda g: off - (g - 12) if g >= 12 else off

            def grp3(fn, off):
                return (off, lambda g: fn(g // 4) if g % 4 == 3 else None)

            pipeline([(0, c_dma), (1, c_r), (2, c_rb), (3, c_tr), (4, c_rT), (6, c_gp), (7, c_add), (8, c_st),
                      grp3(c_sqrt, 9), grp3(c_rcp, 10), (tail(14), c_on), (tail(15), c_og), (tail(16), c_ob), (5, c_mm)], 16)
            S.final_wait("sp", ["d_out%d" % k for k in range(NOB)])
            S.emit()
    return nc


_NC_CACHE = {}


def _prep_inputs(inp):
    f = lambda a: np.ascontiguousarray(np.asarray(a, dtype=np.float32))
    x = f(inp["x"])
    p = f(inp["p"])[0]
    w_in = f(inp["w_in"])[0]
    b_in = f(inp["b_in"])[0]
    perm = []
    for i in range(4):
        perm += list(range(i * 128, (i + 1) * 128)) + list(range(512 + i * 128, 512 + (i + 1) * 128))
    for i in range(4):
        perm += list(range(1536 + i * 128, 1536 + (i + 1) * 128))
        perm += list(range(2048 + i * 128, 2048 + (i + 1) * 128))
        perm += list(range(1024 + i * 128, 1024 + (i + 1) * 128))
    perm = np.array(perm)
    w_in_p = np.ascontiguousarray(w_in[:, perm])
    b_in_p = b_in[perm].reshape(20, 128).T
    col = lambda v, n: f(v).reshape(n, 128).T
    dww = f(inp["conf_dw_w"])[0]
    dww_p = dww.T.reshape(4, 128, 31).transpose(1, 0, 2).reshape(128, 124)
    scw = f(inp["sc_w"])[0]
    scw_p = scw.T.reshape(4, 128, 3).transpose(1, 0, 2).reshape(128, 12)
    rows = np.concatenate([f(inp["ln_in_g"]), f(inp["ln_in_b"]), f(inp["ln1_g"])[0], f(inp["ln1_b"])[0],
                           f(inp["ln2_g"])[0], f(inp["ln2_b"])[0], f(inp["b_out"])[0], f(inp["b_pg"])[0]])[None, :]
    w_r = np.ascontiguousarray(np.concatenate([f(inp["w_rg"])[0], f(inp["w_re"])[0]], axis=1))
    b_r = np.concatenate([f(inp["b_rg"])[0], f(inp["b_re"])[0]])[:, None]
    shared = {
        "rows": np.ascontiguousarray(rows), "w_in": w_in_p, "w_out": f(inp["w_out"])[0], "w_r": w_r,
        "b_r": np.ascontiguousarray(b_r), "w_gu": np.ascontiguousarray(np.concatenate([f(inp["w_gate"])[0], f(inp["w_up"])[0]], axis=-1)),
        "w_down": f(inp["w_down"])[0], "w_pg": f(inp["w_pg"])[0], "w_pp": f(inp["w_pp"])[0],
        "ident": np.eye(128, dtype=np.float32),
        "tri": np.triu(np.ones((128, 128), np.float32), k=1),
        "tid": (np.arange(16, dtype=np.int32)[None, :] * 128 + np.arange(128, dtype=np.int32)[:, None]).astype(np.int32),
    }
    maps = []
    for c in range(NCORES):
        b, q = divmod(c, 4)
        lo = q * NT
        if q == 0:
            halo = np.zeros((HALO, D), np.float32)
            mask = 0.0
        else:
            halo = x[b, lo - HALO:lo]
            mask = 1.0
        xs = np.ascontiguousarray(np.concatenate([halo, x[b, lo:lo + NT]], axis=0))
        pp = np.concatenate([b_in_p, col(inp["ln_in_g"], 8), col(inp["ln_in_b"], 8), dww_p,
                             col(f(inp["conf_dw_b"])[0], 4), col(f(inp["conf_ln_g"])[0], 4), col(f(inp["conf_ln_b"])[0], 4),
                             scw_p, col(f(inp["sc_b"])[0], 4), np.full((128, 1), mask, np.float32)], axis=1)
        m = dict(shared)
        m["xs"] = xs
        m["ps"] = np.ascontiguousarray(p[b, lo:lo + NT])
        m["pp"] = np.ascontiguousarray(pp.astype(np.float32))
        maps.append(m)
    return maps


def kernel(**inputs):
    if "nc" not in _NC_CACHE:
        _NC_CACHE["nc"] = build_nc()
    nc = _NC_CACHE["nc"]
    maps = _prep_inputs(inputs)
    res = run_bass_kernel_spmd(nc, maps, core_ids=list(range(NCORES)))
    outs = [np.asarray(r["out"], dtype=np.float32) for r in res.results]
    full = np.stack(outs, axis=0).reshape(2, 4 * NT, D)
    return full
```
